# Optimizing a Trainium2 kernel written in Bass

```python
import jax
import jax.numpy as jnp
from jax import lax
import numpy as np

D_MODEL = 1024
BATCH = 8
SEQ = 4096
DEPTH = 1

N_MEM = 256
EPS = 1e-6
NEG_INF = -1e30
SEL_FORCE = 1e4

R_HEADS = 4
R_DK = 128
R_DV = 256
R_CHUNK = 128
ROPE_BASE = 10000.0

NSA_HEADS = 8
NSA_GROUPS = 2
NSA_HPG = NSA_HEADS // NSA_GROUPS
NSA_DH = 128
CMP_LEN = 32
CMP_STRIDE = 16
SEL_LEN = 64
SEL_TOPK = 16
SEL_QCHUNK = 32
WINDOW = 512
WIN_QBLOCK = 128

X_HEADS = 4
X_DH = D_MODEL // X_HEADS

N_EGROUPS = 4
EXP_PER_GROUP = 8
N_EXPERTS = N_EGROUPS * EXP_PER_GROUP
EXP_TOPK = 2
D_EXPERT = 512
MOE_BLOCK = 256

RET_QK = R_HEADS * R_DK
RET_V = R_HEADS * R_DV
NSA_Q = NSA_HEADS * NSA_DH
NSA_KV = NSA_GROUPS * NSA_DH
SPLITS = (RET_QK, RET_QK, RET_V, RET_V, NSA_Q, NSA_KV, NSA_KV, NSA_KV, NSA_KV, NSA_KV, NSA_KV,
          3 * NSA_HEADS, D_MODEL, D_MODEL)
D_IN = sum(SPLITS)

kernel_name = 'hybrid_retention_nsa_hmoe_block'


def rmsnorm(x, g):
    xf = x.astype(jnp.float32)
    y = xf * lax.rsqrt(jnp.mean(xf * xf, axis=-1, keepdims=True) + EPS)
    return (y * g.astype(jnp.float32)).astype(x.dtype)


def masked_softmax(s, mask):
    p = jax.nn.softmax(jnp.where(mask, s, NEG_INF), axis=-1)
    return jnp.where(jnp.any(mask, axis=-1, keepdims=True), p, 0.0)


def rotate_pairs(t, cos, sin):
    t1 = t[..., 0::2]
    t2 = t[..., 1::2]
    out = jnp.stack([t1 * cos - t2 * sin, t1 * sin + t2 * cos], axis=-1)
    return out.reshape(t.shape).astype(t.dtype)


def retention(q, k, v, g):
    bsz, seq = q.shape[0], q.shape[1]
    nc = seq // R_CHUNK
    pos = jnp.arange(seq, dtype=jnp.float32)
    inv_freq = ROPE_BASE ** (-jnp.arange(0, R_DK, 2, dtype=jnp.float32) / R_DK)
    ang = pos[:, None] * inv_freq[None, :]
    cos = jnp.cos(ang)[None, :, None, :]
    sin = jnp.sin(ang)[None, :, None, :]
    q = rotate_pairs(q, cos, sin)
    k = rotate_pairs(k, cos, sin) * (R_DK ** -0.5)
    log_g = jnp.log1p(-jnp.exp2(-5.0 - jnp.arange(R_HEADS, dtype=jnp.float32)))
    n = jnp.arange(R_CHUNK, dtype=jnp.float32)
    diff = n[:, None] - n[None, :]
    decay = jnp.where(diff >= 0, jnp.exp(log_g[:, None, None] * jnp.maximum(diff, 0.0)), 0.0)
    qc = q.reshape(bsz, nc, R_CHUNK, R_HEADS, R_DK)
    kc = k.reshape(bsz, nc, R_CHUNK, R_HEADS, R_DK)
    vc = v.reshape(bsz, nc, R_CHUNK, R_HEADS, R_DV)
    scores = jnp.einsum('bcnhd,bcmhd->bchnm', qc, kc) * decay[None, None]
    inner = jnp.einsum('bchnm,bcmhe->bcnhe', scores, vc)
    zeta = jnp.exp(log_g[:, None] * (R_CHUNK - 1.0 - n)[None, :])
    kv = jnp.einsum('bcmhd,hm,bcmhe->bchde', kc, zeta, vc)
    chunk_decay = jnp.exp(log_g * R_CHUNK).astype(kv.dtype)

    def step(state, kv_c):
        return state * chunk_decay[None, :, None, None] + kv_c, state

    _, prev = lax.scan(step, jnp.zeros((bsz, R_HEADS, R_DK, R_DV), kv.dtype), jnp.moveaxis(kv, 1, 0))
    prev = jnp.moveaxis(prev, 0, 1)
    xi = jnp.exp(log_g[:, None] * (n + 1.0)[None, :])
    cross = jnp.einsum('bcnhd,bchde->bcnhe', qc, prev) * xi.T[None, None, :, :, None]
    o = (inner + cross).reshape(bsz, seq, R_HEADS, R_DV).astype(jnp.float32)
    o = o * lax.rsqrt(jnp.mean(o * o, axis=-1, keepdims=True) + EPS)
    return jax.nn.silu(g) * o.reshape(bsz, seq, RET_V).astype(g.dtype)


def compress_blocks(kv, pe, w1, w2):
    seq = kv.shape[1]
    ncmp = (seq - CMP_LEN) // CMP_STRIDE + 1
    idx = jnp.arange(ncmp)[:, None] * CMP_STRIDE + jnp.arange(CMP_LEN)[None, :]
    blocks = kv[:, idx] + pe[None, None, :, None, :]
    hid = jax.nn.gelu(jnp.einsum('bnlgd,lde->bnge', blocks, w1))
    return jnp.einsum('bnge,ef->bngf', hid, w2)


def gather_blocks(blocks, idx):
    return jax.vmap(jax.vmap(lambda blk, ix: blk[ix]))(blocks, idx)


def nsa(q, k_c, v_c, k_s, v_s, k_w, v_w, gate_logits, pe_k, w1_k, w2_k, pe_v, w1_v, w2_v):
    bsz, seq = q.shape[0], q.shape[1]
    scale = NSA_DH ** -0.5
    qg = q.reshape(bsz, seq, NSA_GROUPS, NSA_HPG, NSA_DH)
    shp = (bsz, seq, NSA_GROUPS, NSA_DH)
    k_c, v_c, k_s, v_s, k_w, v_w = [a.reshape(shp) for a in (k_c, v_c, k_s, v_s, k_w, v_w)]
    t = jnp.arange(seq)

    kcmp = compress_blocks(k_c, pe_k, w1_k, w2_k)
    vcmp = compress_blocks(v_c, pe_v, w1_v, w2_v)
    ncmp = kcmp.shape[1]
    cstart = jnp.arange(ncmp) * CMP_STRIDE
    cmask = (cstart + CMP_LEN - 1)[None, :] <= t[:, None]
    s_c = jnp.einsum('bsghd,bngd->bghsn', qg, kcmp).astype(jnp.float32) * scale
    p_c = masked_softmax(s_c, cmask)
    o_cmp = jnp.einsum('bghsn,bngd->bsghd', p_c.astype(q.dtype), vcmp)

    nb = seq // SEL_LEN
    jstart = jnp.arange(nb) * SEL_LEN
    overlap = ((cstart[:, None] < jstart[None, :] + SEL_LEN) &
               (cstart[:, None] + CMP_LEN > jstart[None, :])).astype(jnp.float32)
    imp = jnp.einsum('bghsn,nj->bgsj', p_c, overlap)
    tb = t // SEL_LEN
    jj = jnp.arange(nb)
    forced = (jj[None, :] == 0) | (jj[None, :] == tb[:, None]) | (jj[None, :] == tb[:, None] - 1)
    future = jj[None, :] > tb[:, None]
    imp = jnp.where(future, -SEL_FORCE, jnp.where(forced, SEL_FORCE, imp))
    k_sel = min(SEL_TOPK, nb)
    _, sel_idx = lax.top_k(imp, k_sel)

    ksb = k_s.reshape(bsz, nb, SEL_LEN, NSA_GROUPS, NSA_DH).transpose(0, 3, 1, 2, 4)
    vsb = v_s.reshape(bsz, nb, SEL_LEN, NSA_GROUPS, NSA_DH).transpose(0, 3, 1, 2, 4)
    nqc = seq // SEL_QCHUNK
    q_chunks = jnp.moveaxis(qg.reshape(bsz, nqc, SEL_QCHUNK, NSA_GROUPS, NSA_HPG, NSA_DH), 1, 0)
    idx_chunks = jnp.moveaxis(sel_idx.reshape(bsz, NSA_GROUPS, nqc, SEL_QCHUNK, k_sel), 2, 0)
    q_starts = jnp.arange(nqc) * SEL_QCHUNK

    def sel_body(args):
        qc, ix, st = args
        kg = gather_blocks(ksb, ix)
        vg = gather_blocks(vsb, ix)
        s = jnp.einsum('bqghd,bgqnld->bghqnl', qc, kg).astype(jnp.float32) * scale
        kpos = ix[..., None] * SEL_LEN + jnp.arange(SEL_LEN)
        tq = st + jnp.arange(SEL_QCHUNK)
        m = (kpos <= tq[None, None, :, None, None])[:, :, None]
        s = jnp.where(m, s, NEG_INF).reshape(bsz, NSA_GROUPS, NSA_HPG, SEL_QCHUNK, k_sel * SEL_LEN)
        p = jax.nn.softmax(s, axis=-1).reshape(bsz, NSA_GROUPS, NSA_HPG, SEL_QCHUNK, k_sel, SEL_LEN)
        return jnp.einsum('bghqnl,bgqnld->bqghd', p.astype(qc.dtype), vg)

    o_sel = lax.map(sel_body, (q_chunks, idx_chunks, q_starts))
    o_sel = jnp.moveaxis(o_sel, 0, 1).reshape(bsz, seq, NSA_GROUPS, NSA_HPG, NSA_DH)

    kpad = jnp.pad(k_w, ((0, 0), (WINDOW, 0), (0, 0), (0, 0)))
    vpad = jnp.pad(v_w, ((0, 0), (WINDOW, 0), (0, 0), (0, 0)))
    nwb = seq // WIN_QBLOCK
    q_blocks = jnp.moveaxis(qg.reshape(bsz, nwb, WIN_QBLOCK, NSA_GROUPS, NSA_HPG, NSA_DH), 1, 0)

    def win_body(args):
        qb, i = args
        st = i * WIN_QBLOCK
        kb = lax.dynamic_slice_in_dim(kpad, st, WINDOW + WIN_QBLOCK, axis=1)
        vb = lax.dynamic_slice_in_dim(vpad, st, WINDOW + WIN_QBLOCK, axis=1)
        s = jnp.einsum('bqghd,bkgd->bghqk', qb, kb).astype(jnp.float32) * scale
        kp = st - WINDOW + jnp.arange(WINDOW + WIN_QBLOCK)
        tq = st + jnp.arange(WIN_QBLOCK)
        m = (kp[None, :] <= tq[:, None]) & (kp[None, :] > tq[:, None] - WINDOW) & (kp[None, :] >= 0)
        p = jax.nn.softmax(jnp.where(m, s, NEG_INF), axis=-1)
        return jnp.einsum('bghqk,bkgd->bqghd', p.astype(qb.dtype), vb)

    o_win = lax.map(win_body, (q_blocks, jnp.arange(nwb)))
    o_win = jnp.moveaxis(o_win, 0, 1).reshape(bsz, seq, NSA_GROUPS, NSA_HPG, NSA_DH)

    gates = jax.nn.sigmoid(gate_logits.astype(jnp.float32)).reshape(bsz, seq, NSA_GROUPS, NSA_HPG, 3)
    gates = gates.astype(q.dtype)
    o = gates[..., 0, None] * o_cmp + gates[..., 1, None] * o_sel + gates[..., 2, None] * o_win
    return o.reshape(bsz, seq, NSA_Q)


def cross_attention(h, m, w_q, w_kv, w_o):
    bsz, seq = h.shape[0], h.shape[1]
    nmem = m.shape[1]
    q = (h @ w_q).reshape(bsz, seq, X_HEADS, X_DH)
    k, v = jnp.split(m @ w_kv, 2, axis=-1)
    k = k.reshape(bsz, nmem, X_HEADS, X_DH)
    v = v.reshape(bsz, nmem, X_HEADS, X_DH)
    s = jnp.einsum('bshd,bmhd->bhsm', q, k).astype(jnp.float32) * (X_DH ** -0.5)
    p = jax.nn.softmax(s, axis=-1).astype(h.dtype)
    o = jnp.einsum('bhsm,bmhd->bshd', p, v).reshape(bsz, seq, D_MODEL)
    return o @ w_o


def hier_moe(h, w_grp, b_grp, w_rt, b_rt, w1, w3, w2):
    n_tok = h.shape[0]
    lg = (h @ w_grp + b_grp).astype(jnp.float32)
    pg = jax.nn.softmax(lg, axis=-1)
    grp = jnp.argmax(lg, axis=-1)
    g_gate = jnp.take_along_axis(pg, grp[:, None], axis=-1)[:, 0]
    le = (h @ w_rt + b_rt).astype(jnp.float32).reshape(n_tok, N_EGROUPS, EXP_PER_GROUP)
    le_g = jnp.take_along_axis(le, grp[:, None, None], axis=1)[:, 0]
    pe = jax.nn.softmax(le_g, axis=-1)
    top_p, top_i = lax.top_k(pe, EXP_TOPK)
    wts = g_gate[:, None] * top_p / jnp.sum(top_p, axis=-1, keepdims=True)
    eid = (grp[:, None] * EXP_PER_GROUP + top_i).reshape(-1).astype(jnp.int32)
    wflat = wts.reshape(-1)
    n_asg = n_tok * EXP_TOPK
    tok = (jnp.arange(n_asg) // EXP_TOPK).astype(jnp.int32)
    order = jnp.argsort(eid)
    se = eid[order]
    counts = jnp.bincount(eid, length=N_EXPERTS)
    padded = (counts + MOE_BLOCK - 1) // MOE_BLOCK * MOE_BLOCK
    starts = jnp.cumsum(counts) - counts
    pends = jnp.cumsum(padded)
    pstarts = pends - padded
    dest = pstarts[se] + (jnp.arange(n_asg) - starts[se])
    cap = ((n_asg + MOE_BLOCK - 1) // MOE_BLOCK + N_EXPERTS) * MOE_BLOCK
    nblk = cap // MOE_BLOCK
    buf_tok = jnp.full((cap,), n_tok, jnp.int32).at[dest].set(tok[order])
    buf_w = jnp.zeros((cap,), h.dtype).at[dest].set(wflat[order].astype(h.dtype))
    blk_e = jnp.minimum(jnp.searchsorted(pends, jnp.arange(nblk) * MOE_BLOCK, side='right'), N_EXPERTS - 1)
    hp = jnp.concatenate([h, jnp.zeros((1, h.shape[1]), h.dtype)], axis=0)

    def body(args):
        e, tk, wb = args
        xb = hp[tk]
        y = (jax.nn.silu(xb @ w1[e]) * (xb @ w3[e])) @ w2[e]
        return y * wb[:, None]

    ys = lax.map(body, (blk_e, buf_tok.reshape(nblk, MOE_BLOCK), buf_w.reshape(nblk, MOE_BLOCK)))
    out = jnp.zeros((n_tok + 1, h.shape[1]), h.dtype).at[buf_tok].add(ys.reshape(cap, h.shape[1]))
    return out[:n_tok]


def setup_inputs(seed: int = 0) -> dict:
    key = jax.random.key(seed)
    ks = jax.random.split(key, 32)
    f32 = jnp.float32

    def nrm(k, shape, fan_in):
        return jax.random.normal(k, shape, f32) * (fan_in ** -0.5)

    def gain(k, shape):
        return 1.0 + 0.02 * jax.random.normal(k, shape, f32)

    L = DEPTH
    return {
        'x': jax.random.normal(ks[0], (BATCH, SEQ, D_MODEL), f32),
        'mem': jax.random.normal(ks[1], (BATCH, N_MEM, D_MODEL), f32),
        'norm_mix_g': gain(ks[2], (L, D_MODEL)),
        'w_in': nrm(ks[3], (L, D_MODEL, D_IN), D_MODEL),
        'w_ret_o': nrm(ks[4], (L, RET_V, D_MODEL), RET_V),
        'w_nsa_o': nrm(ks[5], (L, NSA_Q, D_MODEL), NSA_Q),
        'w_out': nrm(ks[6], (L, D_MODEL, D_MODEL), D_MODEL),
        'cmp_pe_k': 0.1 * jax.random.normal(ks[7], (L, CMP_LEN, NSA_DH), f32),
        'cmp_w1_k': nrm(ks[8], (L, CMP_LEN, NSA_DH, NSA_DH), CMP_LEN * NSA_DH),
        'cmp_w2_k': nrm(ks[9], (L, NSA_DH, NSA_DH), NSA_DH),
        'cmp_pe_v': 0.1 * jax.random.normal(ks[10], (L, CMP_LEN, NSA_DH), f32),
        'cmp_w1_v': nrm(ks[11], (L, CMP_LEN, NSA_DH, NSA_DH), CMP_LEN * NSA_DH),
        'cmp_w2_v': nrm(ks[12], (L, NSA_DH, NSA_DH), NSA_DH),
        'norm_x_g': gain(ks[13], (L, D_MODEL)),
        'norm_mem_g': gain(ks[14], (L, D_MODEL)),
        'w_xq': nrm(ks[15], (L, D_MODEL, D_MODEL), D_MODEL),
        'w_xkv': nrm(ks[16], (L, D_MODEL, 2 * D_MODEL), D_MODEL),
        'w_xo': nrm(ks[17], (L, D_MODEL, D_MODEL), D_MODEL),
        'norm_ffn_g': gain(ks[18], (L, D_MODEL)),
        'w_grp': nrm(ks[19], (L, D_MODEL, N_EGROUPS), D_MODEL),
        'b_grp': 0.01 * jax.random.normal(ks[20], (L, N_EGROUPS), f32),
        'w_rt': nrm(ks[21], (L, D_MODEL, N_EXPERTS), D_MODEL),
        'b_rt': 0.01 * jax.random.normal(ks[22], (L, N_EXPERTS), f32),
        'w_e1': nrm(ks[23], (L, N_EXPERTS, D_MODEL, D_EXPERT), D_MODEL),
        'w_e3': nrm(ks[24], (L, N_EXPERTS, D_MODEL, D_EXPERT), D_MODEL),
        'w_e2': nrm(ks[25], (L, N_EXPERTS, D_EXPERT, D_MODEL), D_EXPERT),
        'norm_f_g': gain(ks[26], (D_MODEL,)),
    }


def reference(x, mem, norm_mix_g, w_in, w_ret_o, w_nsa_o, w_out, cmp_pe_k, cmp_w1_k, cmp_w2_k,
              cmp_pe_v, cmp_w1_v, cmp_w2_v, norm_x_g, norm_mem_g, w_xq, w_xkv, w_xo, norm_ffn_g,
              w_grp, b_grp, w_rt, b_rt, w_e1, w_e3, w_e2, norm_f_g):
    bsz, seq = x.shape[0], x.shape[1]
    split_points = np.cumsum(SPLITS)[:-1].tolist()
    for l in range(DEPTH):
        h = rmsnorm(x, norm_mix_g[l])
        proj = h @ w_in[l]
        (rq, rk, rv, rg, nq, ck, cv, sk, sv, wk, wv, ngl, ga, gb) = jnp.split(proj, split_points, axis=-1)
        y_ret = retention(rq.reshape(bsz, seq, R_HEADS, R_DK), rk.reshape(bsz, seq, R_HEADS, R_DK),
                          rv.reshape(bsz, seq, R_HEADS, R_DV), rg) @ w_ret_o[l]
        y_nsa = nsa(nq, ck, cv, sk, sv, wk, wv, ngl, cmp_pe_k[l], cmp_w1_k[l], cmp_w2_k[l],
                    cmp_pe_v[l], cmp_w1_v[l], cmp_w2_v[l]) @ w_nsa_o[l]
        y = jax.nn.sigmoid(ga) * y_ret + jax.nn.sigmoid(gb) * y_nsa
        x = x + y @ w_out[l]
        x = x + cross_attention(rmsnorm(x, norm_x_g[l]), rmsnorm(mem, norm_mem_g[l]),
                                w_xq[l], w_xkv[l], w_xo[l])
        hf = rmsnorm(x, norm_ffn_g[l]).reshape(bsz * seq, D_MODEL)
        x = x + hier_moe(hf, w_grp[l], b_grp[l], w_rt[l], b_rt[l], w_e1[l], w_e3[l], w_e2[l]).reshape(bsz, seq, D_MODEL)
    return rmsnorm(x, norm_f_g)
```

```python
import contextlib
import os as _os
import numpy as np
import ml_dtypes
import concourse.bass as bass
import concourse.mybir as mybir
from concourse.bass_utils import run_bass_kernel_spmd

F32 = mybir.dt.float32
BF16 = mybir.dt.bfloat16
I32 = mybir.dt.int32
U32 = mybir.dt.uint32
ALU = mybir.AluOpType
AF = mybir.ActivationFunctionType
AX = mybir.AxisListType

S = 4096
D = 1024
NT = S // 128
NQ = S // 512
EPS = 1e-6
NEG = -30000.0
D_IN = 7704
TM_COLS = 3608
FM_COLS = 4096
R_HEADS = 4
GAMMAS = [1.0 - 2.0 ** (-5.0 - h) for h in range(R_HEADS)]
NCMP = 255
NB = S // 64
CAP = 384
NEXP = 32


class Buf:
    __slots__ = ("w", "r", "multi", "name", "excl")

    def __init__(self, name="", multi=False, excl=False):
        self.excl = excl
        self.w = {}
        self.r = {}
        self.multi = multi
        self.name = name


class Sched:
    def __init__(self, nc):
        self.nc = nc
        self.es = contextlib.ExitStack()
        self.eng = {"pe": nc.tensor, "act": nc.scalar, "dve": nc.vector,
                    "pool": nc.gpsimd, "sp": nc.sync}
        self.sem = {k: self.es.enter_context(nc.semaphore("s_" + k)) for k in self.eng}
        self.cnt = {k: 0 for k in self.eng}
        self.waited = {k: {} for k in self.eng}
        self.ndma = 24
        self.dsem = [self.es.enter_context(nc.semaphore("d%d" % i)) for i in range(self.ndma)]
        self.dcnt = [0] * self.ndma
        self.dnext = 0
        self.nst = 16
        self.ssem = [self.es.enter_context(nc.semaphore("st%d" % i)) for i in range(self.nst)]
        self.scnt = [0] * self.nst
        self.snext = 0
        self.pending = []
        self.max_pending = 8

    def _semof(self, key):
        if isinstance(key, str):
            return self.sem[key]
        return self.dsem[key[1]] if key[0] == "d" else self.ssem[key[1]]

    def _flush_until(self, key, val):
        while self.pending:
            p = self.pending.pop(0)
            self._emit_store(p)
            if p["key"] == key and p["val"] >= val:
                break

    def flush_stores(self):
        while self.pending:
            self._emit_store(self.pending.pop(0))

    def _emit_store(self, p):
        key, val = p["key"], p["val"]
        if val > 16:
            self._wait("sp", key, val - 16)
        for k, v in p["deps"].items():
            self._wait("sp", k, v)
        self.eng["sp"].dma_start(out=p["out"], in_=p["in_"]).then_inc(self.ssem[key[1]], 16)

    def dma_store(self, out, in_, reads=(), writes=()):
        deps = {}
        for b in reads:
            for k, v in b.w.items():
                if v > deps.get(k, 0):
                    deps[k] = v
        for b in writes:
            assert b.multi
        i = self.snext
        self.snext = (i + 1) % self.nst
        self.scnt[i] += 1
        key, val = ("s", i), 16 * self.scnt[i]
        self._mark(key, val, reads, writes)
        self.pending.append({"key": key, "val": val, "deps": deps, "out": out, "in_": in_})
        if len(self.pending) > self.max_pending:
            self._emit_store(self.pending.pop(0))

    def _wait(self, e, key, val):
        if self.waited[e].get(key, 0) >= val:
            return
        if not isinstance(key, str) and key[0] == "s":
            if any(p["key"] == key and p["val"] <= val for p in self.pending):
                self._flush_until(key, val)
        self.eng[e].wait_ge(self._semof(key), val)
        self.waited[e][key] = val

    def _deps(self, e, reads, writes, same_ok):
        deps = {}
        for b in reads:
            for k, v in b.w.items():
                if v > deps.get(k, 0):
                    deps[k] = v
            if b.excl:
                for k, v in b.r.items():
                    if k != e and v > deps.get(k, 0):
                        deps[k] = v
        for b in writes:
            if b.multi:
                continue
            for k, v in b.w.items():
                if v > deps.get(k, 0):
                    deps[k] = v
            for k, v in b.r.items():
                if v > deps.get(k, 0):
                    deps[k] = v
        for k, v in deps.items():
            if same_ok and k == e:
                continue
            self._wait(e, k, v)

    def _mark(self, key, val, reads, writes):
        for b in writes:
            if b.multi:
                b.w[key] = val
            else:
                b.w = {key: val}
                b.r = {}
        for b in reads:
            if b not in writes:
                b.r[key] = val

    def op(self, e, fn, reads=(), writes=(), same_ok=False):
        self._deps(e, reads, writes, same_ok)
        ins = fn(self.eng[e])
        self.cnt[e] += 1
        ins.then_inc(self.sem[e], 1)
        self._mark(e, self.cnt[e], reads, writes)

    def dma(self, q, out, in_, reads=(), writes=(), **kw):
        i = self.dnext
        self.dnext = (i + 1) % self.ndma
        key = ("d", i)
        if self.dcnt[i]:
            self._wait(q, key, 16 * self.dcnt[i])
        self._deps(q, reads, writes, False)
        self.dcnt[i] += 1
        self.eng[q].dma_start(out=out, in_=in_, **kw).then_inc(self.dsem[i], 16)
        self._mark(key, 16 * self.dcnt[i], reads, writes)

    def idma(self, out, out_offset, in_, in_offset, reads=(), writes=(), **kw):
        q = "pool"
        i = self.dnext
        self.dnext = (i + 1) % self.ndma
        key = ("d", i)
        if self.dcnt[i]:
            self._wait(q, key, 16 * self.dcnt[i])
        self._deps(q, reads, writes, False)
        self.dcnt[i] += 1
        self.eng[q].indirect_dma_start(out=out, out_offset=out_offset, in_=in_,
                                       in_offset=in_offset, **kw).then_inc(self.dsem[i], 16)
        self._mark(key, 16 * self.dcnt[i], reads, writes)

    def barrier(self):
        self.flush_stores()
        for e in self.eng:
            for i in range(self.nst):
                if self.scnt[i]:
                    self._wait(e, ("s", i), 16 * self.scnt[i])
            for k in self.eng:
                if k != e and self.cnt[k]:
                    self._wait(e, k, self.cnt[k])
            for i in range(self.ndma):
                if self.dcnt[i]:
                    self._wait(e, ("d", i), 16 * self.dcnt[i])

    def finish(self, bufs):
        self.flush_stores()
        for b in bufs:
            for k, v in b.w.items():
                self._wait("sp", k, v)


class Pool2:
    def __init__(self, items):
        self.items = items
        self.i = 0

    def next(self):
        it = self.items[self.i]
        self.i = (self.i + 1) % len(self.items)
        return it


class Ctx:
    def __init__(self, nc):
        self.nc = nc
        self.sc = Sched(nc)
        self.es = self.sc.es
        self.banks = []
        for i in range(8):
            t = self.es.enter_context(nc.psum_tensor("bank%d" % i, [128, 512], F32))
            self.banks.append((t, Buf("bank%d" % i, excl=True)))
        self.bank_i = 0

    def bank(self, lo=0, hi=8):
        if not (lo <= self.bank_i < hi):
            self.bank_i = lo
        b = self.banks[self.bank_i]
        self.bank_i += 1
        if self.bank_i >= hi:
            self.bank_i = lo
        return b

    def sb(self, stack, name, shape, dt):
        t = stack.enter_context(self.nc.sbuf_tensor("sb_" + name, shape, dt))
        return t, Buf(name)

    def sbpool(self, stack, name, shape, dt, n):
        return Pool2([self.sb(stack, "%s%d" % (name, i), shape, dt) for i in range(n)])

    def dram(self, name, shape, dt):
        t = self.nc.dram_tensor(name, shape, dt, kind="Internal")
        return t, Buf(name, multi=True)


def mm(cx, bank, out_ap, lhsT, rhs, start, stop, reads):
    cx.sc.op("pe", lambda e: e.matmul(out_ap, lhsT, rhs, start=start, stop=stop),
             reads=reads, writes=[bank], same_ok=True)


def tr(cx, bank, out_ap, in_ap, ident, reads):
    cx.sc.op("pe", lambda e: e.transpose(out_ap, in_ap, ident),
             reads=reads, writes=[bank], same_ok=True)


def stage1(cx, I, SC):
    nc, sc = cx.nc, cx.sc
    with contextlib.ExitStack() as st:
        hT, hT_b = cx.sb(st, "hT", [128, 8, S], BF16)
        gbc, gbc_b = cx.sb(st, "gbc", [128, D], F32)
        ident, ident_b = cx.sb(st, "ident1", [128, 128], BF16)
        xts = cx.sbpool(st, "xt", [128, D], F32, 4)
        junk, junk_b = cx.sb(st, "junk", [128, D], BF16)
        hbs = cx.sbpool(st, "hb", [128, D], BF16, 4)
        stat = cx.sbpool(st, "stat", [128, 4], F32, 4)
        sc.dma("sp", gbc[:], I["g_mix"][:, :], writes=[gbc_b])
        sc.dma("sp", ident[:], I["ident_bf"][:, :], writes=[ident_b])
        x_d = I["x"]
        def a1(t):
            xt, xt_b = xts.next()
            hb, hb_b = hbs.next()
            sm, sm_b = stat.next()
            sc.dma("sp", xt[:], x_d[t * 128:(t + 1) * 128, :], writes=[xt_b])
            sc.op("act", lambda e: e.activation(out=junk[:], in_=xt[:], func=AF.Square,
                                                accum_out=sm[:, 0:1]),
                  reads=[xt_b], writes=[junk_b, sm_b])
            sc.op("act", lambda e: e.activation(out=sm[:, 1:2], in_=sm[:, 0:1], func=AF.Sqrt,
                                                bias=EPS_AP(cx), scale=1.0 / D),
                  reads=[sm_b, cx.eps_b], writes=[sm_b])
            sc.op("dve", lambda e: e.reciprocal(out=sm[:, 2:3], in_=sm[:, 1:2]),
                  reads=[sm_b], writes=[sm_b])
            sc.op("dve", lambda e: e.scalar_tensor_tensor(out=hb[:], in0=xt[:], scalar=sm[:, 2:3],
                                                          in1=gbc[:], op0=ALU.mult, op1=ALU.mult),
                  reads=[xt_b, sm_b, gbc_b], writes=[hb_b])
            return hb, hb_b

        pre1 = {0: a1(0), 1: a1(1)}
        for t in range(NT):
            hb, hb_b = pre1.pop(t)
            bt, bb = cx.bank(0, 4)
            pbf = bt[:, :].bitcast(BF16)
            for k in range(8):
                tr(cx, bb, pbf[:, k * 128:(k + 1) * 128], hb[:, k * 128:(k + 1) * 128], ident[:],
                   reads=[hb_b, ident_b])
            src = pbf.rearrange("p (k n) -> p k n", k=8)
            dst = hT[:, :, t * 128:(t + 1) * 128]
            if t % 2 == 0:
                sc.op("act", lambda e: e.copy(out=dst, in_=src), reads=[bb], writes=[hT_b])
            else:
                sc.op("dve", lambda e: e.tensor_copy(out=dst, in_=src), reads=[bb], writes=[hT_b])
            if t + 2 < NT:
                pre1[t + 2] = a1(t + 2)

        w_d = I["w_in"]
        wv_ = w_d.rearrange("(k p) c -> p k c", p=128)
        wts = cx.sbpool(st, "wblk", [128, 8, 512], BF16, 2)
        tabs = cx.sbpool(st, "ropetab", [128, 2, 512], F32, 3)
        ropeP = cx.sbpool(st, "ropeP", [128, 512], F32, 3)
        ropeQ = cx.sbpool(st, "ropeQ", [128, 512], F32, 3)
        ropeC = cx.sbpool(st, "ropeC", [128, 512], F32, 3)
        outs = cx.sbpool(st, "ev", [128, 512], BF16, 4)
        outf = cx.sbpool(st, "evf", [128, 32], F32, 2)

        def load_w(c0, ncol):
            wt, wt_b = wts.next()
            sc.dma("pool", wt[:, :, 0:ncol], wv_[:, :, c0:c0 + ncol], writes=[wt_b])
            return wt, wt_b

        evi = [0]

        def copy_ev(dst, src, rd, wr, func=None):
            if func is not None:
                sc.op("act", lambda e: e.activation(out=dst, in_=src, func=func), reads=rd, writes=wr)
                return
            evi[0] += 1
            if evi[0] % 2:
                sc.op("act", lambda e: e.copy(out=dst, in_=src), reads=rd, writes=wr)
            else:
                sc.op("dve", lambda e: e.tensor_copy(out=dst, in_=src), reads=rd, writes=wr)

        tm_blocks = [
            (0, 512, "rope_q", SC["q_r"], 0), (512, 512, "rope_k", SC["k_r"], 0),
            (1024, 512, "copy", SC["v_r"], 0), (1536, 512, "copy", SC["v_r"], 512),
            (2048, 512, "silu", SC["g_r"], 0), (2560, 512, "silu", SC["g_r"], 512),
            (3072, 512, "copy", SC["svwv"], 0), (3584, 24, "sig32", SC["gl"], 0),
        ]
        for (c0, ncol, kind, (dst_t, dst_b), dc0) in tm_blocks:
            wt, wt_b = load_w(c0, ncol)
            for t in range(NT):
                bt, bb = cx.bank(0, 4)
                for k in range(8):
                    mm(cx, bb, bt[:, 0:ncol], hT[:, k, t * 128:(t + 1) * 128], wt[:, k, 0:ncol],
                       k == 0, k == 7, reads=[hT_b, wt_b])
                rows = slice(t * 128, (t + 1) * 128)
                if kind in ("rope_q", "rope_k"):
                    tb, tb_b = tabs.next()
                    pp, pp_b = ropeP.next()
                    qq, qq_b = ropeQ.next()
                    pc, pc_b = ropeC.next()
                    ev, ev_b = outs.next()
                    tname = "rope_q_tab" if kind == "rope_q" else "rope_k_tab"
                    sc.dma("sp", tb[:], I[tname][rows, :, :], writes=[tb_b])
                    sc.op("act", lambda e: e.copy(out=pc[:], in_=bt[:, 0:512]), reads=[bb], writes=[pc_b])
                    sc.op("dve", lambda e: e.tensor_tensor(out=pp[:], in0=bt[:, 0:512], in1=tb[:, 0, :], op=ALU.mult),
                          reads=[bb, tb_b], writes=[pp_b])
                    sc.op("pool", lambda e: e.tensor_tensor(out=qq[:], in0=pc[:], in1=tb[:, 1, :], op=ALU.mult),
                          reads=[pc_b, tb_b], writes=[qq_b])
                    pv_ = pp[:, :].rearrange("p (h d) -> p h d", h=4)
                    qv_ = qq[:, :].rearrange("p (h d) -> p h d", h=4)
                    evv = ev[:, :].rearrange("p (h d) -> p h d", h=4)
                    sc.op("dve", lambda e: e.tensor_tensor(out=evv[:, :, 0:64], in0=pv_[:, :, 0:64], in1=qv_[:, :, 64:128],
                                                           op=ALU.subtract),
                          reads=[pp_b, qq_b], writes=[ev_b])
                    sc.op("pool", lambda e: e.tensor_tensor(out=evv[:, :, 64:128], in0=qv_[:, :, 0:64], in1=pv_[:, :, 64:128],
                                                            op=ALU.add),
                          reads=[pp_b, qq_b], writes=[ev_b])
                    sc.dma_store(dst_t.ap()[rows, dc0:dc0 + 512], ev[:, :], reads=[ev_b], writes=[dst_b])
                elif kind == "sig32":
                    ev, ev_b = outf.next()
                    copy_ev(ev[:, 0:ncol], bt[:, 0:ncol], [bb], [ev_b], func=AF.Sigmoid)
                    sc.dma_store(dst_t.ap()[rows, 0:ncol], ev[:, 0:ncol], reads=[ev_b], writes=[dst_b])
                else:
                    ev, ev_b = outs.next()
                    copy_ev(ev[:, 0:ncol], bt[:, 0:ncol], [bb], [ev_b],
                            func=AF.Silu if kind == "silu" else None)
                    sc.dma_store(dst_t.ap()[rows, dc0:dc0 + ncol], ev[:, 0:ncol], reads=[ev_b], writes=[dst_b])

        fm_dst = ([(SC["nqT"], i) for i in range(8)] + [(SC["ckT"], i) for i in range(2)] +
                  [(SC["cvT"], i) for i in range(2)] + [(SC["skT"], i) for i in range(2)] +
                  [(SC["wkT"], i) for i in range(2)] + [(SC["gaT"], i) for i in range(8)] +
                  [(SC["gbT"], i) for i in range(8)])
        for wb in range(8):
            wt, wt_b = load_w(TM_COLS + wb * 512, 512)
            for j in range(4):
                blk = wb * 4 + j
                (dst_t, dst_b), di = fm_dst[blk]
                is_gate = blk >= 16
                for q in range(NQ):
                    bt, bb = cx.bank(0, 4)
                    for k in range(8):
                        mm(cx, bb, bt[:, :], wt[:, k, j * 128:(j + 1) * 128], hT[:, k, q * 512:(q + 1) * 512],
                           k == 0, k == 7, reads=[hT_b, wt_b])
                    ev, ev_b = outs.next()
                    copy_ev(ev[:, :], bt[:, :], [bb], [ev_b], func=AF.Sigmoid if is_gate else None)
                    sc.dma_store(dst_t.ap()[di, :, q * 512:(q + 1) * 512], ev[:, :], reads=[ev_b], writes=[dst_b])


def stage2(cx, I, SC, side=None):
    nc, sc = cx.nc, cx.sc
    with contextlib.ExitStack() as st:
        ident, ident_b = cx.sb(st, "ident2", [128, 128], BF16)
        decT, decT_b = cx.sb(st, "decT", [128, 512], F32)
        xif, xif_b = cx.sb(st, "xif", [128, 512], F32)
        zef, zef_b = cx.sb(st, "zef", [128, 512], F32)
        sc.dma("sp", ident[:], I["ident_bf"][:, :], writes=[ident_b])
        sc.dma("sp", decT[:], I["decayT"][:, :], writes=[decT_b])
        sc.dma("sp", xif[:], I["xi_full"][:, :], writes=[xif_b])
        sc.dma("sp", zef[:], I["zeta_full"][:, :], writes=[zef_b])
        states = [cx.sb(st, "state%d" % i, [128, 4, 256], F32) for i in range(2)]
        sc.op("dve", lambda e: e.memset(states[0][0][:], 0.0), writes=[states[0][1]])
        NSTB = 6
        stbs = [cx.sb(st, "stb%d" % i, [128, 4, 256], BF16) for i in range(NSTB)]
        sc.op("pool", lambda e: e.memset(stbs[0][0][:], 0.0), writes=[stbs[0][1]])
        LD = 6
        qs = cx.sbpool(st, "rq", [128, 512], BF16, LD)
        ks = cx.sbpool(st, "rk", [128, 512], BF16, LD)
        vs = cx.sbpool(st, "rv", [128, 1024], BF16, LD)
        gs = cx.sbpool(st, "rg", [128, 1024], BF16, LD)
        qxs = cx.sbpool(st, "rqx", [128, 512], BF16, 3)
        kzs = cx.sbpool(st, "rkz", [128, 512], BF16, 3)
        tps = cx.sbpool(st, "rtp", [128, 12, 128], BF16, 3)
        sms = cx.sbpool(st, "rsm", [128, 512], BF16, 3)
        ros = cx.sbpool(st, "rro", [128, 1024], BF16, 3)
        rts = cx.sbpool(st, "rrt", [128, 8, 128], BF16, 3)
        nst = cx.sbpool(st, "rns", [128, 12], F32, 4)
        junk, junk_b = cx.sb(st, "rjunk", [128, 256], BF16)
        retT_t, retT_b = SC["retT"]
        tiles = {}

        def load(c):
            rows = slice(c * 128, (c + 1) * 128)
            q, q_b = qs.next(); k, k_b = ks.next(); v, v_b = vs.next(); g, g_b = gs.next()
            sc.dma("sp", k[:], SC["k_r"][0].ap()[rows, :], reads=[SC["k_r"][1]], writes=[k_b])
            sc.dma("sp", v[:], SC["v_r"][0].ap()[rows, :], reads=[SC["v_r"][1]], writes=[v_b])
            sc.dma("sp", q[:], SC["q_r"][0].ap()[rows, :], reads=[SC["q_r"][1]], writes=[q_b])
            sc.dma("sp", g[:], SC["g_r"][0].ap()[rows, :], reads=[SC["g_r"][1]], writes=[g_b])
            tiles[c] = dict(q=q, q_b=q_b, k=k, k_b=k_b, v=v, v_b=v_b, g=g, g_b=g_b)

        def P1(c):
            T = tiles[c]
            k, k_b, v, v_b = T["k"], T["k_b"], T["v"], T["v_b"]
            kz, kz_b = kzs.next()
            sc.op("pool", lambda e: e.tensor_tensor(out=kz[:], in0=k[:], in1=zef[:], op=ALU.mult),
                  reads=[k_b, zef_b], writes=[kz_b])
            kbanks = [cx.bank(), cx.bank()]
            for h in range(4):
                kt, kb = kbanks[h // 2]
                mm(cx, kb, kt[:, (h % 2) * 256:(h % 2 + 1) * 256], kz[:, h * 128:(h + 1) * 128],
                   v[:, h * 256:(h + 1) * 256], True, True, [kz_b, v_b])
            old, old_b = states[c % 2]
            new, new_b = states[(c + 1) % 2]
            for h in range(4):
                kt, kb = kbanks[h // 2]
                sc.op("dve", lambda e: e.scalar_tensor_tensor(out=new[:, h, :], in0=old[:, h, :],
                                                              scalar=float(GAMMAS[h] ** 128),
                                                              in1=kt[:, (h % 2) * 256:(h % 2 + 1) * 256],
                                                              op0=ALU.mult, op1=ALU.add),
                      reads=[kb, old_b], writes=[new_b])
            sb, sb_b = stbs[(c + 1) % NSTB]
            sc.op("act", lambda e: e.copy(out=sb[:], in_=new[:]), reads=[new_b], writes=[sb_b])

        def phaseA(c):
            T = tiles[c]
            q, q_b, k, k_b = T["q"], T["q_b"], T["k"], T["k_b"]
            qx, qx_b = qxs.next()
            sc.op("pool", lambda e: e.tensor_tensor(out=qx[:], in0=q[:], in1=xif[:], op=ALU.mult),
                  reads=[q_b, xif_b], writes=[qx_b])
            tp, tp_b = tps.next()
            b1t, b1b = cx.bank(); b2t, b2b = cx.bank()
            p1 = b1t[:, :].bitcast(BF16); p2 = b2t[:, :].bitcast(BF16)
            for h in range(4):
                tr(cx, b1b, p1[:, h * 128:(h + 1) * 128], q[:, h * 128:(h + 1) * 128], ident[:], [q_b, ident_b])
            for h in range(4):
                tr(cx, b2b, p2[:, h * 128:(h + 1) * 128], k[:, h * 128:(h + 1) * 128], ident[:], [k_b, ident_b])
            for h in range(4):
                tr(cx, b1b, p1[:, (4 + h) * 128:(5 + h) * 128], qx[:, h * 128:(h + 1) * 128], ident[:], [qx_b, ident_b])
            sc.op("act", lambda e: e.copy(out=tp[:, 0:8, :], in_=p1.rearrange("p (a n) -> p a n", a=8)),
                  reads=[b1b], writes=[tp_b])
            sc.op("dve", lambda e: e.tensor_copy(out=tp[:, 8:12, :], in_=p2[:, 0:512].rearrange("p (a n) -> p a n", a=4)),
                  reads=[b2b], writes=[tp_b])
            T.update(tp=tp, tp_b=tp_b)

        def phaseA2(c):
            T = tiles[c]
            tp, tp_b = T["tp"], T["tp_b"]
            bst, bsb = cx.bank()
            for h in range(4):
                mm(cx, bsb, bst[:, h * 128:(h + 1) * 128], tp[:, 8 + h, :], tp[:, h, :], True, True, [tp_b])
            sm, sm_b = sms.next()
            sc.op("dve", lambda e: e.tensor_tensor(out=sm[:], in0=bst[:, :], in1=decT[:], op=ALU.mult),
                  reads=[bsb, decT_b], writes=[sm_b])
            T.update(sm=sm, sm_b=sm_b)

        def phaseB(c):
            T = tiles.pop(c)
            v, v_b, g, g_b, tp, tp_b, sm, sm_b = T["v"], T["v_b"], T["g"], T["g_b"], T["tp"], T["tp_b"], T["sm"], T["sm_b"]
            sb, sb_b = stbs[c % NSTB]
            obanks = [cx.bank(), cx.bank()]
            for h in range(4):
                ot, ob = obanks[h // 2]
                oap = ot[:, (h % 2) * 256:(h % 2 + 1) * 256]
                mm(cx, ob, oap, sm[:, h * 128:(h + 1) * 128], v[:, h * 256:(h + 1) * 256], True, False, [sm_b, v_b])
                mm(cx, ob, oap, tp[:, 4 + h, :], sb[:, h, :], False, True, [tp_b, sb_b])
            ns, ns_b = nst.next()
            for h in range(4):
                ot, ob = obanks[h // 2]
                oap = ot[:, (h % 2) * 256:(h % 2 + 1) * 256]
                sc.op("act", lambda e: e.activation(out=junk[:], in_=oap, func=AF.Square, accum_out=ns[:, h:h + 1]),
                      reads=[ob], writes=[junk_b, ns_b])
            sc.op("act", lambda e: e.activation(out=ns[:, 4:8], in_=ns[:, 0:4], func=AF.Sqrt,
                                                bias=EPS_AP(cx), scale=1.0 / 256),
                  reads=[ns_b, cx.eps_b], writes=[ns_b])
            sc.op("dve", lambda e: e.reciprocal(out=ns[:, 8:12], in_=ns[:, 4:8]), reads=[ns_b], writes=[ns_b])
            ro, ro_b = ros.next()
            for h in range(4):
                ot, ob = obanks[h // 2]
                oap = ot[:, (h % 2) * 256:(h % 2 + 1) * 256]
                sc.op("dve", lambda e: e.scalar_tensor_tensor(out=ro[:, h * 256:(h + 1) * 256], in0=oap,
                                                              scalar=ns[:, 8 + h:9 + h], in1=g[:, h * 256:(h + 1) * 256],
                                                              op0=ALU.mult, op1=ALU.mult),
                      reads=[ob, ns_b, g_b], writes=[ro_b])

            def fin():
                btt, btb = cx.bank()
                pt = btt[:, :].bitcast(BF16)
                for j in range(8):
                    tr(cx, btb, pt[:, j * 128:(j + 1) * 128], ro[:, j * 128:(j + 1) * 128], ident[:], [ro_b, ident_b])
                rt, rt_b = rts.next()
                if c % 2:
                    sc.op("act", lambda e: e.copy(out=rt[:], in_=pt.rearrange("p (a n) -> p a n", a=8)),
                          reads=[btb], writes=[rt_b])
                else:
                    sc.op("dve", lambda e: e.tensor_copy(out=rt[:], in_=pt.rearrange("p (a n) -> p a n", a=8)),
                          reads=[btb], writes=[rt_b])
                sc.dma_store(retT_t.ap()[c, :, :], rt[:, :, :].rearrange("p a n -> p (a n)"), reads=[rt_b], writes=[retT_b])
            return fin

        AHEAD = 3
        for c in range(min(AHEAD + 1, NT)):
            load(c)
        for c in range(min(AHEAD, NT - 1)):
            P1(c)
        phaseA(0)
        phaseA2(0)
        prev = None
        for c in range(NT):
            if c + AHEAD + 1 < NT:
                load(c + AHEAD + 1)
            if c + 1 < NT:
                phaseA(c + 1)
            f = phaseB(c)
            if prev is not None:
                prev()
            prev = f
            if c + AHEAD < NT - 1:
                P1(c + AHEAD)
            if c + 1 < NT:
                phaseA2(c + 1)
            if side is not None and c >= 2:
                next(side, None)
        prev()
        if side is not None:
            for _ in side:
                pass


GELU_C = 0.7978845608028654


def stage3_gen(cx, I, SC, kcmpT, kcmpT_b, vext, vext_b):
    nc, sc = cx.nc, cx.sc
    with contextlib.ExitStack() as st:
        cT = {}
        for nm in ("ckT", "cvT"):
            for g in range(2):
                t, b = cx.sb(st, "c3_%s%d" % (nm, g), [128, S], BF16)
                sc.dma("sp", t[:], SC[nm][0].ap()[g, :, :], reads=[SC[nm][1]], writes=[b])
                cT[(nm, g)] = (t, b)
        sc.op("pool", lambda e: e.memset(kcmpT[:], 0.0), writes=[kcmpT_b])
        for g in range(2):
            for nt_ in range(2):
                sc.dma("sp", vext[:, g, nt_, 128:193], I["vext_const"][nt_, :, :], writes=[vext_b])
        yield
        for kv, nm in ((0, "ckT"), (1, "cvT")):
            sfx = "k" if kv == 0 else "v"
            w1, w1_b = cx.sb(st, "c3w1" + sfx, [128, 32, 128], BF16)
            w2, w2_b = cx.sb(st, "c3w2" + sfx, [128, 128], BF16)
            peT, peT_b = cx.sb(st, "c3pe" + sfx, [128, 32], BF16)
            bias, bias_b = cx.sb(st, "c3b" + sfx, [128, 1], F32)
            sc.dma("pool", w1[:], I["cmp_w1_" + sfx].rearrange("l d e -> d l e"), writes=[w1_b])
            sc.dma("pool", w2[:], I["cmp_w2_" + sfx][:, :], writes=[w2_b])
            sc.dma("pool", peT[:], I["cmp_peT_" + sfx][:, :], writes=[peT_b])
            bt, bb = cx.bank()
            for l in range(32):
                mm(cx, bb, bt[:, 0:1], w1[:, l, :], peT[:, l:l + 1], l == 0, l == 31, [w1_b, peT_b])
            sc.op("act", lambda e: e.copy(out=bias[:], in_=bt[:, 0:1]), reads=[bb], writes=[bias_b])
            yield
            for g in range(2):
                ct, ct_b = cT[(nm, g)]
                ht, hb = cx.bank()
                for l in range(32):
                    mm(cx, hb, ht[:, 0:NCMP], w1[:, l, :], ct[:, l:l + 16 * (NCMP - 1) + 1:16],
                       l == 0, l == 31, [w1_b, ct_b])
                xh, xh_b = cx.sb(st, "c3xh%s%d" % (sfx, g), [128, 256], F32)
                u, u_b = cx.sb(st, "c3u%s%d" % (sfx, g), [128, 256], F32)
                hid, hid_b = cx.sb(st, "c3hid%s%d" % (sfx, g), [128, 256], BF16)
                sc.op("pool", lambda e: e.memset(hid[:], 0.0), writes=[hid_b])
                n = NCMP
                sc.op("act", lambda e: e.activation(out=xh[:, 0:n], in_=ht[:, 0:n], func=AF.Identity,
                                                    bias=bias[:, 0:1], scale=1.0),
                      reads=[hb, bias_b], writes=[xh_b])
                yield
                sc.op("dve", lambda e: e.tensor_tensor(out=u[:, 0:n], in0=xh[:, 0:n], in1=xh[:, 0:n], op=ALU.mult),
                      reads=[xh_b], writes=[u_b])
                sc.op("dve", lambda e: e.tensor_scalar(out=u[:, 0:n], in0=u[:, 0:n], scalar1=0.044715, scalar2=1.0,
                                                       op0=ALU.mult, op1=ALU.add),
                      reads=[u_b], writes=[u_b])
                sc.op("dve", lambda e: e.tensor_tensor(out=u[:, 0:n], in0=u[:, 0:n], in1=xh[:, 0:n], op=ALU.mult),
                      reads=[u_b, xh_b], writes=[u_b])
                sc.op("act", lambda e: e.activation(out=u[:, 0:n], in_=u[:, 0:n], func=AF.Sigmoid,
                                                    scale=2.0 * GELU_C),
                      reads=[u_b], writes=[u_b])
                sc.op("dve", lambda e: e.tensor_tensor(out=hid[:, 0:n], in0=u[:, 0:n], in1=xh[:, 0:n], op=ALU.mult),
                      reads=[u_b, xh_b], writes=[hid_b])
                yield
                if kv == 0:
                    ot, ob = cx.bank()
                    mm(cx, ob, ot[:, 0:256], w2[:, :], hid[:, :], True, True, [w2_b, hid_b])
                    sc.op("act", lambda e: e.copy(out=kcmpT[:, g, 0:n], in_=ot[:, 0:n]), reads=[ob], writes=[kcmpT_b])
                else:
                    for nt_ in range(2):
                        ot, ob = cx.bank()
                        mm(cx, ob, ot[:, 0:128], hid[:, nt_ * 128:(nt_ + 1) * 128], w2[:, :], True, True, [w2_b, hid_b])
                        sc.op("act", lambda e: e.copy(out=vext[:, g, nt_, 0:128], in_=ot[:, 0:128]),
                              reads=[ob], writes=[vext_b])


def stage3(cx, I, SC, kcmpT, kcmpT_b, vext, vext_b):
    for _ in stage3_gen(cx, I, SC, kcmpT, kcmpT_b, vext, vext_b):
        pass


def stage4(cx, I, SC, kcmpT, kcmpT_b, vext, vext_b, HM):
    nc, sc = cx.nc, cx.sc
    scale = 128 ** -0.5
    with contextlib.ExitStack() as st:
        ident, ident_b = cx.sb(st, "ident4", [128, 128], BF16)
        sc.dma("sp", ident[:], I["ident_bf"][:, :], writes=[ident_b])
        skT, skT_b = cx.sb(st, "skTs", [128, 2, S], BF16)
        wkT, wkT_b = cx.sb(st, "wkTs", [128, 2, S], BF16)
        for g in range(2):
            sc.dma("sp", skT[:, g, :], SC["skT"][0].ap()[g, :, :], reads=[SC["skT"][1]], writes=[skT_b])
            sc.dma("sp", wkT[:, g, :], SC["wkT"][0].ap()[g, :, :], reads=[SC["wkT"][1]], writes=[wkT_b])
        svx, svx_b = cx.sb(st, "svx", [128, NT, 2, 129], BF16)
        wvx, wvx_b = cx.sb(st, "wvx", [128, NT, 2, 129], BF16)
        sc.op("pool", lambda e: e.memset(svx[:, :, :, 128:129], 1.0), writes=[svx_b])
        sc.op("pool", lambda e: e.memset(wvx[:, :, :, 128:129], 1.0), writes=[wvx_b])
        svwv = SC["svwv"][0].ap()
        for t in range(NT):
            rows = slice(t * 128, (t + 1) * 128)
            sc.dma("sp", svx[:, t, :, 0:128], svwv[rows, 0:256].rearrange("p (g d) -> p g d", g=2),
                   reads=[SC["svwv"][1]], writes=[svx_b])
            sc.dma("sp", wvx[:, t, :, 0:128], svwv[rows, 256:512].rearrange("p (g d) -> p g d", g=2),
                   reads=[SC["svwv"][1]], writes=[wvx_b])
        mb, mb_b = cx.sb(st, "maskb", [128, 8, 512], BF16)
        sc.dma("sp", mb[:], I["mask_bias"][:, :, :], writes=[mb_b])
        eexp, eexp_b = cx.sb(st, "eexp", [128, S], BF16)
        sc.dma("sp", eexp[:], I["eexp"][:, :], writes=[eexp_b])
        cms = cx.sbpool(st, "cmT", [128, 2, 512], BF16, 2)
        nqs = cx.sbpool(st, "nq", [128, 8, 512], BF16, 2)
        gls = cx.sbpool(st, "gls", [128, 4, 24], F32, 2)
        ims = cx.sbpool(st, "impm", [128, 4, 2, 2, 64], F32, 2)
        obr_sets = [[cx.sb(st, "obr%d_%d" % (p_, b), [128, 4, 1024], BF16) for b in range(3)] for p_ in range(2)]
        obr_cur = [obr_sets[0]]
        imp, imp_b = cx.sb(st, "imp", [128, 4, 2, 64], F32)
        pts = cx.sbpool(st, "pt", [128, 512], BF16, 6)
        smalls = cx.sbpool(st, "sm4", [128, 8], F32, 8)
        sw1, sw1_b = cx.sb(st, "sw1", [128, 4, 2, 64], F32)
        sw2, sw2_b = cx.sb(st, "sw2", [128, 4, 2, 64], F32)
        kn, kn_b = cx.sb(st, "kn", [128, 4, 2, 64], F32)
        knb = [Buf("kn%d" % i) for i in range(8)]
        m8a = [cx.sb(st, "m8a%d" % i, [128, 8], F32) for i in range(8)]
        m8b = [cx.sb(st, "m8b%d" % i, [128, 8], F32) for i in range(8)]
        selb = [cx.sb(st, "selb%d" % i, [128, 64], BF16) for i in range(8)]
        selT, selT_b = cx.sb(st, "selT", [128, 2, 512], BF16)
        sc.op("pool", lambda e: e.memset(selT[:], 0.0), writes=[selT_b])
        ots = cx.sbpool(st, "ot4", [128, 8, 128], BF16, 4)
        nsaT_t, nsaT_b = SC["nsaT"]
        SB0, SB1 = 0, 3
        ACC = [[cx.banks[3], cx.banks[4]], [cx.banks[5], cx.banks[6]]]
        MISC = cx.banks[7]
        acc_set = [0]

        def evac(aset, h, br, gl, gl_b, oset, cmp_imp=None):
            ob_t, ob_b = oset[br]
            gcol = 3 * h + br
            for bi in range(2):
                at, ab = aset[bi]
                sm, sm_b = smalls.next()
                zc = at[:, 128:512:256]
                if br == 0:
                    sc.op("dve", lambda e: e.tensor_scalar(out=sm[:, 0:2], in0=zc, scalar1=1e-30, scalar2=None, op0=ALU.max),
                          reads=[ab], writes=[sm_b])
                    sc.op("dve", lambda e: e.reciprocal(out=sm[:, 2:4], in_=sm[:, 0:2]), reads=[sm_b], writes=[sm_b])
                else:
                    sc.op("dve", lambda e: e.reciprocal(out=sm[:, 2:4], in_=zc), reads=[ab], writes=[sm_b])
                sc.op("dve", lambda e: e.tensor_tensor(out=sm[:, 4:6], in0=sm[:, 2:4], in1=gl[:, 2 * bi:2 * bi + 2, gcol],
                                                       op=ALU.mult),
                      reads=[sm_b, gl_b], writes=[sm_b])
                for jj in range(2):
                    j = 2 * bi + jj
                    src = at[:, jj * 256:jj * 256 + 128]
                    dst = ob_t[:, j, h * 128:(h + 1) * 128]
                    sc.op("dve", lambda e: e.tensor_scalar(out=dst, in0=src, scalar1=sm[:, 4 + jj:5 + jj], scalar2=None,
                                                           op0=ALU.mult),
                          reads=[ab, sm_b], writes=[ob_b])
                    if cmp_imp is not None:
                        g, firsth = cmp_imp
                        isrc = at[:, jj * 256 + 129:jj * 256 + 193]
                        idst = imp[:, j, g, :]
                        if firsth:
                            sc.op("dve", lambda e: e.tensor_scalar(out=idst, in0=isrc, scalar1=sm[:, 2 + jj:3 + jj],
                                                                   scalar2=None, op0=ALU.mult),
                                  reads=[ab, sm_b], writes=[imp_b])
                        else:
                            sc.op("dve", lambda e: e.scalar_tensor_tensor(out=idst, in0=isrc, scalar=sm[:, 2 + jj:3 + jj],
                                                                          in1=idst, op0=ALU.mult, op1=ALU.add),
                                  reads=[ab, sm_b, imp_b], writes=[imp_b])

        def pv(aset, started, j, width, lhsT, rhs, rd):
            bi, jj = j // 2, j % 2
            at, ab = aset[bi]
            first = not started[bi]
            started[bi] = True
            mm(cx, ab, at[:, jj * 256:jj * 256 + width], lhsT, rhs, first, False, rd)

        def run_stream(items, L=2):
            pend = []
            for it in items:
                if "plain" in it:
                    it["plain"]()
                    continue
                pend.append((it, it["score"]()))
                if len(pend) > L:
                    it0, c0 = pend.pop(0)
                    it0["pv"](c0)
            for it0, c0 in pend:
                it0["pv"](c0)

        def make_item(h, br, lhsT, lhs_rd, nq, nq_b, extras, vop, v_rd, width, slices, state, last, gl, gl_b, cmp_imp, oset):
            cs = slice(min(slices) * 128, (max(slices) + 1) * 128)

            def score():
                bt, bb = cx.bank(SB0, SB1)
                mm(cx, bb, bt[:, cs], lhsT, nq[:, h, cs], True, len(extras) == 0, lhs_rd + [nq_b])
                for ei, (l_, r_, rd) in enumerate(extras):
                    mm(cx, bb, bt[:, cs], l_, r_[:, cs], False, ei == len(extras) - 1, rd)
                pt, pt_b = pts.next()
                sc.op("act", lambda e: e.activation(out=pt[:, cs], in_=bt[:, cs], func=AF.Exp, scale=scale),
                      reads=[bb], writes=[pt_b])
                return pt, pt_b

            def pvf(c):
                pt, pt_b = c
                if state["aset"] is None:
                    state["aset"] = ACC[acc_set[0]]
                    acc_set[0] ^= 1
                    state["started"] = [False, False]
                for j in slices:
                    pv(state["aset"], state["started"], j, width, pt[:, j * 128:(j + 1) * 128], vop, [pt_b] + v_rd)
                if last:
                    evac(state["aset"], h, br, gl, gl_b, oset, cmp_imp=cmp_imp)
            return {"score": score, "pv": pvf}

        NQL = int(_os.environ.get("STG4Q", NQ))
        cm_valid = HM["cm_valid"]

        def do_loads(q):
            qc = slice(q * 512, (q + 1) * 512)
            nq, nq_b = nqs.next()
            gl, gl_b = gls.next()
            im, im_b = ims.next()
            cmT, cmT_b = cms.next()
            sc.dma("sp", nq[:], SC["nqT"][0].ap()[:, :, qc].rearrange("h p n -> p h n"),
                   reads=[SC["nqT"][1]], writes=[nq_b])
            sc.dma("sp", gl[:], SC["gl"][0].ap()[qc, :].rearrange("(j p) c -> p j c", p=128),
                   reads=[SC["gl"][1]], writes=[gl_b])
            sc.dma("sp", im[:], I["imp_masks"][qc, :, :, :].rearrange("(j p) a g c -> p j a g c", p=128), writes=[im_b])
            sc.dma("sp", cmT[:], I["cmp_bias"][:, :, qc], writes=[cmT_b])
            return dict(nq=nq, nq_b=nq_b, gl=gl, gl_b=gl_b, im=im, im_b=im_b, cmT=cmT, cmT_b=cmT_b)

        def cmp_items(q, L, oset):
            nq, nq_b, gl, gl_b, cmT, cmT_b = L["nq"], L["nq_b"], L["gl"], L["gl_b"], L["cmT"], L["cmT_b"]
            nts = [n_ for n_ in range(2) if cm_valid[n_][:, q * 512:(q + 1) * 512].any()]
            items = []
            for h in range(8):
                g = h // 4
                state = {"aset": None}
                for n_ in nts:
                    allv = bool(cm_valid[n_][:, q * 512:(q + 1) * 512].all())
                    extras = [] if allv else [(ident[:], cmT[:, n_, :], [ident_b, cmT_b])]
                    items.append(make_item(h, 0, kcmpT[:, g, n_ * 128:(n_ + 1) * 128], [kcmpT_b], nq, nq_b, extras,
                                           vext[:, g, n_, :], [vext_b], 193, [0, 1, 2, 3], state, n_ == nts[-1],
                                           gl, gl_b, (g, h % 4 == 0), oset))
            return items

        def make_fin(q, oset):
            def fin():
                for j in range(4):
                    mt, mbk = MISC
                    ot, ot_b = ots.next()
                    for half in range(2):
                        for c4 in range(4):
                            c = half * 4 + c4
                            for b_ in range(3):
                                mm(cx, mbk, mt[:, c4 * 128:(c4 + 1) * 128], oset[b_][0][:, j, c * 128:(c + 1) * 128], ident[:],
                                   c4 == 0 and b_ == 0, False, [oset[b_][1], ident_b])
                        src = mt[:, :].rearrange("p (a n) -> p a n", a=4)
                        if half:
                            sc.op("act", lambda e: e.copy(out=ot[:, 4:8, :], in_=src), reads=[mbk], writes=[ot_b])
                        else:
                            sc.op("dve", lambda e: e.tensor_copy(out=ot[:, 0:4, :], in_=src), reads=[mbk], writes=[ot_b])
                    rows = slice(q * 512 + j * 128, q * 512 + (j + 1) * 128)
                    sc.dma_store(nsaT_t.ap()[q * 4 + j, :, :], ot[:, :, :].rearrange("p a n -> p (a n)"), reads=[ot_b], writes=[nsaT_b])
            return fin

        LQ = {0: do_loads(0)}
        run_stream(cmp_items(0, LQ[0], obr_sets[0]))
        prev_fin = None
        for q in range(NQL):
            L = LQ.pop(q)
            nq, nq_b, gl, gl_b, im, im_b = L["nq"], L["nq_b"], L["gl"], L["gl_b"], L["im"], L["im_b"]
            oset = obr_sets[q % 2]
            sc.op("dve", lambda e: e.tensor_tensor(out=sw1[:], in0=imp[:], in1=im[:, :, 0, :, :], op=ALU.mult),
                  reads=[imp_b, im_b], writes=[sw1_b])
            sc.op("dve", lambda e: e.tensor_tensor(out=sw1[:], in0=sw1[:], in1=im[:, :, 1, :, :], op=ALU.add),
                  reads=[sw1_b, im_b], writes=[sw1_b])
            for g in range(2):
                for j in range(4):
                    i8 = g * 4 + j
                    sc.op("dve", lambda e: e.max(out=m8a[i8][0][:, :], in_=sw1[:, j, g, :]), reads=[sw1_b], writes=[m8a[i8][1]])
            for g in range(2):
                for j in range(4):
                    i8 = g * 4 + j
                    sc.op("dve", lambda e: e.tensor_scalar(out=kn[:, j, g, :], in0=sw1[:, j, g, :], scalar1=m8a[i8][0][:, 7:8],
                                                           scalar2=-1e9, op0=ALU.is_ge, op1=ALU.mult),
                          reads=[sw1_b, m8a[i8][1]], writes=[knb[i8]])
            sc.op("dve", lambda e: e.tensor_tensor(out=sw2[:], in0=kn[:], in1=sw1[:], op=ALU.add),
                  reads=knb + [sw1_b], writes=[sw2_b])
            for g in range(2):
                for j in range(4):
                    i8 = g * 4 + j
                    sc.op("dve", lambda e: e.max(out=m8b[i8][0][:, :], in_=sw2[:, j, g, :]), reads=[sw2_b], writes=[m8b[i8][1]])
            for g in range(2):
                for j in range(4):
                    i8 = g * 4 + j
                    sc.op("dve", lambda e: e.tensor_scalar(out=selb[i8][0][:, :], in0=sw1[:, j, g, :], scalar1=m8b[i8][0][:, 7:8],
                                                           scalar2=-1.0, op0=ALU.is_ge, op1=ALU.add),
                          reads=[sw1_b, m8b[i8][1]], writes=[selb[i8][1]])

            def sel_transposes():
                mt, mbk = MISC
                pm = mt[:, :].bitcast(BF16)
                for i8 in range(8):
                    tr(cx, mbk, pm[0:64, i8 * 128:(i8 + 1) * 128], selb[i8][0][:, :], ident[:], [selb[i8][1], ident_b])
                sc.op("act", lambda e: e.copy(out=selT[0:64, :, :], in_=pm[0:64, :].rearrange("p (g n) -> p g n", g=2)),
                      reads=[mbk], writes=[selT_b])
            if q + 1 < NQL:
                LQ[q + 1] = do_loads(q + 1)
            items = []
            for br in (2, 1):
                if br == 1:
                    if prev_fin is not None:
                        items.append({"plain": prev_fin})
                    if q + 1 < NQL:
                        items.extend(cmp_items(q + 1, LQ[q + 1], obr_sets[(q + 1) % 2]))
                    items.append({"plain": sel_transposes})
                kT, kT_b = (skT, skT_b) if br == 1 else (wkT, wkT_b)
                vx, vx_b = (svx, svx_b) if br == 1 else (wvx, wvx_b)
                kt_lo = 0 if br == 1 else max(0, 4 * q - 4)
                kts = list(range(kt_lo, 4 * q + 4))
                for h in range(8):
                    g = h // 4
                    state = {"aset": None}
                    for kt in kts:
                        r = kt - 4 * q
                        ks = slice(kt * 128, (kt + 1) * 128)
                        extras = []
                        if br == 1:
                            extras.append((eexp[:, ks], selT[:, g, :], [eexp_b, selT_b]))
                            if r >= 0:
                                extras.append((ident[:], mb[:, r, :], [ident_b, mb_b]))
                        else:
                            mi = r if r >= 0 else 8 + r
                            extras.append((ident[:], mb[:, mi, :], [ident_b, mb_b]))
                        slices = [j for j in range(4)
                                  if (0 if br == 1 else max(0, 4 * q + j - 4)) <= kt <= 4 * q + j]
                        items.append(make_item(h, br, kT[:, g, ks], [kT_b], nq, nq_b, extras, vx[:, kt, g, :], [vx_b],
                                               129, slices, state, kt == kts[-1], gl, gl_b, None, oset))
            run_stream(items)
            prev_fin = make_fin(q, oset)
        if prev_fin is not None:
            prev_fin()


def load_w_bf(cx, st, name, src_ap, kchunks, ncols):
    t, b = cx.sb(st, name, [128, kchunks, ncols], BF16)
    v = src_ap.rearrange("(k p) c -> p k c", p=128)
    step = max(1, 4096 // ncols)
    for k0 in range(0, kchunks, step):
        cx.sc.dma("pool", t[:, k0:k0 + step, :], v[:, k0:k0 + step, :], writes=[b])
    return t, b


def rms_rows(cx, xt, xt_b, gbc, gbc_b, out_t, out_b, junk, junk_b, sm, sm_b):
    sc = cx.sc
    sc.op("act", lambda e: e.activation(out=junk[:], in_=xt[:], func=AF.Square, accum_out=sm[:, 0:1]),
          reads=[xt_b], writes=[junk_b, sm_b])
    sc.op("act", lambda e: e.activation(out=sm[:, 1:2], in_=sm[:, 0:1], func=AF.Sqrt,
                                        bias=EPS_AP(cx), scale=1.0 / D),
          reads=[sm_b, cx.eps_b], writes=[sm_b])
    sc.op("dve", lambda e: e.reciprocal(out=sm[:, 2:3], in_=sm[:, 1:2]), reads=[sm_b], writes=[sm_b])
    sc.op("dve", lambda e: e.scalar_tensor_tensor(out=out_t[:], in0=xt[:], scalar=sm[:, 2:3], in1=gbc[:],
                                                  op0=ALU.mult, op1=ALU.mult),
          reads=[xt_b, sm_b, gbc_b], writes=[out_b])


def stage5(cx, I, SC):
    nc, sc = cx.nc, cx.sc
    with contextlib.ExitStack() as st:
        wro, wro_b = load_w_bf(cx, st, "w_ret_o", I["w_ret_o"], 8, 1024)
        wno, wno_b = load_w_bf(cx, st, "w_nsa_o", I["w_nsa_o"], 8, 1024)
        wou, wou_b = load_w_bf(cx, st, "w_out", I["w_out"], 8, 1024)
        ins = {nm: cx.sbpool(st, "s5" + nm, [128, 8, 512], BF16, 2) for nm in ("gaT", "gbT")}
        ins.update({nm: cx.sbpool(st, "s5" + nm, [128, 4, 8, 128], BF16, 2) for nm in ("retT", "nsaT")})
        yTs = cx.sbpool(st, "s5y", [128, 8, 512], BF16, 2)
        t1s = cx.sbpool(st, "s5t1", [128, 512], F32, 2)
        t2s = cx.sbpool(st, "s5t2", [128, 512], F32, 2)
        xts = cx.sbpool(st, "s5x", [128, D], F32, 2)
        xos = cx.sbpool(st, "s5xo", [128, D], F32, 2)
        for q in range(NQ):
            qc = slice(q * 512, (q + 1) * 512)
            cur = {}
            for nm in ("retT", "nsaT"):
                t, b = ins[nm].next()
                sc.dma("sp", t[:, :, :, :], SC[nm][0].ap()[q * 4:q * 4 + 4, :, :].rearrange("j p (a n) -> p j a n", a=8),
                       reads=[SC[nm][1]], writes=[b])
                cur[nm] = (t, b)
            for nm in ("gaT", "gbT"):
                t, b = ins[nm].next()
                sc.dma("sp", t[:], SC[nm][0].ap()[:, :, qc].rearrange("a p n -> p a n"),
                       reads=[SC[nm][1]], writes=[b])
                cur[nm] = (t, b)
            yT, yT_b = yTs.next()
            for c in range(8):
                cs = slice(c * 128, (c + 1) * 128)
                at, ab = cx.bank()
                bt, bb = cx.bank()
                for k in range(8):
                    mm(cx, ab, at[:, :].rearrange("p (j n) -> p j n", j=4), wro[:, k, cs], cur["retT"][0][:, :, k, :], k == 0, k == 7, [wro_b, cur["retT"][1]])
                for k in range(8):
                    mm(cx, bb, bt[:, :].rearrange("p (j n) -> p j n", j=4), wno[:, k, cs], cur["nsaT"][0][:, :, k, :], k == 0, k == 7, [wno_b, cur["nsaT"][1]])
                t1, t1_b = t1s.next()
                t2, t2_b = t2s.next()
                sc.op("dve", lambda e: e.tensor_tensor(out=t1[:], in0=at[:, :], in1=cur["gaT"][0][:, c, :], op=ALU.mult),
                      reads=[ab, cur["gaT"][1]], writes=[t1_b])
                sc.op("dve", lambda e: e.tensor_tensor(out=t2[:], in0=bt[:, :], in1=cur["gbT"][0][:, c, :], op=ALU.mult),
                      reads=[bb, cur["gbT"][1]], writes=[t2_b])
                sc.op("pool", lambda e: e.tensor_tensor(out=yT[:, c, :], in0=t1[:], in1=t2[:], op=ALU.add),
                      reads=[t1_b, t2_b], writes=[yT_b])
            for j in range(4):
                rows = slice(q * 512 + j * 128, q * 512 + (j + 1) * 128)
                xt, xt_b = xts.next()
                xo, xo_b = xos.next()
                sc.dma("sp", xt[:], I["x"][rows, :], writes=[xt_b])
                for half in range(2):
                    ot, ob = cx.bank()
                    for c in range(8):
                        mm(cx, ob, ot[:, :], yT[:, c, j * 128:(j + 1) * 128], wou[:, c, half * 512:(half + 1) * 512],
                           c == 0, c == 7, [yT_b, wou_b])
                    sc.op("dve", lambda e: e.tensor_tensor(out=xo[:, half * 512:(half + 1) * 512], in0=ot[:, :],
                                                           in1=xt[:, half * 512:(half + 1) * 512], op=ALU.add),
                          reads=[ob, xt_b], writes=[xo_b])
                sc.dma_store(SC["x1"][0].ap()[rows, :], xo[:], reads=[xo_b], writes=[SC["x1"][1]])


def stage6(cx, I, SC):
    nc, sc = cx.nc, cx.sc
    with contextlib.ExitStack() as st:
        ident, ident_b = cx.sb(st, "ident6", [128, 128], BF16)
        sc.dma("sp", ident[:], I["ident_bf"][:, :], writes=[ident_b])
        gx, gx_b = cx.sb(st, "g_x", [128, D], F32)
        gm, gm_b = cx.sb(st, "g_mem", [128, D], F32)
        sc.dma("sp", gx[:], I["g_x"][:, :], writes=[gx_b])
        sc.dma("sp", gm[:], I["g_mem"][:, :], writes=[gm_b])
        wq, wq_b = load_w_bf(cx, st, "w_xq", I["w_xq"], 8, 1024)
        wkv, wkv_b = load_w_bf(cx, st, "w_xkv", I["w_xkv"], 8, 2048)
        wo, wo_b = load_w_bf(cx, st, "w_xo", I["w_xo"], 8, 1024)
        junk, junk_b = cx.sb(st, "s6junk", [128, D], BF16)
        sms = cx.sbpool(st, "s6sm", [128, 4], F32, 8)
        xas = cx.sbpool(st, "s6xa", [128, D], F32, 4)
        hbs = cx.sbpool(st, "s6hb", [128, D], BF16, 6)
        memnT, memnT_b = cx.sb(st, "memnT", [128, 8, 256], BF16)
        kT, kT_b = cx.sb(st, "xkT", [128, 8, 256], BF16)
        vx, vx_b = cx.sb(st, "xvx", [128, 2, 4, 257], BF16)
        sc.op("pool", lambda e: e.memset(vx[:, :, :, 256:257], 1.0), writes=[vx_b])
        for mt in range(2):
            xt, xt_b = xas.next()
            hb, hb_b = hbs.next()
            sm, sm_b = sms.next()
            sc.dma("sp", xt[:, :], I["mem"][mt * 128:(mt + 1) * 128, :], writes=[xt_b])

            class _V:
                pass
            xv = xt[:, :]
            sc.op("act", lambda e: e.activation(out=junk[:], in_=xv, func=AF.Square, accum_out=sm[:, 0:1]),
                  reads=[xt_b], writes=[junk_b, sm_b])
            sc.op("act", lambda e: e.activation(out=sm[:, 1:2], in_=sm[:, 0:1], func=AF.Sqrt, bias=EPS_AP(cx), scale=1.0 / D),
                  reads=[sm_b, cx.eps_b], writes=[sm_b])
            sc.op("dve", lambda e: e.reciprocal(out=sm[:, 2:3], in_=sm[:, 1:2]), reads=[sm_b], writes=[sm_b])
            sc.op("dve", lambda e: e.scalar_tensor_tensor(out=hb[:], in0=xv, scalar=sm[:, 2:3], in1=gm[:],
                                                          op0=ALU.mult, op1=ALU.mult),
                  reads=[xt_b, sm_b, gm_b], writes=[hb_b])
            bt, bb = cx.bank()
            pb = bt[:, :].bitcast(BF16)
            for k in range(8):
                tr(cx, bb, pb[:, k * 128:(k + 1) * 128], hb[:, k * 128:(k + 1) * 128], ident[:], [hb_b, ident_b])
            sc.op("act", lambda e: e.copy(out=memnT[:, :, mt * 128:(mt + 1) * 128], in_=pb.rearrange("p (k n) -> p k n", k=8)),
                  reads=[bb], writes=[memnT_b])
        for c in range(8):
            bt, bb = cx.bank()
            for k in range(8):
                mm(cx, bb, bt[:, 0:256], wkv[:, k, c * 128:(c + 1) * 128], memnT[:, k, :], k == 0, k == 7, [wkv_b, memnT_b])
            sc.op("act", lambda e: e.copy(out=kT[:, c, :], in_=bt[:, 0:256]), reads=[bb], writes=[kT_b])
        for mt in range(2):
            for half in range(2):
                bt, bb = cx.bank()
                for k in range(8):
                    mm(cx, bb, bt[:, :], memnT[:, k, mt * 128:(mt + 1) * 128],
                       wkv[:, k, 1024 + half * 512:1024 + (half + 1) * 512], k == 0, k == 7, [wkv_b, memnT_b])
                sc.op("act", lambda e: e.copy(out=vx[:, mt, 2 * half:2 * half + 2, 0:256],
                                              in_=bt[:, :].rearrange("p (h d) -> p h d", h=2)),
                      reads=[bb], writes=[vx_b])
        xrs = cx.sbpool(st, "s6xr", [128, D], F32, 3)
        hxTs = cx.sbpool(st, "s6hxT", [128, 8, 512], BF16, 2)
        qTs = cx.sbpool(st, "s6qT", [128, 8, 512], BF16, 2)
        pts = cx.sbpool(st, "s6pt", [128, 512], BF16, 6)
        otm = cx.sbpool(st, "s6o", [128, 4, D], BF16, 2)
        oTs = cx.sbpool(st, "s6oT", [128, 8, 128], BF16, 3)
        xos = cx.sbpool(st, "s6xo", [128, D], F32, 2)
        zs = cx.sbpool(st, "s6z", [128, 2], F32, 8)
        scale = 256 ** -0.5
        def phaseA(q):
            hxT, hxT_b = hxTs.next()
            hbl = []
            for j in range(4):
                rows = slice(q * 512 + j * 128, q * 512 + (j + 1) * 128)
                x1t, x1t_b = xas.next()
                sc.dma("sp", x1t[:], SC["x1"][0].ap()[rows, :], reads=[SC["x1"][1]], writes=[x1t_b])
                hb, hb_b = hbs.next()
                sm, sm_b = sms.next()
                sc.op("act", lambda e: e.activation(out=junk[:], in_=x1t[:], func=AF.Square, accum_out=sm[:, 0:1]),
                      reads=[x1t_b], writes=[junk_b, sm_b])
                sc.op("act", lambda e: e.activation(out=sm[:, 1:2], in_=sm[:, 0:1], func=AF.Sqrt, bias=EPS_AP(cx), scale=1.0 / D),
                      reads=[sm_b, cx.eps_b], writes=[sm_b])
                sc.op("dve", lambda e: e.reciprocal(out=sm[:, 2:3], in_=sm[:, 1:2]), reads=[sm_b], writes=[sm_b])
                sc.op("dve", lambda e: e.scalar_tensor_tensor(out=hb[:], in0=x1t[:], scalar=sm[:, 2:3], in1=gx[:],
                                                              op0=ALU.mult, op1=ALU.mult),
                      reads=[x1t_b, sm_b, gx_b], writes=[hb_b])
                hbl.append((hb, hb_b))
            for j in range(4):
                hb, hb_b = hbl[j]
                bt, bb = cx.bank()
                pb = bt[:, :].bitcast(BF16)
                for k in range(8):
                    tr(cx, bb, pb[:, k * 128:(k + 1) * 128], hb[:, k * 128:(k + 1) * 128], ident[:], [hb_b, ident_b])
                if j % 2:
                    sc.op("act", lambda e: e.copy(out=hxT[:, :, j * 128:(j + 1) * 128], in_=pb.rearrange("p (k n) -> p k n", k=8)),
                          reads=[bb], writes=[hxT_b])
                else:
                    sc.op("dve", lambda e: e.tensor_copy(out=hxT[:, :, j * 128:(j + 1) * 128], in_=pb.rearrange("p (k n) -> p k n", k=8)),
                          reads=[bb], writes=[hxT_b])
            qT, qT_b = qTs.next()
            for c in range(8):
                bt, bb = cx.bank()
                for k in range(8):
                    mm(cx, bb, bt[:, :], wq[:, k, c * 128:(c + 1) * 128], hxT[:, k, :], k == 0, k == 7, [wq_b, hxT_b])
                if c % 2:
                    sc.op("act", lambda e: e.copy(out=qT[:, c, :], in_=bt[:, :]), reads=[bb], writes=[qT_b])
                else:
                    sc.op("dve", lambda e: e.tensor_copy(out=qT[:, c, :], in_=bt[:, :]), reads=[bb], writes=[qT_b])
            return (qT, qT_b)

        def phaseB(q, ctxA):
            qT, qT_b = ctxA
            o, o_b = otm.next()
            pend = []

            def emit_pv(it):
                h, mt, pt, pt_b = it
                for j in range(4):
                    at, ab = cx.banks[4 + j]
                    mm(cx, ab, at[:, 0:257], pt[:, j * 128:(j + 1) * 128], vx[:, mt, h, :], mt == 0, mt == 1, [pt_b, vx_b])
                    if mt == 1:
                        z, z_b = zs.next()
                        sc.op("dve", lambda e: e.reciprocal(out=z[:, 0:1], in_=at[:, 256:257]), reads=[ab], writes=[z_b])
                        if j % 2:
                            sc.op("act", lambda e: e.activation(out=o[:, j, h * 256:(h + 1) * 256], in_=at[:, 0:256], func=AF.Copy,
                                                                scale=z[:, 0:1]),
                                  reads=[ab, z_b], writes=[o_b])
                        else:
                            sc.op("dve", lambda e: e.tensor_scalar(out=o[:, j, h * 256:(h + 1) * 256], in0=at[:, 0:256],
                                                                   scalar1=z[:, 0:1], scalar2=None, op0=ALU.mult),
                                  reads=[ab, z_b], writes=[o_b])

            for h in range(4):
                for mt in range(2):
                    bt, bb = cx.bank(0, 4)
                    for dc in range(2):
                        mm(cx, bb, bt[:, :], kT[:, 2 * h + dc, mt * 128:(mt + 1) * 128], qT[:, 2 * h + dc, :],
                           dc == 0, dc == 1, [kT_b, qT_b])
                    pt, pt_b = pts.next()
                    sc.op("act", lambda e: e.activation(out=pt[:], in_=bt[:, :], func=AF.Exp, scale=scale),
                          reads=[bb], writes=[pt_b])
                    pend.append((h, mt, pt, pt_b))
                    if len(pend) > 2:
                        emit_pv(pend.pop(0))
            for it in pend:
                emit_pv(it)
            def emit_T(j):
                bt, bb = cx.bank(0, 4)
                pb = bt[:, :].bitcast(BF16)
                for c in range(8):
                    tr(cx, bb, pb[:, c * 128:(c + 1) * 128], o[:, j, c * 128:(c + 1) * 128], ident[:], [o_b, ident_b])
                oT, oT_b = oTs.next()
                if j % 2:
                    sc.op("act", lambda e: e.copy(out=oT[:], in_=pb.rearrange("p (a n) -> p a n", a=8)), reads=[bb], writes=[oT_b])
                else:
                    sc.op("dve", lambda e: e.tensor_copy(out=oT[:], in_=pb.rearrange("p (a n) -> p a n", a=8)), reads=[bb], writes=[oT_b])
                rows = slice(q * 512 + j * 128, q * 512 + (j + 1) * 128)
                xr, xr_b = xrs.next()
                sc.dma("sp", xr[:], SC["x1"][0].ap()[rows, :], reads=[SC["x1"][1]], writes=[xr_b])
                return oT, oT_b, xr, xr_b

            nxt = emit_T(0)
            for j in range(4):
                rows = slice(q * 512 + j * 128, q * 512 + (j + 1) * 128)
                oT, oT_b, xr, xr_b = nxt
                if j + 1 < 4:
                    nxt = emit_T(j + 1)
                xo, xo_b = xos.next()
                for half in range(2):
                    ot, ob = cx.bank(0, 4)
                    for c in range(8):
                        mm(cx, ob, ot[:, :], oT[:, c, :], wo[:, c, half * 512:(half + 1) * 512], c == 0, c == 7, [oT_b, wo_b])
                    sc.op("dve", lambda e: e.tensor_tensor(out=xo[:, half * 512:(half + 1) * 512], in0=ot[:, :],
                                                           in1=xr[:, half * 512:(half + 1) * 512], op=ALU.add),
                          reads=[ob, xr_b], writes=[xo_b])
                sc.dma_store(SC["x2"][0].ap()[rows, :], xo[:], reads=[xo_b], writes=[SC["x2"][1]])

        ctxs = {0: phaseA(0)}
        if NQ > 1:
            ctxs[1] = phaseA(1)
        for q in range(NQ):
            phaseB(q, ctxs.pop(q))
            if q + 2 < NQ:
                ctxs[q + 2] = phaseA(q + 2)


def stage7(cx, I, SC, out_ap, out_b):
    nc, sc = cx.nc, cx.sc
    IOA = bass.IndirectOffsetOnAxis
    with contextlib.ExitStack() as st:
        identf, identf_b = cx.sb(st, "identf", [128, 128], F32)
        ident, ident_b = cx.sb(st, "ident7", [128, 128], BF16)
        sc.dma("sp", identf[:], I["ident_f"][:, :], writes=[identf_b])
        sc.dma("sp", ident[:], I["ident_bf"][:, :], writes=[ident_b])
        gf, gf_b = cx.sb(st, "g_ffn", [128, D], F32)
        gfin, gfin_b = cx.sb(st, "g_fin", [128, D], F32)
        sc.dma("sp", gf[:], I["g_ffn"][:, :], writes=[gf_b])
        sc.dma("sp", gfin[:], I["g_fin"][:, :], writes=[gfin_b])
        wr, wr_b = cx.sb(st, "wrouter", [128, 8, 36], F32)
        sc.dma("sp", wr[:], I["w_router"].rearrange("(k p) c -> p k c", p=128), writes=[wr_b])
        brow, brow_b = cx.sb(st, "brow", [1, 36], F32)
        sc.dma("sp", brow[:], I["b_router"][:, :], writes=[brow_b])
        onesr, onesr_b = cx.sb(st, "onesr", [1, 128], F32)
        sc.op("dve", lambda e: e.memset(onesr[:], 1.0), writes=[onesr_b])
        ustr, ustr_b = cx.sb(st, "ustr", [128, 128], BF16)
        sc.dma("sp", ustr[:], I["ustrict"][:, :], writes=[ustr_b])
        onesb, onesb_b = cx.sb(st, "onesb", [128, 128], BF16)
        sc.op("pool", lambda e: e.memset(onesb[:], 1.0), writes=[onesb_b])
        slotb, slotb_b = cx.sb(st, "slotb", [128, 32], F32)
        sc.dma("sp", slotb[:], I["slotbase"][:, :], writes=[slotb_b])
        tokid, tokid_b = cx.sb(st, "tokid", [128, NT], I32)
        sc.dma("sp", tokid[:], I["tokid"][:, :], writes=[tokid_b])
        cnt, cnt_b = cx.sb(st, "cnt", [128, 32], F32)
        sc.op("dve", lambda e: e.memset(cnt[:], 0.0), writes=[cnt_b])
        dest, dest_b = cx.sb(st, "dest", [128, NT, 2], I32)
        wall, wall_b = cx.sb(st, "wall", [128, NT, 2], F32)
        zi, zi_b = cx.sb(st, "zi", [128, 96], I32)
        sc.op("pool", lambda e: e.memset(zi[:], 0), writes=[zi_b])
        btok_t, btok_b = SC["buf_tok"]
        binit = Buf("btok_init")
        sc.dma("sp", btok_t.ap().rearrange("(p c) o -> p (c o)", p=128), zi[:], reads=[zi_b], writes=[binit])
        hfbf_t, hfbf_b = SC["hf_bf"]
        ys_t, ys_b = SC["ys"]
        junk, junk_b = cx.sb(st, "s7junk", [128, D], BF16)
        with contextlib.ExitStack() as st2:
            xts = cx.sbpool(st2, "s7x", [128, D], F32, 3)
            hfs = cx.sbpool(st2, "s7hf", [128, D], F32, 3)
            hbs = cx.sbpool(st2, "s7hb", [128, D], BF16, 3)
            hfTs = cx.sbpool(st2, "s7hfT", [128, 8, 128], F32, 3)
            sms = cx.sbpool(st2, "s7sm", [128, 4], F32, 3)
            LG, LG_b = cx.sb(st2, "s7LG", [128, NT, 36], F32)
            def a7(t):
                rows = slice(t * 128, (t + 1) * 128)
                xt, xt_b = xts.next(); hf, hf_b = hfs.next(); hb, hb_b = hbs.next()
                sm, sm_b = sms.next()
                sc.dma("sp", xt[:], SC["x2"][0].ap()[rows, :], reads=[SC["x2"][1]], writes=[xt_b])
                rms_rows(cx, xt, xt_b, gf, gf_b, hf, hf_b, junk, junk_b, sm, sm_b)
                sc.op("pool", lambda e: e.tensor_copy(out=hb[:], in_=hf[:]), reads=[hf_b], writes=[hb_b])
                sc.dma_store(hfbf_t.ap()[rows, :], hb[:], reads=[hb_b], writes=[hfbf_b])
                return hf, hf_b

            pre7 = {0: a7(0), 1: a7(1)}
            for t in range(NT):
                hf, hf_b = pre7.pop(t)
                hfT, hfT_b = hfTs.next()
                for half in range(2):
                    bt, bb = cx.bank()
                    for k4 in range(4):
                        k = half * 4 + k4
                        tr(cx, bb, bt[:, k4 * 128:(k4 + 1) * 128], hf[:, k * 128:(k + 1) * 128], identf[:], [hf_b, identf_b])
                    src = bt[:, :].rearrange("p (k n) -> p k n", k=4)
                    if half == 0:
                        sc.op("act", lambda e: e.copy(out=hfT[:, 0:4, :], in_=src), reads=[bb], writes=[hfT_b])
                    else:
                        sc.op("dve", lambda e: e.tensor_copy(out=hfT[:, 4:8, :], in_=src), reads=[bb], writes=[hfT_b])
                lt, lb = cx.bank()
                for k in range(8):
                    mm(cx, lb, lt[:, 0:36], hfT[:, k, :], wr[:, k, :], k == 0, False, [hfT_b, wr_b])
                mm(cx, lb, lt[:, 0:36], onesr[:, :], brow[:, :], False, True, [onesr_b, brow_b])
                sc.op("act", lambda e: e.copy(out=LG[:, t, :], in_=lt[:, 0:36]), reads=[lb], writes=[LG_b])
                if t + 2 < NT:
                    pre7[t + 2] = a7(t + 2)
            def T(name, shape, dt=F32):
                return cx.sb(st2, "s7_" + name, shape, dt)
            MX, MX_b = T("MX", [128, NT]); E4, E4_b = T("E4", [128, NT, 4]); PEN, PEN_b = T("PEN", [128, NT, 4])
            EX, EX_b = T("EX", [128, NT, 4]); SE, SE_b = T("SE", [128, NT]); GG, GG_b = T("GG", [128, NT])
            LEM, LEM_b = T("LEM", [128, NT, 32]); L1, L1_b = T("L1", [128, NT]); M1, M1_b = T("M1", [128, NT, 32])
            LEM2, LEM2_b = T("LEM2", [128, NT, 32]); L2, L2_b = T("L2", [128, NT]); M2, M2_b = T("M2", [128, NT, 32])
            DL, DL_b = T("DL", [128, NT]); SG, SG_b = T("SG", [128, NT]); MB, MB_b = T("MB", [128, NT, 32], BF16)
            RS, RS_b = T("RS", [128, NT, 32]); TMP, TMP_b = T("TMP", [128, NT, 32]); DF, DF_b = T("DF", [128, NT, 2])
            sc.op("dve", lambda e: e.tensor_reduce(out=MX[:], in_=LG[:, :, 0:4], axis=AX.X, op=ALU.max), reads=[LG_b], writes=[MX_b])
            sc.op("dve", lambda e: e.tensor_tensor(out=E4[:], in0=LG[:, :, 0:4], in1=MX[:, :].unsqueeze(2).broadcast_to([128, NT, 4]),
                                                   op=ALU.subtract), reads=[LG_b, MX_b], writes=[E4_b])
            sc.op("dve", lambda e: e.tensor_scalar(out=PEN[:], in0=E4[:], scalar1=1e12, scalar2=None, op0=ALU.mult),
                  reads=[E4_b], writes=[PEN_b])
            sc.op("act", lambda e: e.activation(out=EX[:], in_=E4[:], func=AF.Exp), reads=[E4_b], writes=[EX_b])
            sc.op("dve", lambda e: e.tensor_reduce(out=SE[:], in_=EX[:], axis=AX.X, op=ALU.add), reads=[EX_b], writes=[SE_b])
            sc.op("dve", lambda e: e.reciprocal(out=GG[:], in_=SE[:]), reads=[SE_b], writes=[GG_b])
            sc.op("dve", lambda e: e.tensor_tensor(out=LEM[:, :, :].rearrange("p t (g i) -> p t g i", g=4),
                                                   in0=LG[:, :, 4:36].rearrange("p t (g i) -> p t g i", g=4),
                                                   in1=PEN[:, :, :].unsqueeze(3).broadcast_to([128, NT, 4, 8]), op=ALU.add),
                  reads=[LG_b, PEN_b], writes=[LEM_b])
            sc.op("dve", lambda e: e.tensor_reduce(out=L1[:], in_=LEM[:], axis=AX.X, op=ALU.max), reads=[LEM_b], writes=[L1_b])
            sc.op("dve", lambda e: e.tensor_tensor(out=M1[:], in0=LEM[:], in1=L1[:, :].unsqueeze(2).broadcast_to([128, NT, 32]),
                                                   op=ALU.is_ge), reads=[LEM_b, L1_b], writes=[M1_b])
            sc.op("dve", lambda e: e.scalar_tensor_tensor(out=LEM2[:], in0=M1[:], scalar=-1e9, in1=LEM[:], op0=ALU.mult, op1=ALU.add),
                  reads=[M1_b, LEM_b], writes=[LEM2_b])
            sc.op("dve", lambda e: e.tensor_reduce(out=L2[:], in_=LEM2[:], axis=AX.X, op=ALU.max), reads=[LEM2_b], writes=[L2_b])
            sc.op("dve", lambda e: e.tensor_tensor(out=M2[:], in0=LEM2[:], in1=L2[:, :].unsqueeze(2).broadcast_to([128, NT, 32]),
                                                   op=ALU.is_ge), reads=[LEM2_b, L2_b], writes=[M2_b])
            sc.op("dve", lambda e: e.tensor_tensor(out=DL[:], in0=L1[:], in1=L2[:], op=ALU.subtract), reads=[L1_b, L2_b], writes=[DL_b])
            sc.op("act", lambda e: e.activation(out=SG[:], in_=DL[:], func=AF.Sigmoid), reads=[DL_b], writes=[SG_b])
            sc.op("dve", lambda e: e.tensor_tensor(out=wall[:, :, 0], in0=GG[:], in1=SG[:], op=ALU.mult),
                  reads=[GG_b, SG_b], writes=[wall_b])
            sc.op("dve", lambda e: e.tensor_tensor(out=wall[:, :, 1], in0=GG[:], in1=wall[:, :, 0], op=ALU.subtract),
                  reads=[GG_b, wall_b], writes=[wall_b])
            sc.op("dve", lambda e: e.tensor_tensor(out=MB[:], in0=M1[:], in1=M2[:], op=ALU.add), reads=[M1_b, M2_b], writes=[MB_b])
            for hb_ in range(2):
                rk, rkb = cx.bank()
                for tt in range(16):
                    t = hb_ * 16 + tt
                    oap = rk[:, tt * 32:(tt + 1) * 32]
                    mm(cx, rkb, oap, ustr[:], MB[:, t, :], tt == 0, False, [ustr_b, MB_b])
                    for tp in range(t):
                        mm(cx, rkb, oap, onesb[:], MB[:, tp, :], False, False, [onesb_b, MB_b])
                sc.op("dve", lambda e: e.scalar_tensor_tensor(out=RS[:, hb_ * 16:(hb_ + 1) * 16, :],
                                                              in0=rk[:, :].rearrange("p (t e) -> p t e", t=16),
                                                              scalar=float(CAP - 1),
                                                              in1=slotb[:, :].unsqueeze(1).broadcast_to([128, 16, 32]),
                                                              op0=ALU.min, op1=ALU.add),
                      reads=[rkb, slotb_b], writes=[RS_b])
            for k_, (MK, MK_b) in enumerate(((M1, M1_b), (M2, M2_b))):
                sc.op("dve", lambda e: e.tensor_tensor(out=TMP[:], in0=MK[:], in1=RS[:], op=ALU.mult), reads=[MK_b, RS_b], writes=[TMP_b])
                sc.op("dve", lambda e: e.tensor_reduce(out=DF[:, :, k_], in_=TMP[:], axis=AX.X, op=ALU.add), reads=[TMP_b], writes=[DF_b])
            sc.op("dve", lambda e: e.tensor_copy(out=dest[:], in_=DF[:]), reads=[DF_b], writes=[dest_b])
            for t in range(NT):
                for k_ in range(2):
                    sc.idma(out=btok_t.ap()[:, :], out_offset=IOA(ap=dest[:, t, k_:k_ + 1], axis=0),
                            in_=tokid[:, t:t + 1], in_offset=None,
                            reads=[dest_b, tokid_b, binit], writes=[btok_b])
        sc.barrier()
        with contextlib.ExitStack() as st2:
            w1s = cx.sbpool(st2, "we1", [128, 8, 512], BF16, 2)
            w3s = cx.sbpool(st2, "we3", [128, 8, 512], BF16, 2)
            w2s = cx.sbpool(st2, "we2", [128, 4, 1024], BF16, 2)
            idxs = cx.sbpool(st2, "s7idx", [128, 1], I32, 8)
            xbs = cx.sbpool(st2, "s7xb", [128, D], BF16, 6)
            xbTs = cx.sbpool(st2, "s7xbT", [128, 8, CAP], BF16, 2)
            sils = cx.sbpool(st2, "s7sil", [128, CAP], F32, 4)
            acts = cx.sbpool(st2, "s7act", [128, 4, CAP], BF16, 2)
            ybs = cx.sbpool(st2, "s7yb", [128, D], BF16, 4)
            def prep(ex):
                w1, w1_b = w1s.next(); w3, w3_b = w3s.next(); w2, w2_b = w2s.next()
                v1 = I["w_e1"][ex].rearrange("(k p) c -> p k c", p=128)
                v3 = I["w_e3"][ex].rearrange("(k p) c -> p k c", p=128)
                v2 = I["w_e2"][ex].rearrange("(k p) c -> p k c", p=128)
                sc.dma("pool", w1[:], v1, writes=[w1_b])
                sc.dma("pool", w3[:], v3, writes=[w3_b])
                sc.dma("pool", w2[:], v2, writes=[w2_b])
                xbT, xbT_b = xbTs.next()
                for blk in range(CAP // 128):
                    r0 = ex * CAP + blk * 128
                    idx, idx_b = idxs.next()
                    xb, xb_b = xbs.next()
                    sc.dma("sp", idx[:], btok_t.ap()[r0:r0 + 128, :], reads=[btok_b], writes=[idx_b])
                    sc.idma(out=xb[:, :], out_offset=None, in_=hfbf_t.ap()[:, :], in_offset=IOA(ap=idx[:, 0:1], axis=0),
                            reads=[idx_b, hfbf_b], writes=[xb_b])
                    bt, bb = cx.bank()
                    pb = bt[:, :].bitcast(BF16)
                    for k in range(8):
                        tr(cx, bb, pb[:, k * 128:(k + 1) * 128], xb[:, k * 128:(k + 1) * 128], ident[:], [xb_b, ident_b])
                    if blk % 2:
                        sc.op("dve", lambda e: e.tensor_copy(out=xbT[:, :, blk * 128:(blk + 1) * 128],
                                                             in_=pb.rearrange("p (k n) -> p k n", k=8)),
                              reads=[bb], writes=[xbT_b])
                    else:
                        sc.op("act", lambda e: e.copy(out=xbT[:, :, blk * 128:(blk + 1) * 128], in_=pb.rearrange("p (k n) -> p k n", k=8)),
                              reads=[bb], writes=[xbT_b])
                return dict(w1=w1, w1_b=w1_b, w3=w3, w3_b=w3_b, w2=w2, w2_b=w2_b, xbT=xbT, xbT_b=xbT_b)

            def main1(ex, P):
                w1, w1_b, w3, w3_b, xbT, xbT_b = P["w1"], P["w1_b"], P["w3"], P["w3_b"], P["xbT"], P["xbT_b"]
                act, act_b = acts.next()
                for c in range(4):
                    cs = slice(c * 128, (c + 1) * 128)
                    at, ab = cx.bank()
                    b3t, b3b = cx.bank()
                    for k in range(8):
                        mm(cx, ab, at[:, 0:CAP], w1[:, k, cs], xbT[:, k, :], k == 0, k == 7, [w1_b, xbT_b])
                    for k in range(8):
                        mm(cx, b3b, b3t[:, 0:CAP], w3[:, k, cs], xbT[:, k, :], k == 0, k == 7, [w3_b, xbT_b])
                    sl, sl_b = sils.next()
                    sc.op("act", lambda e: e.activation(out=sl[:], in_=at[:, 0:CAP], func=AF.Silu), reads=[ab], writes=[sl_b])
                    sc.op("dve", lambda e: e.tensor_tensor(out=act[:, c, :], in0=b3t[:, 0:CAP], in1=sl[:], op=ALU.mult),
                          reads=[b3b, sl_b], writes=[act_b])
                P["act"], P["act_b"] = act, act_b

            def main2(ex, P):
                act, act_b, w2, w2_b = P["act"], P["act_b"], P["w2"], P["w2_b"]
                for blk in range(CAP // 128):
                    r0 = ex * CAP + blk * 128
                    yb, yb_b = ybs.next()
                    for half in range(2):
                        ot, ob = cx.bank()
                        for c in range(4):
                            mm(cx, ob, ot[:, :], act[:, c, blk * 128:(blk + 1) * 128], w2[:, c, half * 512:(half + 1) * 512],
                               c == 0, c == 3, [act_b, w2_b])
                        if half == 0:
                            sc.op("act", lambda e: e.copy(out=yb[:, 0:512], in_=ot[:, :]), reads=[ob], writes=[yb_b])
                        else:
                            sc.op("dve", lambda e: e.tensor_copy(out=yb[:, 512:1024], in_=ot[:, :]), reads=[ob], writes=[yb_b])
                    sc.dma_store(ys_t.ap()[r0:r0 + 128, :], yb[:], reads=[yb_b], writes=[ys_b])

            PX = {0: prep(0)}
            for ex in range(NEXP):
                main1(ex, PX[ex])
                if ex + 1 < NEXP:
                    PX[ex + 1] = prep(ex + 1)
                main2(ex, PX.pop(ex))
        sc.barrier()
        with contextlib.ExitStack() as st2:
            xts = cx.sbpool(st2, "s7fx", [128, D], F32, 4)
            y1s = cx.sbpool(st2, "s7y1", [128, D], BF16, 4)
            y2s = cx.sbpool(st2, "s7y2", [128, D], BF16, 4)
            x3s = cx.sbpool(st2, "s7x3", [128, D], F32, 4)
            ous = cx.sbpool(st2, "s7ou", [128, D], F32, 4)
            sms = cx.sbpool(st2, "s7fsm", [128, 4], F32, 4)
            for t in range(NT):
                rows = slice(t * 128, (t + 1) * 128)
                xt, xt_b = xts.next(); y1, y1_b = y1s.next(); y2, y2_b = y2s.next()
                x3, x3_b = x3s.next(); ou, ou_b = ous.next(); sm, sm_b = sms.next()
                sc.dma("sp", xt[:], SC["x2"][0].ap()[rows, :], reads=[SC["x2"][1]], writes=[xt_b])
                sc.idma(out=y1[:, :], out_offset=None, in_=ys_t.ap()[:, :], in_offset=IOA(ap=dest[:, t, 0:1], axis=0),
                        reads=[dest_b, ys_b], writes=[y1_b])
                sc.idma(out=y2[:, :], out_offset=None, in_=ys_t.ap()[:, :], in_offset=IOA(ap=dest[:, t, 1:2], axis=0),
                        reads=[dest_b, ys_b], writes=[y2_b])
                sc.op("dve", lambda e: e.scalar_tensor_tensor(out=x3[:], in0=y1[:], scalar=wall[:, t, 0:1], in1=xt[:],
                                                              op0=ALU.mult, op1=ALU.add),
                      reads=[y1_b, wall_b, xt_b], writes=[x3_b])
                sc.op("dve", lambda e: e.scalar_tensor_tensor(out=x3[:], in0=y2[:], scalar=wall[:, t, 1:2], in1=x3[:],
                                                              op0=ALU.mult, op1=ALU.add),
                      reads=[y2_b, wall_b, x3_b], writes=[x3_b])
                rms_rows(cx, x3, x3_b, gfin, gfin_b, ou, ou_b, junk, junk_b, sm, sm_b)
                sc.dma_store(out_ap[rows, :], ou[:], reads=[ou_b], writes=[out_b])


def EPS_AP(cx):
    return cx.eps[:, 0:1]


def build(debug=None):
    nc = bass.Bass("TRN2", target_bir_lowering=False)
    cx = Ctx(nc)
    sc = cx.sc
    I = {}

    def inp(name, shape, dt):
        I[name] = nc.dram_tensor(name, shape, dt, kind="ExternalInput").ap()

    inp("x", [S, D], F32)
    inp("w_in", [D, D_IN], F32)
    inp("g_mix", [128, D], F32)
    inp("ident_bf", [128, 128], BF16)
    inp("rope_q_tab", [S, 2, 512], F32)
    inp("rope_k_tab", [S, 2, 512], F32)
    inp("decayT", [128, 512], F32)
    inp("xi_full", [128, 512], F32)
    inp("zeta_full", [128, 512], F32)
    for sfx in ("k", "v"):
        inp("cmp_w1_" + sfx, [32, 128, 128], F32)
        inp("cmp_w2_" + sfx, [128, 128], F32)
        inp("cmp_peT_" + sfx, [128, 32], F32)
    inp("vext_const", [2, 128, 65], BF16)
    inp("mask_bias", [128, 8, 512], BF16)
    inp("cmp_bias", [128, 2, S], BF16)
    inp("eexp", [128, S], BF16)
    inp("imp_masks", [S, 2, 2, 64], F32)
    for nm in ("w_ret_o", "w_nsa_o", "w_out", "w_xq", "w_xo"):
        inp(nm, [D, D], F32)
    inp("w_xkv", [D, 2 * D], F32)
    inp("mem", [256, D], F32)
    inp("g_x", [128, D], F32)
    inp("g_mem", [128, D], F32)
    inp("g_ffn", [128, D], F32)
    inp("g_fin", [128, D], F32)
    inp("ident_f", [128, 128], F32)
    inp("w_router", [D, 36], F32)
    inp("b_router", [1, 36], F32)
    inp("ustrict", [128, 128], BF16)
    inp("slotbase", [128, 32], F32)
    inp("tokid", [128, NT], I32)
    inp("w_e1", [NEXP, D, 512], F32)
    inp("w_e3", [NEXP, D, 512], F32)
    inp("w_e2", [NEXP, 512, D], F32)
    out = nc.dram_tensor("out", [S, D], F32, kind="ExternalOutput")
    SC = {}
    for name, shape, dt in [
        ("q_r", [S, 512], BF16), ("k_r", [S, 512], BF16), ("v_r", [S, 1024], BF16),
        ("g_r", [S, 1024], BF16), ("svwv", [S, 512], BF16), ("gl", [S, 24], F32),
        ("nqT", [8, 128, S], BF16), ("ckT", [2, 128, S], BF16), ("cvT", [2, 128, S], BF16),
        ("skT", [2, 128, S], BF16), ("wkT", [2, 128, S], BF16),
        ("gaT", [8, 128, S], BF16), ("gbT", [8, 128, S], BF16),
        ("retT", [NT, 128, D], BF16), ("nsaT", [NT, 128, D], BF16),
        ("x1", [S, D], F32), ("x2", [S, D], F32),
        ("hf_bf", [S, D], BF16), ("ys", [NEXP * CAP, D], BF16), ("buf_tok", [NEXP * CAP, 1], I32),
        ("dbg_kcmpT", [128, 2, 256], BF16), ("dbg_vext", [128, 2, 2, 193], BF16),
    ]:
        if debug and name in debug:
            t = nc.dram_tensor(name, shape, dt, kind="ExternalOutput")
            SC[name] = (t, Buf(name, multi=True))
        else:
            SC[name] = cx.dram(name, shape, dt)
    cx.eps, cx.eps_b = cx.sb(cx.es, "eps", [128, 1], F32)
    sc.op("dve", lambda e: e.memset(cx.eps[:], EPS), writes=[cx.eps_b])

    stop = int(_os.environ.get("STOPAFTER", 99))
    stage1(cx, I, SC)
    sc.barrier()
    if stop <= 1:
        return nc
    kcmpT, kcmpT_b = cx.sb(cx.es, "kcmpT", [128, 2, 256], BF16)
    vext, vext_b = cx.sb(cx.es, "vext", [128, 2, 2, 193], BF16)
    stage2(cx, I, SC, side=stage3_gen(cx, I, SC, kcmpT, kcmpT_b, vext, vext_b))
    sc.barrier()
    if stop <= 3:
        return nc
    HM = host_masks()
    stage4(cx, I, SC, kcmpT, kcmpT_b, vext, vext_b, HM)
    sc.barrier()
    if stop <= 4:
        return nc
    stage5(cx, I, SC)
    sc.barrier()
    if stop <= 5:
        return nc
    stage6(cx, I, SC)
    sc.barrier()
    if stop <= 6:
        return nc
    out_b = Buf("out", multi=True)
    stage7(cx, I, SC, out.ap(), out_b)
    sc.finish([out_b])
    if debug and "dbg_kcmpT" in debug:
        sc.dma("sp", SC["dbg_kcmpT"][0].ap()[:, :, :], kcmpT[:], reads=[kcmpT_b], writes=[SC["dbg_kcmpT"][1]])
        sc.dma("sp", SC["dbg_vext"][0].ap()[:, :, :, :], vext[:], reads=[vext_b], writes=[SC["dbg_vext"][1]])

    sc.finish([b for (_, b) in SC.values()])
    return nc


def host_consts():
    c = {}
    c["ident_bf"] = np.eye(128, dtype=np.float32).astype(ml_dtypes.bfloat16)
    pos = np.arange(S, dtype=np.float32)
    inv_freq = (10000.0 ** (-np.arange(0, 128, 2, dtype=np.float32) / 128)).astype(np.float32)
    ang = pos[:, None] * inv_freq[None, :]
    cos = np.cos(ang).astype(np.float32)
    sin = np.sin(ang).astype(np.float32)
    tq = np.stack([np.tile(cos, (1, 8)), np.tile(sin, (1, 8))], axis=1)
    c["rope_q_tab"] = np.ascontiguousarray(tq, dtype=np.float32)
    c["rope_k_tab"] = np.ascontiguousarray(tq * np.float32(128 ** -0.5), dtype=np.float32)
    n = np.arange(128, dtype=np.float64)
    decT = np.zeros((128, 4, 128), np.float64)
    xi = np.zeros((128, 4, 128), np.float64)
    ze = np.zeros((128, 4, 128), np.float64)
    for h in range(4):
        lg = np.log(GAMMAS[h])
        diff = n[None, :] - n[:, None]
        decT[:, h, :] = np.where(diff >= 0, np.exp(lg * np.maximum(diff, 0.0)), 0.0)
        xi[:, h, :] = np.exp(lg * (n + 1.0))[:, None]
        ze[:, h, :] = np.exp(lg * (127.0 - n))[:, None]
    c["decayT"] = decT.reshape(128, 512).astype(np.float32)
    c["xi_full"] = xi.reshape(128, 512).astype(np.float32)
    c["zeta_full"] = ze.reshape(128, 512).astype(np.float32)
    cstart = np.arange(256) * 16
    jstart = np.arange(NB) * 64
    ov = ((cstart[:, None] < jstart[None, :] + 64) & (cstart[:, None] + 32 > jstart[None, :])).astype(np.float32)
    ov[255, :] = 0.0
    vc = np.concatenate([np.ones((256, 1), np.float32), ov], axis=1)
    c["vext_const"] = np.ascontiguousarray(vc.reshape(2, 128, 65)).astype(ml_dtypes.bfloat16)
    return c


def host_masks():
    t = np.arange(S)
    n = np.arange(256)
    valid = (16 * n[:, None] + 31 <= t[None, :]) & (n[:, None] < NCMP)
    return {"cm_valid": [valid[0:128], valid[128:256]]}


def host_consts2():
    c = {}
    hm = host_masks()
    cb = np.where(np.stack(hm["cm_valid"], axis=1), 0.0, NEG).astype(np.float32)
    c["cmp_bias"] = np.ascontiguousarray(cb).astype(ml_dtypes.bfloat16)
    m = np.arange(128)[:, None]
    nn = np.arange(512)[None, :]
    mbias = np.zeros((128, 8, 512), np.float32)
    for r in range(4):
        mbias[:, r, :] = np.where(128 * r + m <= nn, 0.0, NEG)
    for r in range(-4, 0):
        mbias[:, 8 + r, :] = np.where(128 * r + m > nn - 512, 0.0, NEG)
    c["mask_bias"] = mbias.astype(ml_dtypes.bfloat16)
    e = np.zeros((128, S), np.float32)
    e[np.arange(S) // 64, np.arange(S)] = -NEG
    c["eexp"] = e.astype(ml_dtypes.bfloat16)
    t = np.arange(S)
    tb = t // 64
    jj = np.arange(NB)
    forced = (jj[None, :] == 0) | (jj[None, :] == tb[:, None]) | (jj[None, :] == tb[:, None] - 1)
    future = jj[None, :] > tb[:, None]
    m1 = np.where(future | forced, 0.0, 1.0)
    m2 = np.where(future, -1e4, np.where(forced, 1e4, 0.0))
    mm_ = np.stack([m1, m2], axis=1)[:, :, None, :]
    c["imp_masks"] = np.ascontiguousarray(np.broadcast_to(mm_, (S, 2, 2, NB))).astype(np.float32)
    c["ident_f"] = np.eye(128, dtype=np.float32)
    c["ustrict"] = np.triu(np.ones((128, 128), np.float32), 1).astype(ml_dtypes.bfloat16)
    c["slotbase"] = np.ascontiguousarray(np.broadcast_to((np.arange(32) * CAP).astype(np.float32)[None, :], (128, 32)))
    c["tokid"] = np.ascontiguousarray((np.arange(NT)[None, :] * 128 + np.arange(128)[:, None]).astype(np.int32))
    return c


def permute_w_in(w):
    offs = np.cumsum([0, 512, 512, 1024, 1024, 1024, 256, 256, 256, 256, 256, 256, 24, 1024, 1024])
    rq, rk, rv, rg, nq, ck, cv, sk, sv, wk, wv, ngl, ga, gb = [np.arange(offs[i], offs[i + 1]) for i in range(14)]
    hp = np.concatenate([np.arange(0, 128, 2), np.arange(1, 128, 2)])
    perm4 = np.concatenate([h * 128 + hp for h in range(4)])
    order = np.concatenate([rq[perm4], rk[perm4], rv, rg, sv, wv, ngl, nq, ck, cv, sk, wk, ga, gb])
    return np.ascontiguousarray(w[:, order])


def kernel(**inputs):
    f = lambda a: np.ascontiguousarray(np.asarray(a, dtype=np.float32))
    bc = lambda v: np.ascontiguousarray(np.broadcast_to(f(v).reshape(1, D), (128, D)))
    shared = {}
    shared.update(host_consts())
    shared.update(host_consts2())
    shared["w_in"] = permute_w_in(f(inputs["w_in"])[0])
    shared["g_mix"] = bc(inputs["norm_mix_g"][0])
    for sfx in ("k", "v"):
        shared["cmp_w1_" + sfx] = f(inputs["cmp_w1_" + sfx][0])
        shared["cmp_w2_" + sfx] = f(inputs["cmp_w2_" + sfx][0])
        shared["cmp_peT_" + sfx] = np.ascontiguousarray(f(inputs["cmp_pe_" + sfx][0]).T)
    for nm in ("w_ret_o", "w_nsa_o", "w_out", "w_xq", "w_xo", "w_xkv"):
        shared[nm] = f(inputs[nm][0])
    shared["g_x"] = bc(inputs["norm_x_g"][0])
    shared["g_mem"] = bc(inputs["norm_mem_g"][0])
    shared["g_ffn"] = bc(inputs["norm_ffn_g"][0])
    shared["g_fin"] = bc(inputs["norm_f_g"])
    shared["w_router"] = np.ascontiguousarray(np.concatenate([f(inputs["w_grp"][0]), f(inputs["w_rt"][0])], axis=1))
    shared["b_router"] = np.ascontiguousarray(np.concatenate([f(inputs["b_grp"][0]), f(inputs["b_rt"][0])])[None, :])
    for nm in ("w_e1", "w_e3", "w_e2"):
        shared[nm] = f(inputs[nm][0])
    x = f(inputs["x"])
    mem = f(inputs["mem"])
    n = x.shape[0]
    in_maps = []
    for b in range(n):
        m = dict(shared)
        m["x"] = np.ascontiguousarray(x[b])
        m["mem"] = np.ascontiguousarray(mem[b])
        in_maps.append(m)
    nc = build()
    res = run_bass_kernel_spmd(nc, in_maps, core_ids=list(range(n)))
    return np.stack([np.asarray(r["out"], dtype=np.float32) for r in res.results], axis=0)
```

```python
import contextlib
import os as _os
import numpy as np
import ml_dtypes
import concourse.bass as bass
import concourse.mybir as mybir
from concourse.bass_utils import run_bass_kernel_spmd

F32 = mybir.dt.float32
BF16 = mybir.dt.bfloat16
I32 = mybir.dt.int32
U32 = mybir.dt.uint32
ALU = mybir.AluOpType
AF = mybir.ActivationFunctionType
AX = mybir.AxisListType

S = 4096
D = 1024
NT = S // 128
NQ = S // 512
EPS = 1e-6
NEG = -30000.0
D_IN = 7704
TM_COLS = 3608
FM_COLS = 4096
R_HEADS = 4
GAMMAS = [1.0 - 2.0 ** (-5.0 - h) for h in range(R_HEADS)]
NCMP = 255
NB = S // 64
CAP = 384
NEXP = 32


class Buf:
    __slots__ = ("w", "r", "multi", "name", "excl")

    def __init__(self, name="", multi=False, excl=False):
        self.excl = excl
        self.w = {}
        self.r = {}
        self.multi = multi
        self.name = name


class Sched:
    def __init__(self, nc):
        self.nc = nc
        self.es = contextlib.ExitStack()
        self.eng = {"pe": nc.tensor, "act": nc.scalar, "dve": nc.vector,
                    "pool": nc.gpsimd, "sp": nc.sync}
        self.sem = {k: self.es.enter_context(nc.semaphore("s_" + k)) for k in self.eng}
        self.cnt = {k: 0 for k in self.eng}
        self.waited = {k: {} for k in self.eng}
        self.ndma = 24
        self.dsem = [self.es.enter_context(nc.semaphore("d%d" % i)) for i in range(self.ndma)]
        self.dcnt = [0] * self.ndma
        self.dnext = 0
        self.nst = 16
        self.ssem = [self.es.enter_context(nc.semaphore("st%d" % i)) for i in range(self.nst)]
        self.scnt = [0] * self.nst
        self.snext = 0
        self.pending = []
        self.max_pending = 8

    def _semof(self, key):
        if isinstance(key, str):
            return self.sem[key]
        return self.dsem[key[1]] if key[0] == "d" else self.ssem[key[1]]

    def _flush_until(self, key, val):
        while self.pending:
            p = self.pending.pop(0)
            self._emit_store(p)
            if p["key"] == key and p["val"] >= val:
                break

    def flush_stores(self):
        while self.pending:
            self._emit_store(self.pending.pop(0))

    def _emit_store(self, p):
        key, val = p["key"], p["val"]
        if val > 16:
            self._wait("sp", key, val - 16)
        for k, v in p["deps"].items():
            self._wait("sp", k, v)
        self.eng["sp"].dma_start(out=p["out"], in_=p["in_"]).then_inc(self.ssem[key[1]], 16)

    def dma_store(self, out, in_, reads=(), writes=()):
        deps = {}
        for b in reads:
            for k, v in b.w.items():
                if v > deps.get(k, 0):
                    deps[k] = v
        for b in writes:
            assert b.multi
        i = self.snext
        self.snext = (i + 1) % self.nst
        self.scnt[i] += 1
        key, val = ("s", i), 16 * self.scnt[i]
        self._mark(key, val, reads, writes)
        self.pending.append({"key": key, "val": val, "deps": deps, "out": out, "in_": in_})
        if len(self.pending) > self.max_pending:
            self._emit_store(self.pending.pop(0))

    def _wait(self, e, key, val):
        if self.waited[e].get(key, 0) >= val:
            return
        if not isinstance(key, str) and key[0] == "s":
            if any(p["key"] == key and p["val"] <= val for p in self.pending):
                self._flush_until(key, val)
        self.eng[e].wait_ge(self._semof(key), val)
        self.waited[e][key] = val

    def _deps(self, e, reads, writes, same_ok):
        deps = {}
        for b in reads:
            for k, v in b.w.items():
                if v > deps.get(k, 0):
                    deps[k] = v
            if b.excl:
                for k, v in b.r.items():
                    if k != e and v > deps.get(k, 0):
                        deps[k] = v
        for b in writes:
            if b.multi:
                continue
            for k, v in b.w.items():
                if v > deps.get(k, 0):
                    deps[k] = v
            for k, v in b.r.items():
                if v > deps.get(k, 0):
                    deps[k] = v
        for k, v in deps.items():
            if same_ok and k == e:
                continue
            self._wait(e, k, v)

    def _mark(self, key, val, reads, writes):
        for b in writes:
            if b.multi:
                b.w[key] = val
            else:
                b.w = {key: val}
                b.r = {}
        for b in reads:
            if b not in writes:
                b.r[key] = val

    def op(self, e, fn, reads=(), writes=(), same_ok=False):
        self._deps(e, reads, writes, same_ok)
        ins = fn(self.eng[e])
        self.cnt[e] += 1
        ins.then_inc(self.sem[e], 1)
        self._mark(e, self.cnt[e], reads, writes)

    def dma(self, q, out, in_, reads=(), writes=(), **kw):
        i = self.dnext
        self.dnext = (i + 1) % self.ndma
        key = ("d", i)
        if self.dcnt[i]:
            self._wait(q, key, 16 * self.dcnt[i])
        self._deps(q, reads, writes, False)
        self.dcnt[i] += 1
        self.eng[q].dma_start(out=out, in_=in_, **kw).then_inc(self.dsem[i], 16)
        self._mark(key, 16 * self.dcnt[i], reads, writes)

    def idma(self, out, out_offset, in_, in_offset, reads=(), writes=(), **kw):
        q = "pool"
        i = self.dnext
        self.dnext = (i + 1) % self.ndma
        key = ("d", i)
        if self.dcnt[i]:
            self._wait(q, key, 16 * self.dcnt[i])
        self._deps(q, reads, writes, False)
        self.dcnt[i] += 1
        self.eng[q].indirect_dma_start(out=out, out_offset=out_offset, in_=in_,
                                       in_offset=in_offset, **kw).then_inc(self.dsem[i], 16)
        self._mark(key, 16 * self.dcnt[i], reads, writes)

    def barrier(self):
        self.flush_stores()
        for e in self.eng:
            for i in range(self.nst):
                if self.scnt[i]:
                    self._wait(e, ("s", i), 16 * self.scnt[i])
            for k in self.eng:
                if k != e and self.cnt[k]:
                    self._wait(e, k, self.cnt[k])
            for i in range(self.ndma):
                if self.dcnt[i]:
                    self._wait(e, ("d", i), 16 * self.dcnt[i])

    def finish(self, bufs):
        self.flush_stores()
        for b in bufs:
            for k, v in b.w.items():
                self._wait("sp", k, v)


class Pool2:
    def __init__(self, items):
        self.items = items
        self.i = 0

    def next(self):
        it = self.items[self.i]
        self.i = (self.i + 1) % len(self.items)
        return it


class Ctx:
    def __init__(self, nc):
        self.nc = nc
        self.sc = Sched(nc)
        self.es = self.sc.es
        self.banks = []
        for i in range(8):
            t = self.es.enter_context(nc.psum_tensor("bank%d" % i, [128, 512], F32))
            self.banks.append((t, Buf("bank%d" % i, excl=True)))
        self.bank_i = 0

    def bank(self, lo=0, hi=8):
        if not (lo <= self.bank_i < hi):
            self.bank_i = lo
        b = self.banks[self.bank_i]
        self.bank_i += 1
        if self.bank_i >= hi:
            self.bank_i = lo
        return b

    def sb(self, stack, name, shape, dt):
        t = stack.enter_context(self.nc.sbuf_tensor("sb_" + name, shape, dt))
        return t, Buf(name)

    def sbpool(self, stack, name, shape, dt, n):
        return Pool2([self.sb(stack, "%s%d" % (name, i), shape, dt) for i in range(n)])

    def dram(self, name, shape, dt):
        t = self.nc.dram_tensor(name, shape, dt, kind="Internal")
        return t, Buf(name, multi=True)


def mm(cx, bank, out_ap, lhsT, rhs, start, stop, reads):
    cx.sc.op("pe", lambda e: e.matmul(out_ap, lhsT, rhs, start=start, stop=stop),
             reads=reads, writes=[bank], same_ok=True)


def tr(cx, bank, out_ap, in_ap, ident, reads):
    cx.sc.op("pe", lambda e: e.transpose(out_ap, in_ap, ident),
             reads=reads, writes=[bank], same_ok=True)


def stage1(cx, I, SC):
    nc, sc = cx.nc, cx.sc
    with contextlib.ExitStack() as st:
        hT, hT_b = cx.sb(st, "hT", [128, 8, S], BF16)
        gbc, gbc_b = cx.sb(st, "gbc", [128, D], F32)
        ident, ident_b = cx.sb(st, "ident1", [128, 128], BF16)
        xts = cx.sbpool(st, "xt", [128, D], F32, 4)
        junk, junk_b = cx.sb(st, "junk", [128, D], BF16)
        hbs = cx.sbpool(st, "hb", [128, D], BF16, 4)
        stat = cx.sbpool(st, "stat", [128, 4], F32, 4)
        sc.dma("sp", gbc[:], I["g_mix"][:, :], writes=[gbc_b])
        sc.dma("sp", ident[:], I["ident_bf"][:, :], writes=[ident_b])
        x_d = I["x"]
        def a1(t):
            xt, xt_b = xts.next()
            hb, hb_b = hbs.next()
            sm, sm_b = stat.next()
            sc.dma("sp", xt[:], x_d[t * 128:(t + 1) * 128, :], writes=[xt_b])
            sc.op("act", lambda e: e.activation(out=junk[:], in_=xt[:], func=AF.Square,
                                                accum_out=sm[:, 0:1]),
                  reads=[xt_b], writes=[junk_b, sm_b])
            sc.op("act", lambda e: e.activation(out=sm[:, 1:2], in_=sm[:, 0:1], func=AF.Sqrt,
                                                bias=EPS_AP(cx), scale=1.0 / D),
                  reads=[sm_b, cx.eps_b], writes=[sm_b])
            sc.op("dve", lambda e: e.reciprocal(out=sm[:, 2:3], in_=sm[:, 1:2]),
                  reads=[sm_b], writes=[sm_b])
            sc.op("dve", lambda e: e.scalar_tensor_tensor(out=hb[:], in0=xt[:], scalar=sm[:, 2:3],
                                                          in1=gbc[:], op0=ALU.mult, op1=ALU.mult),
                  reads=[xt_b, sm_b, gbc_b], writes=[hb_b])
            return hb, hb_b

        pre1 = {0: a1(0), 1: a1(1)}
        for t in range(NT):
            hb, hb_b = pre1.pop(t)
            bt, bb = cx.bank(0, 4)
            pbf = bt[:, :].bitcast(BF16)
            for k in range(8):
                tr(cx, bb, pbf[:, k * 128:(k + 1) * 128], hb[:, k * 128:(k + 1) * 128], ident[:],
                   reads=[hb_b, ident_b])
            src = pbf.rearrange("p (k n) -> p k n", k=8)
            dst = hT[:, :, t * 128:(t + 1) * 128]
            if t % 2 == 0:
                sc.op("act", lambda e: e.copy(out=dst, in_=src), reads=[bb], writes=[hT_b])
            else:
                sc.op("dve", lambda e: e.tensor_copy(out=dst, in_=src), reads=[bb], writes=[hT_b])
            if t + 2 < NT:
                pre1[t + 2] = a1(t + 2)

        w_d = I["w_in"]
        wv_ = w_d.rearrange("(k p) c -> p k c", p=128)
        wts = cx.sbpool(st, "wblk", [128, 8, 512], BF16, 2)
        tabs = cx.sbpool(st, "ropetab", [128, 2, 512], F32, 3)
        ropeP = cx.sbpool(st, "ropeP", [128, 512], F32, 3)
        ropeQ = cx.sbpool(st, "ropeQ", [128, 512], F32, 3)
        ropeC = cx.sbpool(st, "ropeC", [128, 512], F32, 3)
        outs = cx.sbpool(st, "ev", [128, 512], BF16, 4)
        outf = cx.sbpool(st, "evf", [128, 32], F32, 2)

        def load_w(c0, ncol):
            wt, wt_b = wts.next()
            sc.dma("pool", wt[:, :, 0:ncol], wv_[:, :, c0:c0 + ncol], writes=[wt_b])
            return wt, wt_b

        evi = [0]

        def copy_ev(dst, src, rd, wr, func=None):
            if func is not None:
                sc.op("act", lambda e: e.activation(out=dst, in_=src, func=func), reads=rd, writes=wr)
                return
            evi[0] += 1
            if evi[0] % 2:
                sc.op("act", lambda e: e.copy(out=dst, in_=src), reads=rd, writes=wr)
            else:
                sc.op("dve", lambda e: e.tensor_copy(out=dst, in_=src), reads=rd, writes=wr)

        tm_blocks = [
            (0, 512, "rope_q", SC["q_r"], 0), (512, 512, "rope_k", SC["k_r"], 0),
            (1024, 512, "copy", SC["v_r"], 0), (1536, 512, "copy", SC["v_r"], 512),
            (2048, 512, "silu", SC["g_r"], 0), (2560, 512, "silu", SC["g_r"], 512),
            (3072, 512, "copy", SC["svwv"], 0), (3584, 24, "sig32", SC["gl"], 0),
        ]
        for (c0, ncol, kind, (dst_t, dst_b), dc0) in tm_blocks:
            wt, wt_b = load_w(c0, ncol)
            for t in range(NT):
                bt, bb = cx.bank(0, 4)
                for k in range(8):
                    mm(cx, bb, bt[:, 0:ncol], hT[:, k, t * 128:(t + 1) * 128], wt[:, k, 0:ncol],
                       k == 0, k == 7, reads=[hT_b, wt_b])
                rows = slice(t * 128, (t + 1) * 128)
                if kind in ("rope_q", "rope_k"):
                    tb, tb_b = tabs.next()
                    pp, pp_b = ropeP.next()
                    qq, qq_b = ropeQ.next()
                    pc, pc_b = ropeC.next()
                    ev, ev_b = outs.next()
                    tname = "rope_q_tab" if kind == "rope_q" else "rope_k_tab"
                    sc.dma("sp", tb[:], I[tname][rows, :, :], writes=[tb_b])
                    sc.op("act", lambda e: e.copy(out=pc[:], in_=bt[:, 0:512]), reads=[bb], writes=[pc_b])
                    sc.op("dve", lambda e: e.tensor_tensor(out=pp[:], in0=bt[:, 0:512], in1=tb[:, 0, :], op=ALU.mult),
                          reads=[bb, tb_b], writes=[pp_b])
                    sc.op("pool", lambda e: e.tensor_tensor(out=qq[:], in0=pc[:], in1=tb[:, 1, :], op=ALU.mult),
                          reads=[pc_b, tb_b], writes=[qq_b])
                    pv_ = pp[:, :].rearrange("p (h d) -> p h d", h=4)
                    qv_ = qq[:, :].rearrange("p (h d) -> p h d", h=4)
                    evv = ev[:, :].rearrange("p (h d) -> p h d", h=4)
                    sc.op("dve", lambda e: e.tensor_tensor(out=evv[:, :, 0:64], in0=pv_[:, :, 0:64], in1=qv_[:, :, 64:128],
                                                           op=ALU.subtract),
                          reads=[pp_b, qq_b], writes=[ev_b])
                    sc.op("pool", lambda e: e.tensor_tensor(out=evv[:, :, 64:128], in0=qv_[:, :, 0:64], in1=pv_[:, :, 64:128],
                                                            op=ALU.add),
                          reads=[pp_b, qq_b], writes=[ev_b])
                    sc.dma_store(dst_t.ap()[rows, dc0:dc0 + 512], ev[:, :], reads=[ev_b], writes=[dst_b])
                elif kind == "sig32":
                    ev, ev_b = outf.next()
                    copy_ev(ev[:, 0:ncol], bt[:, 0:ncol], [bb], [ev_b], func=AF.Sigmoid)
                    sc.dma_store(dst_t.ap()[rows, 0:ncol], ev[:, 0:ncol], reads=[ev_b], writes=[dst_b])
                else:
                    ev, ev_b = outs.next()
                    copy_ev(ev[:, 0:ncol], bt[:, 0:ncol], [bb], [ev_b],
                            func=AF.Silu if kind == "silu" else None)
                    sc.dma_store(dst_t.ap()[rows, dc0:dc0 + ncol], ev[:, 0:ncol], reads=[ev_b], writes=[dst_b])

        fm_dst = ([(SC["nqT"], i) for i in range(8)] + [(SC["ckT"], i) for i in range(2)] +
                  [(SC["cvT"], i) for i in range(2)] + [(SC["skT"], i) for i in range(2)] +
                  [(SC["wkT"], i) for i in range(2)] + [(SC["gaT"], i) for i in range(8)] +
                  [(SC["gbT"], i) for i in range(8)])
        for wb in range(8):
            wt, wt_b = load_w(TM_COLS + wb * 512, 512)
            for j in range(4):
                blk = wb * 4 + j
                (dst_t, dst_b), di = fm_dst[blk]
                is_gate = blk >= 16
                for q in range(NQ):
                    bt, bb = cx.bank(0, 4)
                    for k in range(8):
                        mm(cx, bb, bt[:, :], wt[:, k, j * 128:(j + 1) * 128], hT[:, k, q * 512:(q + 1) * 512],
                           k == 0, k == 7, reads=[hT_b, wt_b])
                    ev, ev_b = outs.next()
                    copy_ev(ev[:, :], bt[:, :], [bb], [ev_b], func=AF.Sigmoid if is_gate else None)
                    sc.dma_store(dst_t.ap()[di, :, q * 512:(q + 1) * 512], ev[:, :], reads=[ev_b], writes=[dst_b])


def stage2(cx, I, SC, side=None):
    nc, sc = cx.nc, cx.sc
    with contextlib.ExitStack() as st:
        ident, ident_b = cx.sb(st, "ident2", [128, 128], BF16)
        decT, decT_b = cx.sb(st, "decT", [128, 512], F32)
        xif, xif_b = cx.sb(st, "xif", [128, 512], F32)
        zef, zef_b = cx.sb(st, "zef", [128, 512], F32)
        sc.dma("sp", ident[:], I["ident_bf"][:, :], writes=[ident_b])
        sc.dma("sp", decT[:], I["decayT"][:, :], writes=[decT_b])
        sc.dma("sp", xif[:], I["xi_full"][:, :], writes=[xif_b])
        sc.dma("sp", zef[:], I["zeta_full"][:, :], writes=[zef_b])
        states = [cx.sb(st, "state%d" % i, [128, 4, 256], F32) for i in range(2)]
        sc.op("dve", lambda e: e.memset(states[0][0][:], 0.0), writes=[states[0][1]])
        NSTB = 6
        stbs = [cx.sb(st, "stb%d" % i, [128, 4, 256], BF16) for i in range(NSTB)]
        sc.op("pool", lambda e: e.memset(stbs[0][0][:], 0.0), writes=[stbs[0][1]])
        LD = 6
        qs = cx.sbpool(st, "rq", [128, 512], BF16, LD)
        ks = cx.sbpool(st, "rk", [128, 512], BF16, LD)
        vs = cx.sbpool(st, "rv", [128, 1024], BF16, LD)
        gs = cx.sbpool(st, "rg", [128, 1024], BF16, LD)
        qxs = cx.sbpool(st, "rqx", [128, 512], BF16, 3)
        kzs = cx.sbpool(st, "rkz", [128, 512], BF16, 3)
        tps = cx.sbpool(st, "rtp", [128, 12, 128], BF16, 3)
        sms = cx.sbpool(st, "rsm", [128, 512], BF16, 3)
        ros = cx.sbpool(st, "rro", [128, 1024], BF16, 3)
        rts = cx.sbpool(st, "rrt", [128, 8, 128], BF16, 3)
        nst = cx.sbpool(st, "rns", [128, 12], F32, 4)
        junk, junk_b = cx.sb(st, "rjunk", [128, 256], BF16)
        retT_t, retT_b = SC["retT"]
        tiles = {}

        def load(c):
            rows = slice(c * 128, (c + 1) * 128)
            q, q_b = qs.next(); k, k_b = ks.next(); v, v_b = vs.next(); g, g_b = gs.next()
            sc.dma("sp", k[:], SC["k_r"][0].ap()[rows, :], reads=[SC["k_r"][1]], writes=[k_b])
            sc.dma("sp", v[:], SC["v_r"][0].ap()[rows, :], reads=[SC["v_r"][1]], writes=[v_b])
            sc.dma("sp", q[:], SC["q_r"][0].ap()[rows, :], reads=[SC["q_r"][1]], writes=[q_b])
            sc.dma("sp", g[:], SC["g_r"][0].ap()[rows, :], reads=[SC["g_r"][1]], writes=[g_b])
            tiles[c] = dict(q=q, q_b=q_b, k=k, k_b=k_b, v=v, v_b=v_b, g=g, g_b=g_b)

        def P1(c):
            T = tiles[c]
            k, k_b, v, v_b = T["k"], T["k_b"], T["v"], T["v_b"]
            kz, kz_b = kzs.next()
            sc.op("pool", lambda e: e.tensor_tensor(out=kz[:], in0=k[:], in1=zef[:], op=ALU.mult),
                  reads=[k_b, zef_b], writes=[kz_b])
            kbanks = [cx.bank(), cx.bank()]
            for h in range(4):
                kt, kb = kbanks[h // 2]
                mm(cx, kb, kt[:, (h % 2) * 256:(h % 2 + 1) * 256], kz[:, h * 128:(h + 1) * 128],
                   v[:, h * 256:(h + 1) * 256], True, True, [kz_b, v_b])
            old, old_b = states[c % 2]
            new, new_b = states[(c + 1) % 2]
            for h in range(4):
                kt, kb = kbanks[h // 2]
                sc.op("dve", lambda e: e.scalar_tensor_tensor(out=new[:, h, :], in0=old[:, h, :],
                                                              scalar=float(GAMMAS[h] ** 128),
                                                              in1=kt[:, (h % 2) * 256:(h % 2 + 1) * 256],
                                                              op0=ALU.mult, op1=ALU.add),
                      reads=[kb, old_b], writes=[new_b])
            sb, sb_b = stbs[(c + 1) % NSTB]
            sc.op("act", lambda e: e.copy(out=sb[:], in_=new[:]), reads=[new_b], writes=[sb_b])

        def phaseA(c):
            T = tiles[c]
            q, q_b, k, k_b = T["q"], T["q_b"], T["k"], T["k_b"]
            qx, qx_b = qxs.next()
            sc.op("pool", lambda e: e.tensor_tensor(out=qx[:], in0=q[:], in1=xif[:], op=ALU.mult),
                  reads=[q_b, xif_b], writes=[qx_b])
            tp, tp_b = tps.next()
            b1t, b1b = cx.bank(); b2t, b2b = cx.bank()
            p1 = b1t[:, :].bitcast(BF16); p2 = b2t[:, :].bitcast(BF16)
            for h in range(4):
                tr(cx, b1b, p1[:, h * 128:(h + 1) * 128], q[:, h * 128:(h + 1) * 128], ident[:], [q_b, ident_b])
            for h in range(4):
                tr(cx, b2b, p2[:, h * 128:(h + 1) * 128], k[:, h * 128:(h + 1) * 128], ident[:], [k_b, ident_b])
            for h in range(4):
                tr(cx, b1b, p1[:, (4 + h) * 128:(5 + h) * 128], qx[:, h * 128:(h + 1) * 128], ident[:], [qx_b, ident_b])
            sc.op("act", lambda e: e.copy(out=tp[:, 0:8, :], in_=p1.rearrange("p (a n) -> p a n", a=8)),
                  reads=[b1b], writes=[tp_b])
            sc.op("dve", lambda e: e.tensor_copy(out=tp[:, 8:12, :], in_=p2[:, 0:512].rearrange("p (a n) -> p a n", a=4)),
                  reads=[b2b], writes=[tp_b])
            T.update(tp=tp, tp_b=tp_b)

        def phaseA2(c):
            T = tiles[c]
            tp, tp_b = T["tp"], T["tp_b"]
            bst, bsb = cx.bank()
            for h in range(4):
                mm(cx, bsb, bst[:, h * 128:(h + 1) * 128], tp[:, 8 + h, :], tp[:, h, :], True, True, [tp_b])
            sm, sm_b = sms.next()
            sc.op("dve", lambda e: e.tensor_tensor(out=sm[:], in0=bst[:, :], in1=decT[:], op=ALU.mult),
                  reads=[bsb, decT_b], writes=[sm_b])
            T.update(sm=sm, sm_b=sm_b)

        def phaseB(c):
            T = tiles.pop(c)
            v, v_b, g, g_b, tp, tp_b, sm, sm_b = T["v"], T["v_b"], T["g"], T["g_b"], T["tp"], T["tp_b"], T["sm"], T["sm_b"]
            sb, sb_b = stbs[c % NSTB]
            obanks = [cx.bank(), cx.bank()]
            for h in range(4):
                ot, ob = obanks[h // 2]
                oap = ot[:, (h % 2) * 256:(h % 2 + 1) * 256]
                mm(cx, ob, oap, sm[:, h * 128:(h + 1) * 128], v[:, h * 256:(h + 1) * 256], True, False, [sm_b, v_b])
                mm(cx, ob, oap, tp[:, 4 + h, :], sb[:, h, :], False, True, [tp_b, sb_b])
            ns, ns_b = nst.next()
            for h in range(4):
                ot, ob = obanks[h // 2]
                oap = ot[:, (h % 2) * 256:(h % 2 + 1) * 256]
                sc.op("act", lambda e: e.activation(out=junk[:], in_=oap, func=AF.Square, accum_out=ns[:, h:h + 1]),
                      reads=[ob], writes=[junk_b, ns_b])
            sc.op("act", lambda e: e.activation(out=ns[:, 4:8], in_=ns[:, 0:4], func=AF.Sqrt,
                                                bias=EPS_AP(cx), scale=1.0 / 256),
                  reads=[ns_b, cx.eps_b], writes=[ns_b])
            sc.op("dve", lambda e: e.reciprocal(out=ns[:, 8:12], in_=ns[:, 4:8]), reads=[ns_b], writes=[ns_b])
            ro, ro_b = ros.next()
            for h in range(4):
                ot, ob = obanks[h // 2]
                oap = ot[:, (h % 2) * 256:(h % 2 + 1) * 256]
                sc.op("dve", lambda e: e.scalar_tensor_tensor(out=ro[:, h * 256:(h + 1) * 256], in0=oap,
                                                              scalar=ns[:, 8 + h:9 + h], in1=g[:, h * 256:(h + 1) * 256],
                                                              op0=ALU.mult, op1=ALU.mult),
                      reads=[ob, ns_b, g_b], writes=[ro_b])

            def fin():
                btt, btb = cx.bank()
                pt = btt[:, :].bitcast(BF16)
                for j in range(8):
                    tr(cx, btb, pt[:, j * 128:(j + 1) * 128], ro[:, j * 128:(j + 1) * 128], ident[:], [ro_b, ident_b])
                rt, rt_b = rts.next()
                if c % 2:
                    sc.op("act", lambda e: e.copy(out=rt[:], in_=pt.rearrange("p (a n) -> p a n", a=8)),
                          reads=[btb], writes=[rt_b])
                else:
                    sc.op("dve", lambda e: e.tensor_copy(out=rt[:], in_=pt.rearrange("p (a n) -> p a n", a=8)),
                          reads=[btb], writes=[rt_b])
                sc.dma_store(retT_t.ap()[c, :, :], rt[:, :, :].rearrange("p a n -> p (a n)"), reads=[rt_b], writes=[retT_b])
            return fin

        AHEAD = 3
        for c in range(min(AHEAD + 1, NT)):
            load(c)
        for c in range(min(AHEAD, NT - 1)):
            P1(c)
        phaseA(0)
        phaseA2(0)
        prev = None
        for c in range(NT):
            if c + AHEAD + 1 < NT:
                load(c + AHEAD + 1)
            if c + 1 < NT:
                phaseA(c + 1)
            f = phaseB(c)
            if prev is not None:
                prev()
            prev = f
            if c + AHEAD < NT - 1:
                P1(c + AHEAD)
            if c + 1 < NT:
                phaseA2(c + 1)
            if side is not None and c >= 2:
                next(side, None)
        prev()
        if side is not None:
            for _ in side:
                pass


GELU_C = 0.7978845608028654


def stage3_gen(cx, I, SC, kcmpT, kcmpT_b, vext, vext_b):
    nc, sc = cx.nc, cx.sc
    with contextlib.ExitStack() as st:
        cT = {}
        for nm in ("ckT", "cvT"):
            for g in range(2):
                t, b = cx.sb(st, "c3_%s%d" % (nm, g), [128, S], BF16)
                sc.dma("sp", t[:], SC[nm][0].ap()[g, :, :], reads=[SC[nm][1]], writes=[b])
                cT[(nm, g)] = (t, b)
        sc.op("pool", lambda e: e.memset(kcmpT[:], 0.0), writes=[kcmpT_b])
        for g in range(2):
            for nt_ in range(2):
                sc.dma("sp", vext[:, g, nt_, 128:193], I["vext_const"][nt_, :, :], writes=[vext_b])
        yield
        for kv, nm in ((0, "ckT"), (1, "cvT")):
            sfx = "k" if kv == 0 else "v"
            w1, w1_b = cx.sb(st, "c3w1" + sfx, [128, 32, 128], BF16)
            w2, w2_b = cx.sb(st, "c3w2" + sfx, [128, 128], BF16)
            peT, peT_b = cx.sb(st, "c3pe" + sfx, [128, 32], BF16)
            bias, bias_b = cx.sb(st, "c3b" + sfx, [128, 1], F32)
            sc.dma("pool", w1[:], I["cmp_w1_" + sfx].rearrange("l d e -> d l e"), writes=[w1_b])
            sc.dma("pool", w2[:], I["cmp_w2_" + sfx][:, :], writes=[w2_b])
            sc.dma("pool", peT[:], I["cmp_peT_" + sfx][:, :], writes=[peT_b])
            bt, bb = cx.bank()
            for l in range(32):
                mm(cx, bb, bt[:, 0:1], w1[:, l, :], peT[:, l:l + 1], l == 0, l == 31, [w1_b, peT_b])
            sc.op("act", lambda e: e.copy(out=bias[:], in_=bt[:, 0:1]), reads=[bb], writes=[bias_b])
            yield
            for g in range(2):
                ct, ct_b = cT[(nm, g)]
                ht, hb = cx.bank()
                for l in range(32):
                    mm(cx, hb, ht[:, 0:NCMP], w1[:, l, :], ct[:, l:l + 16 * (NCMP - 1) + 1:16],
                       l == 0, l == 31, [w1_b, ct_b])
                xh, xh_b = cx.sb(st, "c3xh%s%d" % (sfx, g), [128, 256], F32)
                u, u_b = cx.sb(st, "c3u%s%d" % (sfx, g), [128, 256], F32)
                hid, hid_b = cx.sb(st, "c3hid%s%d" % (sfx, g), [128, 256], BF16)
                sc.op("pool", lambda e: e.memset(hid[:], 0.0), writes=[hid_b])
                n = NCMP
                sc.op("act", lambda e: e.activation(out=xh[:, 0:n], in_=ht[:, 0:n], func=AF.Identity,
                                                    bias=bias[:, 0:1], scale=1.0),
                      reads=[hb, bias_b], writes=[xh_b])
                yield
                sc.op("dve", lambda e: e.tensor_tensor(out=u[:, 0:n], in0=xh[:, 0:n], in1=xh[:, 0:n], op=ALU.mult),
                      reads=[xh_b], writes=[u_b])
                sc.op("dve", lambda e: e.tensor_scalar(out=u[:, 0:n], in0=u[:, 0:n], scalar1=0.044715, scalar2=1.0,
                                                       op0=ALU.mult, op1=ALU.add),
                      reads=[u_b], writes=[u_b])
                sc.op("dve", lambda e: e.tensor_tensor(out=u[:, 0:n], in0=u[:, 0:n], in1=xh[:, 0:n], op=ALU.mult),
                      reads=[u_b, xh_b], writes=[u_b])
                sc.op("act", lambda e: e.activation(out=u[:, 0:n], in_=u[:, 0:n], func=AF.Sigmoid,
                                                    scale=2.0 * GELU_C),
                      reads=[u_b], writes=[u_b])
                sc.op("dve", lambda e: e.tensor_tensor(out=hid[:, 0:n], in0=u[:, 0:n], in1=xh[:, 0:n], op=ALU.mult),
                      reads=[u_b, xh_b], writes=[hid_b])
                yield
                if kv == 0:
                    ot, ob = cx.bank()
                    mm(cx, ob, ot[:, 0:256], w2[:, :], hid[:, :], True, True, [w2_b, hid_b])
                    sc.op("act", lambda e: e.copy(out=kcmpT[:, g, 0:n], in_=ot[:, 0:n]), reads=[ob], writes=[kcmpT_b])
                else:
                    for nt_ in range(2):
                        ot, ob = cx.bank()
                        mm(cx, ob, ot[:, 0:128], hid[:, nt_ * 128:(nt_ + 1) * 128], w2[:, :], True, True, [w2_b, hid_b])
                        sc.op("act", lambda e: e.copy(out=vext[:, g, nt_, 0:128], in_=ot[:, 0:128]),
                              reads=[ob], writes=[vext_b])


def stage3(cx, I, SC, kcmpT, kcmpT_b, vext, vext_b):
    for _ in stage3_gen(cx, I, SC, kcmpT, kcmpT_b, vext, vext_b):
        pass


def stage4(cx, I, SC, kcmpT, kcmpT_b, vext, vext_b, HM):
    nc, sc = cx.nc, cx.sc
    scale = 128 ** -0.5
    with contextlib.ExitStack() as st:
        ident, ident_b = cx.sb(st, "ident4", [128, 128], BF16)
        sc.dma("sp", ident[:], I["ident_bf"][:, :], writes=[ident_b])
        skT, skT_b = cx.sb(st, "skTs", [128, 2, S], BF16)
        wkT, wkT_b = cx.sb(st, "wkTs", [128, 2, S], BF16)
        for g in range(2):
            sc.dma("sp", skT[:, g, :], SC["skT"][0].ap()[g, :, :], reads=[SC["skT"][1]], writes=[skT_b])
            sc.dma("sp", wkT[:, g, :], SC["wkT"][0].ap()[g, :, :], reads=[SC["wkT"][1]], writes=[wkT_b])
        svx, svx_b = cx.sb(st, "svx", [128, NT, 2, 129], BF16)
        wvx, wvx_b = cx.sb(st, "wvx", [128, NT, 2, 129], BF16)
        sc.op("pool", lambda e: e.memset(svx[:, :, :, 128:129], 1.0), writes=[svx_b])
        sc.op("pool", lambda e: e.memset(wvx[:, :, :, 128:129], 1.0), writes=[wvx_b])
        svwv = SC["svwv"][0].ap()
        for t in range(NT):
            rows = slice(t * 128, (t + 1) * 128)
            sc.dma("sp", svx[:, t, :, 0:128], svwv[rows, 0:256].rearrange("p (g d) -> p g d", g=2),
                   reads=[SC["svwv"][1]], writes=[svx_b])
            sc.dma("sp", wvx[:, t, :, 0:128], svwv[rows, 256:512].rearrange("p (g d) -> p g d", g=2),
                   reads=[SC["svwv"][1]], writes=[wvx_b])
        mb, mb_b = cx.sb(st, "maskb", [128, 8, 512], BF16)
        sc.dma("sp", mb[:], I["mask_bias"][:, :, :], writes=[mb_b])
        eexp, eexp_b = cx.sb(st, "eexp", [128, S], BF16)
        sc.dma("sp", eexp[:], I["eexp"][:, :], writes=[eexp_b])
        cms = cx.sbpool(st, "cmT", [128, 2, 512], BF16, 2)
        nqs = cx.sbpool(st, "nq", [128, 8, 512], BF16, 2)
        gls = cx.sbpool(st, "gls", [128, 4, 24], F32, 2)
        ims = cx.sbpool(st, "impm", [128, 4, 2, 2, 64], F32, 2)
        obr_sets = [[cx.sb(st, "obr%d_%d" % (p_, b), [128, 4, 1024], BF16) for b in range(3)] for p_ in range(2)]
        obr_cur = [obr_sets[0]]
        imp, imp_b = cx.sb(st, "imp", [128, 4, 2, 64], F32)
        pts = cx.sbpool(st, "pt", [128, 512], BF16, 6)
        smalls = cx.sbpool(st, "sm4", [128, 8], F32, 8)
        sw1, sw1_b = cx.sb(st, "sw1", [128, 4, 2, 64], F32)
        sw2, sw2_b = cx.sb(st, "sw2", [128, 4, 2, 64], F32)
        kn, kn_b = cx.sb(st, "kn", [128, 4, 2, 64], F32)
        knb = [Buf("kn%d" % i) for i in range(8)]
        m8a = [cx.sb(st, "m8a%d" % i, [128, 8], F32) for i in range(8)]
        m8b = [cx.sb(st, "m8b%d" % i, [128, 8], F32) for i in range(8)]
        selb = [cx.sb(st, "selb%d" % i, [128, 64], BF16) for i in range(8)]
        selT, selT_b = cx.sb(st, "selT", [128, 2, 512], BF16)
        sc.op("pool", lambda e: e.memset(selT[:], 0.0), writes=[selT_b])
        ots = cx.sbpool(st, "ot4", [128, 8, 128], BF16, 4)
        nsaT_t, nsaT_b = SC["nsaT"]
        SB0, SB1 = 0, 3
        ACC = [[cx.banks[3], cx.banks[4]], [cx.banks[5], cx.banks[6]]]
        MISC = cx.banks[7]
        acc_set = [0]

        def evac(aset, h, br, gl, gl_b, oset, cmp_imp=None):
            ob_t, ob_b = oset[br]
            gcol = 3 * h + br
            for bi in range(2):
                at, ab = aset[bi]
                sm, sm_b = smalls.next()
                zc = at[:, 128:512:256]
                if br == 0:
                    sc.op("dve", lambda e: e.tensor_scalar(out=sm[:, 0:2], in0=zc, scalar1=1e-30, scalar2=None, op0=ALU.max),
                          reads=[ab], writes=[sm_b])
                    sc.op("dve", lambda e: e.reciprocal(out=sm[:, 2:4], in_=sm[:, 0:2]), reads=[sm_b], writes=[sm_b])
                else:
                    sc.op("dve", lambda e: e.reciprocal(out=sm[:, 2:4], in_=zc), reads=[ab], writes=[sm_b])
                sc.op("dve", lambda e: e.tensor_tensor(out=sm[:, 4:6], in0=sm[:, 2:4], in1=gl[:, 2 * bi:2 * bi + 2, gcol],
                                                       op=ALU.mult),
                      reads=[sm_b, gl_b], writes=[sm_b])
                for jj in range(2):
                    j = 2 * bi + jj
                    src = at[:, jj * 256:jj * 256 + 128]
                    dst = ob_t[:, j, h * 128:(h + 1) * 128]
                    sc.op("dve", lambda e: e.tensor_scalar(out=dst, in0=src, scalar1=sm[:, 4 + jj:5 + jj], scalar2=None,
                                                           op0=ALU.mult),
                          reads=[ab, sm_b], writes=[ob_b])
                    if cmp_imp is not None:
                        g, firsth = cmp_imp
                        isrc = at[:, jj * 256 + 129:jj * 256 + 193]
                        idst = imp[:, j, g, :]
                        if firsth:
                            sc.op("dve", lambda e: e.tensor_scalar(out=idst, in0=isrc, scalar1=sm[:, 2 + jj:3 + jj],
                                                                   scalar2=None, op0=ALU.mult),
                                  reads=[ab, sm_b], writes=[imp_b])
                        else:
                            sc.op("dve", lambda e: e.scalar_tensor_tensor(out=idst, in0=isrc, scalar=sm[:, 2 + jj:3 + jj],
                                                                          in1=idst, op0=ALU.mult, op1=ALU.add),
                                  reads=[ab, sm_b, imp_b], writes=[imp_b])

        def pv(aset, started, j, width, lhsT, rhs, rd):
            bi, jj = j // 2, j % 2
            at, ab = aset[bi]
            first = not started[bi]
            started[bi] = True
            mm(cx, ab, at[:, jj * 256:jj * 256 + width], lhsT, rhs, first, False, rd)

        def run_stream(items, L=2):
            pend = []
            for it in items:
                if "plain" in it:
                    it["plain"]()
                    continue
                pend.append((it, it["score"]()))
                if len(pend) > L:
                    it0, c0 = pend.pop(0)
                    it0["pv"](c0)
            for it0, c0 in pend:
                it0["pv"](c0)

        def make_item(h, br, lhsT, lhs_rd, nq, nq_b, extras, vop, v_rd, width, slices, state, last, gl, gl_b, cmp_imp, oset):
            cs = slice(min(slices) * 128, (max(slices) + 1) * 128)

            def score():
                bt, bb = cx.bank(SB0, SB1)
                mm(cx, bb, bt[:, cs], lhsT, nq[:, h, cs], True, len(extras) == 0, lhs_rd + [nq_b])
                for ei, (l_, r_, rd) in enumerate(extras):
                    mm(cx, bb, bt[:, cs], l_, r_[:, cs], False, ei == len(extras) - 1, rd)
                pt, pt_b = pts.next()
                sc.op("act", lambda e: e.activation(out=pt[:, cs], in_=bt[:, cs], func=AF.Exp, scale=scale),
                      reads=[bb], writes=[pt_b])
                return pt, pt_b

            def pvf(c):
                pt, pt_b = c
                if state["aset"] is None:
                    state["aset"] = ACC[acc_set[0]]
                    acc_set[0] ^= 1
                    state["started"] = [False, False]
                for j in slices:
                    pv(state["aset"], state["started"], j, width, pt[:, j * 128:(j + 1) * 128], vop, [pt_b] + v_rd)
                if last:
                    evac(state["aset"], h, br, gl, gl_b, oset, cmp_imp=cmp_imp)
            return {"score": score, "pv": pvf}

        NQL = int(_os.environ.get("STG4Q", NQ))
        cm_valid = HM["cm_valid"]

        def do_loads(q):
            qc = slice(q * 512, (q + 1) * 512)
            nq, nq_b = nqs.next()
            gl, gl_b = gls.next()
            im, im_b = ims.next()
            cmT, cmT_b = cms.next()
            sc.dma("sp", nq[:], SC["nqT"][0].ap()[:, :, qc].rearrange("h p n -> p h n"),
                   reads=[SC["nqT"][1]], writes=[nq_b])
            sc.dma("sp", gl[:], SC["gl"][0].ap()[qc, :].rearrange("(j p) c -> p j c", p=128),
                   reads=[SC["gl"][1]], writes=[gl_b])
            sc.dma("sp", im[:], I["imp_masks"][qc, :, :, :].rearrange("(j p) a g c -> p j a g c", p=128), writes=[im_b])
            sc.dma("sp", cmT[:], I["cmp_bias"][:, :, qc], writes=[cmT_b])
            return dict(nq=nq, nq_b=nq_b, gl=gl, gl_b=gl_b, im=im, im_b=im_b, cmT=cmT, cmT_b=cmT_b)

        def cmp_items(q, L, oset):
            nq, nq_b, gl, gl_b, cmT, cmT_b = L["nq"], L["nq_b"], L["gl"], L["gl_b"], L["cmT"], L["cmT_b"]
            nts = [n_ for n_ in range(2) if cm_valid[n_][:, q * 512:(q + 1) * 512].any()]
            items = []
            for h in range(8):
                g = h // 4
                state = {"aset": None}
                for n_ in nts:
                    allv = bool(cm_valid[n_][:, q * 512:(q + 1) * 512].all())
                    extras = [] if allv else [(ident[:], cmT[:, n_, :], [ident_b, cmT_b])]
                    items.append(make_item(h, 0, kcmpT[:, g, n_ * 128:(n_ + 1) * 128], [kcmpT_b], nq, nq_b, extras,
                                           vext[:, g, n_, :], [vext_b], 193, [0, 1, 2, 3], state, n_ == nts[-1],
                                           gl, gl_b, (g, h % 4 == 0), oset))
            return items

        def make_fin(q, oset):
            def fin():
                for j in range(4):
                    mt, mbk = MISC
                    ot, ot_b = ots.next()
                    for half in range(2):
                        for c4 in range(4):
                            c = half * 4 + c4
                            for b_ in range(3):
                                mm(cx, mbk, mt[:, c4 * 128:(c4 + 1) * 128], oset[b_][0][:, j, c * 128:(c + 1) * 128], ident[:],
                                   c4 == 0 and b_ == 0, False, [oset[b_][1], ident_b])
                        src = mt[:, :].rearrange("p (a n) -> p a n", a=4)
                        if half:
                            sc.op("act", lambda e: e.copy(out=ot[:, 4:8, :], in_=src), reads=[mbk], writes=[ot_b])
                        else:
                            sc.op("dve", lambda e: e.tensor_copy(out=ot[:, 0:4, :], in_=src), reads=[mbk], writes=[ot_b])
                    rows = slice(q * 512 + j * 128, q * 512 + (j + 1) * 128)
                    sc.dma_store(nsaT_t.ap()[q * 4 + j, :, :], ot[:, :, :].rearrange("p a n -> p (a n)"), reads=[ot_b], writes=[nsaT_b])
            return fin

        LQ = {0: do_loads(0)}
        run_stream(cmp_items(0, LQ[0], obr_sets[0]))
        prev_fin = None
        for q in range(NQL):
            L = LQ.pop(q)
            nq, nq_b, gl, gl_b, im, im_b = L["nq"], L["nq_b"], L["gl"], L["gl_b"], L["im"], L["im_b"]
            oset = obr_sets[q % 2]
            if prev_fin is not None:
                prev_fin()
            sc.op("dve", lambda e: e.tensor_tensor(out=sw1[:], in0=imp[:], in1=im[:, :, 0, :, :], op=ALU.mult),
                  reads=[imp_b, im_b], writes=[sw1_b])
            sc.op("dve", lambda e: e.tensor_tensor(out=sw1[:], in0=sw1[:], in1=im[:, :, 1, :, :], op=ALU.add),
                  reads=[sw1_b, im_b], writes=[sw1_b])
            for g in range(2):
                for j in range(4):
                    i8 = g * 4 + j
                    sc.op("dve", lambda e: e.max(out=m8a[i8][0][:, :], in_=sw1[:, j, g, :]), reads=[sw1_b], writes=[m8a[i8][1]])
            for g in range(2):
                for j in range(4):
                    i8 = g * 4 + j
                    sc.op("dve", lambda e: e.tensor_scalar(out=kn[:, j, g, :], in0=sw1[:, j, g, :], scalar1=m8a[i8][0][:, 7:8],
                                                           scalar2=-1e9, op0=ALU.is_ge, op1=ALU.mult),
                          reads=[sw1_b, m8a[i8][1]], writes=[knb[i8]])
            sc.op("dve", lambda e: e.tensor_tensor(out=sw2[:], in0=kn[:], in1=sw1[:], op=ALU.add),
                  reads=knb + [sw1_b], writes=[sw2_b])
            for g in range(2):
                for j in range(4):
                    i8 = g * 4 + j
                    sc.op("dve", lambda e: e.max(out=m8b[i8][0][:, :], in_=sw2[:, j, g, :]), reads=[sw2_b], writes=[m8b[i8][1]])
            for g in range(2):
                for j in range(4):
                    i8 = g * 4 + j
                    sc.op("dve", lambda e: e.tensor_scalar(out=selb[i8][0][:, :], in0=sw1[:, j, g, :], scalar1=m8b[i8][0][:, 7:8],
                                                           scalar2=-1.0, op0=ALU.is_ge, op1=ALU.add),
                          reads=[sw1_b, m8b[i8][1]], writes=[selb[i8][1]])

            def sel_transposes():
                mt, mbk = MISC
                pm = mt[:, :].bitcast(BF16)
                for i8 in range(8):
                    tr(cx, mbk, pm[0:64, i8 * 128:(i8 + 1) * 128], selb[i8][0][:, :], ident[:], [selb[i8][1], ident_b])
                sc.op("act", lambda e: e.copy(out=selT[0:64, :, :], in_=pm[0:64, :].rearrange("p (g n) -> p g n", g=2)),
                      reads=[mbk], writes=[selT_b])
            if q + 1 < NQL:
                LQ[q + 1] = do_loads(q + 1)
            items = []
            for br in (2, 1):
                if br == 1:
                    if q + 1 < NQL:
                        items.extend(cmp_items(q + 1, LQ[q + 1], obr_sets[(q + 1) % 2]))
                    items.append({"plain": sel_transposes})
                kT, kT_b = (skT, skT_b) if br == 1 else (wkT, wkT_b)
                vx, vx_b = (svx, svx_b) if br == 1 else (wvx, wvx_b)
                kt_lo = 0 if br == 1 else max(0, 4 * q - 4)
                kts = list(range(kt_lo, 4 * q + 4))
                for h in range(8):
                    g = h // 4
                    state = {"aset": None}
                    for kt in kts:
                        r = kt - 4 * q
                        ks = slice(kt * 128, (kt + 1) * 128)
                        extras = []
                        if br == 1:
                            extras.append((eexp[:, ks], selT[:, g, :], [eexp_b, selT_b]))
                            if r >= 0:
                                extras.append((ident[:], mb[:, r, :], [ident_b, mb_b]))
                        else:
                            mi = r if r >= 0 else 8 + r
                            extras.append((ident[:], mb[:, mi, :], [ident_b, mb_b]))
                        slices = [j for j in range(4)
                                  if (0 if br == 1 else max(0, 4 * q + j - 4)) <= kt <= 4 * q + j]
                        items.append(make_item(h, br, kT[:, g, ks], [kT_b], nq, nq_b, extras, vx[:, kt, g, :], [vx_b],
                                               129, slices, state, kt == kts[-1], gl, gl_b, None, oset))
            run_stream(items)
            prev_fin = make_fin(q, oset)
        if prev_fin is not None:
            prev_fin()


def load_w_bf(cx, st, name, src_ap, kchunks, ncols):
    t, b = cx.sb(st, name, [128, kchunks, ncols], BF16)
    v = src_ap.rearrange("(k p) c -> p k c", p=128)
    step = max(1, 4096 // ncols)
    for k0 in range(0, kchunks, step):
        cx.sc.dma("pool", t[:, k0:k0 + step, :], v[:, k0:k0 + step, :], writes=[b])
    return t, b


def rms_rows(cx, xt, xt_b, gbc, gbc_b, out_t, out_b, junk, junk_b, sm, sm_b):
    sc = cx.sc
    sc.op("act", lambda e: e.activation(out=junk[:], in_=xt[:], func=AF.Square, accum_out=sm[:, 0:1]),
          reads=[xt_b], writes=[junk_b, sm_b])
    sc.op("act", lambda e: e.activation(out=sm[:, 1:2], in_=sm[:, 0:1], func=AF.Sqrt,
                                        bias=EPS_AP(cx), scale=1.0 / D),
          reads=[sm_b, cx.eps_b], writes=[sm_b])
    sc.op("dve", lambda e: e.reciprocal(out=sm[:, 2:3], in_=sm[:, 1:2]), reads=[sm_b], writes=[sm_b])
    sc.op("dve", lambda e: e.scalar_tensor_tensor(out=out_t[:], in0=xt[:], scalar=sm[:, 2:3], in1=gbc[:],
                                                  op0=ALU.mult, op1=ALU.mult),
          reads=[xt_b, sm_b, gbc_b], writes=[out_b])


def stage5(cx, I, SC):
    nc, sc = cx.nc, cx.sc
    with contextlib.ExitStack() as st:
        wro, wro_b = load_w_bf(cx, st, "w_ret_o", I["w_ret_o"], 8, 1024)
        wno, wno_b = load_w_bf(cx, st, "w_nsa_o", I["w_nsa_o"], 8, 1024)
        wou, wou_b = load_w_bf(cx, st, "w_out", I["w_out"], 8, 1024)
        ins = {nm: cx.sbpool(st, "s5" + nm, [128, 8, 512], BF16, 2) for nm in ("gaT", "gbT")}
        ins.update({nm: cx.sbpool(st, "s5" + nm, [128, 4, 8, 128], BF16, 2) for nm in ("retT", "nsaT")})
        yTs = cx.sbpool(st, "s5y", [128, 8, 512], BF16, 2)
        t1s = cx.sbpool(st, "s5t1", [128, 512], F32, 2)
        t2s = cx.sbpool(st, "s5t2", [128, 512], F32, 2)
        xts = cx.sbpool(st, "s5x", [128, D], F32, 2)
        xos = cx.sbpool(st, "s5xo", [128, D], F32, 2)
        for q in range(NQ):
            qc = slice(q * 512, (q + 1) * 512)
            cur = {}
            for nm in ("retT", "nsaT"):
                t, b = ins[nm].next()
                sc.dma("sp", t[:, :, :, :], SC[nm][0].ap()[q * 4:q * 4 + 4, :, :].rearrange("j p (a n) -> p j a n", a=8),
                       reads=[SC[nm][1]], writes=[b])
                cur[nm] = (t, b)
            for nm in ("gaT", "gbT"):
                t, b = ins[nm].next()
                sc.dma("sp", t[:], SC[nm][0].ap()[:, :, qc].rearrange("a p n -> p a n"),
                       reads=[SC[nm][1]], writes=[b])
                cur[nm] = (t, b)
            yT, yT_b = yTs.next()
            for c in range(8):
                cs = slice(c * 128, (c + 1) * 128)
                at, ab = cx.bank()
                bt, bb = cx.bank()
                for k in range(8):
                    mm(cx, ab, at[:, :].rearrange("p (j n) -> p j n", j=4), wro[:, k, cs], cur["retT"][0][:, :, k, :], k == 0, k == 7, [wro_b, cur["retT"][1]])
                for k in range(8):
                    mm(cx, bb, bt[:, :].rearrange("p (j n) -> p j n", j=4), wno[:, k, cs], cur["nsaT"][0][:, :, k, :], k == 0, k == 7, [wno_b, cur["nsaT"][1]])
                t1, t1_b = t1s.next()
                t2, t2_b = t2s.next()
                sc.op("dve", lambda e: e.tensor_tensor(out=t1[:], in0=at[:, :], in1=cur["gaT"][0][:, c, :], op=ALU.mult),
                      reads=[ab, cur["gaT"][1]], writes=[t1_b])
                sc.op("dve", lambda e: e.tensor_tensor(out=t2[:], in0=bt[:, :], in1=cur["gbT"][0][:, c, :], op=ALU.mult),
                      reads=[bb, cur["gbT"][1]], writes=[t2_b])
                sc.op("pool", lambda e: e.tensor_tensor(out=yT[:, c, :], in0=t1[:], in1=t2[:], op=ALU.add),
                      reads=[t1_b, t2_b], writes=[yT_b])
            for j in range(4):
                rows = slice(q * 512 + j * 128, q * 512 + (j + 1) * 128)
                xt, xt_b = xts.next()
                xo, xo_b = xos.next()
                sc.dma("sp", xt[:], I["x"][rows, :], writes=[xt_b])
                for half in range(2):
                    ot, ob = cx.bank()
                    for c in range(8):
                        mm(cx, ob, ot[:, :], yT[:, c, j * 128:(j + 1) * 128], wou[:, c, half * 512:(half + 1) * 512],
                           c == 0, c == 7, [yT_b, wou_b])
                    sc.op("dve", lambda e: e.tensor_tensor(out=xo[:, half * 512:(half + 1) * 512], in0=ot[:, :],
                                                           in1=xt[:, half * 512:(half + 1) * 512], op=ALU.add),
                          reads=[ob, xt_b], writes=[xo_b])
                sc.dma_store(SC["x1"][0].ap()[rows, :], xo[:], reads=[xo_b], writes=[SC["x1"][1]])


def stage6(cx, I, SC):
    nc, sc = cx.nc, cx.sc
    with contextlib.ExitStack() as st:
        ident, ident_b = cx.sb(st, "ident6", [128, 128], BF16)
        sc.dma("sp", ident[:], I["ident_bf"][:, :], writes=[ident_b])
        gx, gx_b = cx.sb(st, "g_x", [128, D], F32)
        gm, gm_b = cx.sb(st, "g_mem", [128, D], F32)
        sc.dma("sp", gx[:], I["g_x"][:, :], writes=[gx_b])
        sc.dma("sp", gm[:], I["g_mem"][:, :], writes=[gm_b])
        wq, wq_b = load_w_bf(cx, st, "w_xq", I["w_xq"], 8, 1024)
        wkv, wkv_b = load_w_bf(cx, st, "w_xkv", I["w_xkv"], 8, 2048)
        wo, wo_b = load_w_bf(cx, st, "w_xo", I["w_xo"], 8, 1024)
        junk, junk_b = cx.sb(st, "s6junk", [128, D], BF16)
        sms = cx.sbpool(st, "s6sm", [128, 4], F32, 8)
        xas = cx.sbpool(st, "s6xa", [128, D], F32, 4)
        hbs = cx.sbpool(st, "s6hb", [128, D], BF16, 6)
        memnT, memnT_b = cx.sb(st, "memnT", [128, 8, 256], BF16)
        kT, kT_b = cx.sb(st, "xkT", [128, 8, 256], BF16)
        vx, vx_b = cx.sb(st, "xvx", [128, 2, 4, 257], BF16)
        sc.op("pool", lambda e: e.memset(vx[:, :, :, 256:257], 1.0), writes=[vx_b])
        for mt in range(2):
            xt, xt_b = xas.next()
            hb, hb_b = hbs.next()
            sm, sm_b = sms.next()
            sc.dma("sp", xt[:, :], I["mem"][mt * 128:(mt + 1) * 128, :], writes=[xt_b])

            class _V:
                pass
            xv = xt[:, :]
            sc.op("act", lambda e: e.activation(out=junk[:], in_=xv, func=AF.Square, accum_out=sm[:, 0:1]),
                  reads=[xt_b], writes=[junk_b, sm_b])
            sc.op("act", lambda e: e.activation(out=sm[:, 1:2], in_=sm[:, 0:1], func=AF.Sqrt, bias=EPS_AP(cx), scale=1.0 / D),
                  reads=[sm_b, cx.eps_b], writes=[sm_b])
            sc.op("dve", lambda e: e.reciprocal(out=sm[:, 2:3], in_=sm[:, 1:2]), reads=[sm_b], writes=[sm_b])
            sc.op("dve", lambda e: e.scalar_tensor_tensor(out=hb[:], in0=xv, scalar=sm[:, 2:3], in1=gm[:],
                                                          op0=ALU.mult, op1=ALU.mult),
                  reads=[xt_b, sm_b, gm_b], writes=[hb_b])
            bt, bb = cx.bank()
            pb = bt[:, :].bitcast(BF16)
            for k in range(8):
                tr(cx, bb, pb[:, k * 128:(k + 1) * 128], hb[:, k * 128:(k + 1) * 128], ident[:], [hb_b, ident_b])
            sc.op("act", lambda e: e.copy(out=memnT[:, :, mt * 128:(mt + 1) * 128], in_=pb.rearrange("p (k n) -> p k n", k=8)),
                  reads=[bb], writes=[memnT_b])
        for c in range(8):
            bt, bb = cx.bank()
            for k in range(8):
                mm(cx, bb, bt[:, 0:256], wkv[:, k, c * 128:(c + 1) * 128], memnT[:, k, :], k == 0, k == 7, [wkv_b, memnT_b])
            sc.op("act", lambda e: e.copy(out=kT[:, c, :], in_=bt[:, 0:256]), reads=[bb], writes=[kT_b])
        for mt in range(2):
            for half in range(2):
                bt, bb = cx.bank()
                for k in range(8):
                    mm(cx, bb, bt[:, :], memnT[:, k, mt * 128:(mt + 1) * 128],
                       wkv[:, k, 1024 + half * 512:1024 + (half + 1) * 512], k == 0, k == 7, [wkv_b, memnT_b])
                sc.op("act", lambda e: e.copy(out=vx[:, mt, 2 * half:2 * half + 2, 0:256],
                                              in_=bt[:, :].rearrange("p (h d) -> p h d", h=2)),
                      reads=[bb], writes=[vx_b])
        xrs = cx.sbpool(st, "s6xr", [128, D], F32, 3)
        hxTs = cx.sbpool(st, "s6hxT", [128, 8, 512], BF16, 2)
        qTs = cx.sbpool(st, "s6qT", [128, 8, 512], BF16, 2)
        pts = cx.sbpool(st, "s6pt", [128, 512], BF16, 6)
        otm = cx.sbpool(st, "s6o", [128, 4, D], BF16, 2)
        oTs = cx.sbpool(st, "s6oT", [128, 8, 128], BF16, 3)
        xos = cx.sbpool(st, "s6xo", [128, D], F32, 2)
        zs = cx.sbpool(st, "s6z", [128, 2], F32, 8)
        scale = 256 ** -0.5
        def phaseA(q):
            hxT, hxT_b = hxTs.next()
            hbl = []
            for j in range(4):
                rows = slice(q * 512 + j * 128, q * 512 + (j + 1) * 128)
                x1t, x1t_b = xas.next()
                sc.dma("sp", x1t[:], SC["x1"][0].ap()[rows, :], reads=[SC["x1"][1]], writes=[x1t_b])
                hb, hb_b = hbs.next()
                sm, sm_b = sms.next()
                sc.op("act", lambda e: e.activation(out=junk[:], in_=x1t[:], func=AF.Square, accum_out=sm[:, 0:1]),
                      reads=[x1t_b], writes=[junk_b, sm_b])
                sc.op("act", lambda e: e.activation(out=sm[:, 1:2], in_=sm[:, 0:1], func=AF.Sqrt, bias=EPS_AP(cx), scale=1.0 / D),
                      reads=[sm_b, cx.eps_b], writes=[sm_b])
                sc.op("dve", lambda e: e.reciprocal(out=sm[:, 2:3], in_=sm[:, 1:2]), reads=[sm_b], writes=[sm_b])
                sc.op("dve", lambda e: e.scalar_tensor_tensor(out=hb[:], in0=x1t[:], scalar=sm[:, 2:3], in1=gx[:],
                                                              op0=ALU.mult, op1=ALU.mult),
                      reads=[x1t_b, sm_b, gx_b], writes=[hb_b])
                hbl.append((hb, hb_b))
            for j in range(4):
                hb, hb_b = hbl[j]
                bt, bb = cx.bank()
                pb = bt[:, :].bitcast(BF16)
                for k in range(8):
                    tr(cx, bb, pb[:, k * 128:(k + 1) * 128], hb[:, k * 128:(k + 1) * 128], ident[:], [hb_b, ident_b])
                if j % 2:
                    sc.op("act", lambda e: e.copy(out=hxT[:, :, j * 128:(j + 1) * 128], in_=pb.rearrange("p (k n) -> p k n", k=8)),
                          reads=[bb], writes=[hxT_b])
                else:
                    sc.op("dve", lambda e: e.tensor_copy(out=hxT[:, :, j * 128:(j + 1) * 128], in_=pb.rearrange("p (k n) -> p k n", k=8)),
                          reads=[bb], writes=[hxT_b])
            qT, qT_b = qTs.next()
            for c in range(8):
                bt, bb = cx.bank()
                for k in range(8):
                    mm(cx, bb, bt[:, :], wq[:, k, c * 128:(c + 1) * 128], hxT[:, k, :], k == 0, k == 7, [wq_b, hxT_b])
                if c % 2:
                    sc.op("act", lambda e: e.copy(out=qT[:, c, :], in_=bt[:, :]), reads=[bb], writes=[qT_b])
                else:
                    sc.op("dve", lambda e: e.tensor_copy(out=qT[:, c, :], in_=bt[:, :]), reads=[bb], writes=[qT_b])
            return (qT, qT_b)

        def phaseB(q, ctxA):
            qT, qT_b = ctxA
            o, o_b = otm.next()
            pend = []

            def emit_pv(it):
                h, mt, pt, pt_b = it
                for j in range(4):
                    at, ab = cx.banks[4 + j]
                    mm(cx, ab, at[:, 0:257], pt[:, j * 128:(j + 1) * 128], vx[:, mt, h, :], mt == 0, mt == 1, [pt_b, vx_b])
                    if mt == 1:
                        z, z_b = zs.next()
                        sc.op("dve", lambda e: e.reciprocal(out=z[:, 0:1], in_=at[:, 256:257]), reads=[ab], writes=[z_b])
                        if j % 2:
                            sc.op("act", lambda e: e.activation(out=o[:, j, h * 256:(h + 1) * 256], in_=at[:, 0:256], func=AF.Copy,
                                                                scale=z[:, 0:1]),
                                  reads=[ab, z_b], writes=[o_b])
                        else:
                            sc.op("dve", lambda e: e.tensor_scalar(out=o[:, j, h * 256:(h + 1) * 256], in0=at[:, 0:256],
                                                                   scalar1=z[:, 0:1], scalar2=None, op0=ALU.mult),
                                  reads=[ab, z_b], writes=[o_b])

            for h in range(4):
                for mt in range(2):
                    bt, bb = cx.bank(0, 4)
                    for dc in range(2):
                        mm(cx, bb, bt[:, :], kT[:, 2 * h + dc, mt * 128:(mt + 1) * 128], qT[:, 2 * h + dc, :],
                           dc == 0, dc == 1, [kT_b, qT_b])
                    pt, pt_b = pts.next()
                    sc.op("act", lambda e: e.activation(out=pt[:], in_=bt[:, :], func=AF.Exp, scale=scale),
                          reads=[bb], writes=[pt_b])
                    pend.append((h, mt, pt, pt_b))
                    if len(pend) > 2:
                        emit_pv(pend.pop(0))
            for it in pend:
                emit_pv(it)
            def emit_T(j):
                bt, bb = cx.bank(0, 4)
                pb = bt[:, :].bitcast(BF16)
                for c in range(8):
                    tr(cx, bb, pb[:, c * 128:(c + 1) * 128], o[:, j, c * 128:(c + 1) * 128], ident[:], [o_b, ident_b])
                oT, oT_b = oTs.next()
                if j % 2:
                    sc.op("act", lambda e: e.copy(out=oT[:], in_=pb.rearrange("p (a n) -> p a n", a=8)), reads=[bb], writes=[oT_b])
                else:
                    sc.op("dve", lambda e: e.tensor_copy(out=oT[:], in_=pb.rearrange("p (a n) -> p a n", a=8)), reads=[bb], writes=[oT_b])
                rows = slice(q * 512 + j * 128, q * 512 + (j + 1) * 128)
                xr, xr_b = xrs.next()
                sc.dma("sp", xr[:], SC["x1"][0].ap()[rows, :], reads=[SC["x1"][1]], writes=[xr_b])
                return oT, oT_b, xr, xr_b

            nxt = emit_T(0)
            for j in range(4):
                rows = slice(q * 512 + j * 128, q * 512 + (j + 1) * 128)
                oT, oT_b, xr, xr_b = nxt
                if j + 1 < 4:
                    nxt = emit_T(j + 1)
                xo, xo_b = xos.next()
                for half in range(2):
                    ot, ob = cx.bank(0, 4)
                    for c in range(8):
                        mm(cx, ob, ot[:, :], oT[:, c, :], wo[:, c, half * 512:(half + 1) * 512], c == 0, c == 7, [oT_b, wo_b])
                    sc.op("dve", lambda e: e.tensor_tensor(out=xo[:, half * 512:(half + 1) * 512], in0=ot[:, :],
                                                           in1=xr[:, half * 512:(half + 1) * 512], op=ALU.add),
                          reads=[ob, xr_b], writes=[xo_b])
                sc.dma_store(SC["x2"][0].ap()[rows, :], xo[:], reads=[xo_b], writes=[SC["x2"][1]])

        ctxs = {0: phaseA(0)}
        if NQ > 1:
            ctxs[1] = phaseA(1)
        for q in range(NQ):
            phaseB(q, ctxs.pop(q))
            if q + 2 < NQ:
                ctxs[q + 2] = phaseA(q + 2)


def stage7(cx, I, SC, out_ap, out_b):
    nc, sc = cx.nc, cx.sc
    IOA = bass.IndirectOffsetOnAxis
    with contextlib.ExitStack() as st:
        identf, identf_b = cx.sb(st, "identf", [128, 128], F32)
        ident, ident_b = cx.sb(st, "ident7", [128, 128], BF16)
        sc.dma("sp", identf[:], I["ident_f"][:, :], writes=[identf_b])
        sc.dma("sp", ident[:], I["ident_bf"][:, :], writes=[ident_b])
        gf, gf_b = cx.sb(st, "g_ffn", [128, D], F32)
        gfin, gfin_b = cx.sb(st, "g_fin", [128, D], F32)
        sc.dma("sp", gf[:], I["g_ffn"][:, :], writes=[gf_b])
        sc.dma("sp", gfin[:], I["g_fin"][:, :], writes=[gfin_b])
        wr, wr_b = cx.sb(st, "wrouter", [128, 8, 36], F32)
        sc.dma("sp", wr[:], I["w_router"].rearrange("(k p) c -> p k c", p=128), writes=[wr_b])
        brow, brow_b = cx.sb(st, "brow", [1, 36], F32)
        sc.dma("sp", brow[:], I["b_router"][:, :], writes=[brow_b])
        onesr, onesr_b = cx.sb(st, "onesr", [1, 128], F32)
        sc.op("dve", lambda e: e.memset(onesr[:], 1.0), writes=[onesr_b])
        ustr, ustr_b = cx.sb(st, "ustr", [128, 128], BF16)
        sc.dma("sp", ustr[:], I["ustrict"][:, :], writes=[ustr_b])
        onesb, onesb_b = cx.sb(st, "onesb", [128, 128], BF16)
        sc.op("pool", lambda e: e.memset(onesb[:], 1.0), writes=[onesb_b])
        slotb, slotb_b = cx.sb(st, "slotb", [128, 32], F32)
        sc.dma("sp", slotb[:], I["slotbase"][:, :], writes=[slotb_b])
        tokid, tokid_b = cx.sb(st, "tokid", [128, NT], I32)
        sc.dma("sp", tokid[:], I["tokid"][:, :], writes=[tokid_b])
        cnt, cnt_b = cx.sb(st, "cnt", [128, 32], F32)
        sc.op("dve", lambda e: e.memset(cnt[:], 0.0), writes=[cnt_b])
        dest, dest_b = cx.sb(st, "dest", [128, NT, 2], I32)
        wall, wall_b = cx.sb(st, "wall", [128, NT, 2], F32)
        zi, zi_b = cx.sb(st, "zi", [128, 96], I32)
        sc.op("pool", lambda e: e.memset(zi[:], 0), writes=[zi_b])
        btok_t, btok_b = SC["buf_tok"]
        binit = Buf("btok_init")
        sc.dma("sp", btok_t.ap().rearrange("(p c) o -> p (c o)", p=128), zi[:], reads=[zi_b], writes=[binit])
        hfbf_t, hfbf_b = SC["hf_bf"]
        ys_t, ys_b = SC["ys"]
        junk, junk_b = cx.sb(st, "s7junk", [128, D], BF16)
        with contextlib.ExitStack() as st2:
            xts = cx.sbpool(st2, "s7x", [128, D], F32, 3)
            hfs = cx.sbpool(st2, "s7hf", [128, D], F32, 3)
            hbs = cx.sbpool(st2, "s7hb", [128, D], BF16, 3)
            hT2s = cx.sbpool(st2, "s7hT2", [128, 2, 8, 128], BF16, 3)
            los = cx.sbpool(st2, "s7lo", [128, D], BF16, 3)
            whi, whi_b = cx.sb(st2, "s7whi", [128, 8, 36], BF16)
            wlo, wlo_b = cx.sb(st2, "s7wlo", [128, 8, 36], BF16)
            sc.op("dve", lambda e: e.tensor_copy(out=whi[:], in_=wr[:]), reads=[wr_b], writes=[whi_b])
            sc.op("dve", lambda e: e.tensor_tensor(out=wlo[:], in0=wr[:], in1=whi[:], op=ALU.subtract),
                  reads=[wr_b, whi_b], writes=[wlo_b])
            sms = cx.sbpool(st2, "s7sm", [128, 4], F32, 3)
            LG, LG_b = cx.sb(st2, "s7LG", [128, NT, 36], F32)
            def a7(t):
                rows = slice(t * 128, (t + 1) * 128)
                xt, xt_b = xts.next(); hf, hf_b = hfs.next(); hb, hb_b = hbs.next()
                sm, sm_b = sms.next()
                sc.dma("sp", xt[:], SC["x2"][0].ap()[rows, :], reads=[SC["x2"][1]], writes=[xt_b])
                rms_rows(cx, xt, xt_b, gf, gf_b, hf, hf_b, junk, junk_b, sm, sm_b)
                sc.op("pool", lambda e: e.tensor_copy(out=hb[:], in_=hf[:]), reads=[hf_b], writes=[hb_b])
                sc.dma_store(hfbf_t.ap()[rows, :], hb[:], reads=[hb_b], writes=[hfbf_b])
                lo, lo_b = los.next()
                sc.op("pool", lambda e: e.tensor_tensor(out=lo[:], in0=hf[:], in1=hb[:], op=ALU.subtract),
                      reads=[hf_b, hb_b], writes=[lo_b])
                return hb, hb_b, lo, lo_b

            pre7 = {0: a7(0), 1: a7(1)}
            for t in range(NT):
                hb, hb_b, lo, lo_b = pre7.pop(t)
                hT2, hT2_b = hT2s.next()
                for part, (src, src_b) in enumerate(((hb, hb_b), (lo, lo_b))):
                    bt, bb = cx.bank()
                    pb = bt[:, :].bitcast(BF16)
                    for k in range(8):
                        tr(cx, bb, pb[:, k * 128:(k + 1) * 128], src[:, k * 128:(k + 1) * 128], ident[:], [src_b, ident_b])
                    srcv = pb.rearrange("p (k n) -> p k n", k=8)
                    if part == 0:
                        sc.op("act", lambda e: e.copy(out=hT2[:, 0, :, :], in_=srcv), reads=[bb], writes=[hT2_b])
                    else:
                        sc.op("dve", lambda e: e.tensor_copy(out=hT2[:, 1, :, :], in_=srcv), reads=[bb], writes=[hT2_b])
                lt, lb = cx.bank()
                for k in range(8):
                    mm(cx, lb, lt[:, 0:36], hT2[:, 0, k, :], whi[:, k, :], k == 0, False, [hT2_b, whi_b])
                    mm(cx, lb, lt[:, 0:36], hT2[:, 0, k, :], wlo[:, k, :], False, False, [hT2_b, wlo_b])
                    mm(cx, lb, lt[:, 0:36], hT2[:, 1, k, :], whi[:, k, :], False, False, [hT2_b, whi_b])
                mm(cx, lb, lt[:, 0:36], onesr[:, :], brow[:, :], False, True, [onesr_b, brow_b])
                sc.op("act", lambda e: e.copy(out=LG[:, t, :], in_=lt[:, 0:36]), reads=[lb], writes=[LG_b])
                if t + 2 < NT:
                    pre7[t + 2] = a7(t + 2)
            def T(name, shape, dt=F32):
                return cx.sb(st2, "s7_" + name, shape, dt)
            MX, MX_b = T("MX", [128, NT]); E4, E4_b = T("E4", [128, NT, 4]); PEN, PEN_b = T("PEN", [128, NT, 4])
            EX, EX_b = T("EX", [128, NT, 4]); SE, SE_b = T("SE", [128, NT]); GG, GG_b = T("GG", [128, NT])
            LEM, LEM_b = T("LEM", [128, NT, 32]); L1, L1_b = T("L1", [128, NT]); M1, M1_b = T("M1", [128, NT, 32])
            LEM2, LEM2_b = T("LEM2", [128, NT, 32]); L2, L2_b = T("L2", [128, NT]); M2, M2_b = T("M2", [128, NT, 32])
            DL, DL_b = T("DL", [128, NT]); SG, SG_b = T("SG", [128, NT]); MB, MB_b = T("MB", [128, NT, 32], BF16)
            RS, RS_b = T("RS", [128, NT, 32]); TMP, TMP_b = T("TMP", [128, NT, 32]); DF, DF_b = T("DF", [128, NT, 2])
            sc.op("dve", lambda e: e.tensor_reduce(out=MX[:], in_=LG[:, :, 0:4], axis=AX.X, op=ALU.max), reads=[LG_b], writes=[MX_b])
            sc.op("dve", lambda e: e.tensor_tensor(out=E4[:], in0=LG[:, :, 0:4], in1=MX[:, :].unsqueeze(2).broadcast_to([128, NT, 4]),
                                                   op=ALU.subtract), reads=[LG_b, MX_b], writes=[E4_b])
            sc.op("dve", lambda e: e.tensor_scalar(out=PEN[:], in0=E4[:], scalar1=1e12, scalar2=None, op0=ALU.mult),
                  reads=[E4_b], writes=[PEN_b])
            sc.op("act", lambda e: e.activation(out=EX[:], in_=E4[:], func=AF.Exp), reads=[E4_b], writes=[EX_b])
            sc.op("dve", lambda e: e.tensor_reduce(out=SE[:], in_=EX[:], axis=AX.X, op=ALU.add), reads=[EX_b], writes=[SE_b])
            sc.op("dve", lambda e: e.reciprocal(out=GG[:], in_=SE[:]), reads=[SE_b], writes=[GG_b])
            sc.op("dve", lambda e: e.tensor_tensor(out=LEM[:, :, :].rearrange("p t (g i) -> p t g i", g=4),
                                                   in0=LG[:, :, 4:36].rearrange("p t (g i) -> p t g i", g=4),
                                                   in1=PEN[:, :, :].unsqueeze(3).broadcast_to([128, NT, 4, 8]), op=ALU.add),
                  reads=[LG_b, PEN_b], writes=[LEM_b])
            sc.op("dve", lambda e: e.tensor_reduce(out=L1[:], in_=LEM[:], axis=AX.X, op=ALU.max), reads=[LEM_b], writes=[L1_b])
            sc.op("dve", lambda e: e.tensor_tensor(out=M1[:], in0=LEM[:], in1=L1[:, :].unsqueeze(2).broadcast_to([128, NT, 32]),
                                                   op=ALU.is_ge), reads=[LEM_b, L1_b], writes=[M1_b])
            sc.op("dve", lambda e: e.scalar_tensor_tensor(out=LEM2[:], in0=M1[:], scalar=-1e9, in1=LEM[:], op0=ALU.mult, op1=ALU.add),
                  reads=[M1_b, LEM_b], writes=[LEM2_b])
            sc.op("dve", lambda e: e.tensor_reduce(out=L2[:], in_=LEM2[:], axis=AX.X, op=ALU.max), reads=[LEM2_b], writes=[L2_b])
            sc.op("dve", lambda e: e.tensor_tensor(out=M2[:], in0=LEM2[:], in1=L2[:, :].unsqueeze(2).broadcast_to([128, NT, 32]),
                                                   op=ALU.is_ge), reads=[LEM2_b, L2_b], writes=[M2_b])
            sc.op("dve", lambda e: e.tensor_tensor(out=DL[:], in0=L1[:], in1=L2[:], op=ALU.subtract), reads=[L1_b, L2_b], writes=[DL_b])
            sc.op("act", lambda e: e.activation(out=SG[:], in_=DL[:], func=AF.Sigmoid), reads=[DL_b], writes=[SG_b])
            sc.op("dve", lambda e: e.tensor_tensor(out=wall[:, :, 0], in0=GG[:], in1=SG[:], op=ALU.mult),
                  reads=[GG_b, SG_b], writes=[wall_b])
            sc.op("dve", lambda e: e.tensor_tensor(out=wall[:, :, 1], in0=GG[:], in1=wall[:, :, 0], op=ALU.subtract),
                  reads=[GG_b, wall_b], writes=[wall_b])
            sc.op("dve", lambda e: e.tensor_tensor(out=MB[:], in0=M1[:], in1=M2[:], op=ALU.add), reads=[M1_b, M2_b], writes=[MB_b])
            for hb_ in range(2):
                rk, rkb = cx.bank()
                for tt in range(16):
                    t = hb_ * 16 + tt
                    oap = rk[:, tt * 32:(tt + 1) * 32]
                    mm(cx, rkb, oap, ustr[:], MB[:, t, :], tt == 0, False, [ustr_b, MB_b])
                    for tp in range(t):
                        mm(cx, rkb, oap, onesb[:], MB[:, tp, :], False, False, [onesb_b, MB_b])
                sc.op("dve", lambda e: e.scalar_tensor_tensor(out=RS[:, hb_ * 16:(hb_ + 1) * 16, :],
                                                              in0=rk[:, :].rearrange("p (t e) -> p t e", t=16),
                                                              scalar=float(CAP - 1),
                                                              in1=slotb[:, :].unsqueeze(1).broadcast_to([128, 16, 32]),
                                                              op0=ALU.min, op1=ALU.add),
                      reads=[rkb, slotb_b], writes=[RS_b])
            for k_, (MK, MK_b) in enumerate(((M1, M1_b), (M2, M2_b))):
                sc.op("dve", lambda e: e.tensor_tensor(out=TMP[:], in0=MK[:], in1=RS[:], op=ALU.mult), reads=[MK_b, RS_b], writes=[TMP_b])
                sc.op("dve", lambda e: e.tensor_reduce(out=DF[:, :, k_], in_=TMP[:], axis=AX.X, op=ALU.add), reads=[TMP_b], writes=[DF_b])
            sc.op("dve", lambda e: e.tensor_copy(out=dest[:], in_=DF[:]), reads=[DF_b], writes=[dest_b])
            for t in range(NT):
                for k_ in range(2):
                    sc.idma(out=btok_t.ap()[:, :], out_offset=IOA(ap=dest[:, t, k_:k_ + 1], axis=0),
                            in_=tokid[:, t:t + 1], in_offset=None,
                            reads=[dest_b, tokid_b, binit], writes=[btok_b])
        sc.barrier()
        with contextlib.ExitStack() as st2:
            w1s = cx.sbpool(st2, "we1", [128, 8, 512], BF16, 2)
            w3s = cx.sbpool(st2, "we3", [128, 8, 512], BF16, 2)
            w2s = cx.sbpool(st2, "we2", [128, 4, 1024], BF16, 2)
            idxs = cx.sbpool(st2, "s7idx", [128, 1], I32, 8)
            xbs = cx.sbpool(st2, "s7xb", [128, D], BF16, 6)
            xbTs = cx.sbpool(st2, "s7xbT", [128, 8, CAP], BF16, 2)
            sils = cx.sbpool(st2, "s7sil", [128, CAP], F32, 4)
            acts = cx.sbpool(st2, "s7act", [128, 4, CAP], BF16, 2)
            ybs = cx.sbpool(st2, "s7yb", [128, D], BF16, 4)
            def prep(ex):
                w1, w1_b = w1s.next(); w3, w3_b = w3s.next(); w2, w2_b = w2s.next()
                v1 = I["w_e1"][ex].rearrange("(k p) c -> p k c", p=128)
                v3 = I["w_e3"][ex].rearrange("(k p) c -> p k c", p=128)
                v2 = I["w_e2"][ex].rearrange("(k p) c -> p k c", p=128)
                sc.dma("pool", w1[:], v1, writes=[w1_b])
                sc.dma("pool", w3[:], v3, writes=[w3_b])
                sc.dma("pool", w2[:], v2, writes=[w2_b])
                xbT, xbT_b = xbTs.next()
                for blk in range(CAP // 128):
                    r0 = ex * CAP + blk * 128
                    idx, idx_b = idxs.next()
                    xb, xb_b = xbs.next()
                    sc.dma("sp", idx[:], btok_t.ap()[r0:r0 + 128, :], reads=[btok_b], writes=[idx_b])
                    sc.idma(out=xb[:, :], out_offset=None, in_=hfbf_t.ap()[:, :], in_offset=IOA(ap=idx[:, 0:1], axis=0),
                            reads=[idx_b, hfbf_b], writes=[xb_b])
                    bt, bb = cx.bank()
                    pb = bt[:, :].bitcast(BF16)
                    for k in range(8):
                        tr(cx, bb, pb[:, k * 128:(k + 1) * 128], xb[:, k * 128:(k + 1) * 128], ident[:], [xb_b, ident_b])
                    if blk % 2:
                        sc.op("dve", lambda e: e.tensor_copy(out=xbT[:, :, blk * 128:(blk + 1) * 128],
                                                             in_=pb.rearrange("p (k n) -> p k n", k=8)),
                              reads=[bb], writes=[xbT_b])
                    else:
                        sc.op("act", lambda e: e.copy(out=xbT[:, :, blk * 128:(blk + 1) * 128], in_=pb.rearrange("p (k n) -> p k n", k=8)),
                              reads=[bb], writes=[xbT_b])
                return dict(w1=w1, w1_b=w1_b, w3=w3, w3_b=w3_b, w2=w2, w2_b=w2_b, xbT=xbT, xbT_b=xbT_b)

            def main1(ex, P):
                w1, w1_b, w3, w3_b, xbT, xbT_b = P["w1"], P["w1_b"], P["w3"], P["w3_b"], P["xbT"], P["xbT_b"]
                act, act_b = acts.next()
                for c in range(4):
                    cs = slice(c * 128, (c + 1) * 128)
                    at, ab = cx.bank()
                    b3t, b3b = cx.bank()
                    for k in range(8):
                        mm(cx, ab, at[:, 0:CAP], w1[:, k, cs], xbT[:, k, :], k == 0, k == 7, [w1_b, xbT_b])
                    for k in range(8):
                        mm(cx, b3b, b3t[:, 0:CAP], w3[:, k, cs], xbT[:, k, :], k == 0, k == 7, [w3_b, xbT_b])
                    sl, sl_b = sils.next()
                    sc.op("act", lambda e: e.activation(out=sl[:], in_=at[:, 0:CAP], func=AF.Silu), reads=[ab], writes=[sl_b])
                    sc.op("dve", lambda e: e.tensor_tensor(out=act[:, c, :], in0=b3t[:, 0:CAP], in1=sl[:], op=ALU.mult),
                          reads=[b3b, sl_b], writes=[act_b])
                P["act"], P["act_b"] = act, act_b

            def main2(ex, P):
                act, act_b, w2, w2_b = P["act"], P["act_b"], P["w2"], P["w2_b"]
                for blk in range(CAP // 128):
                    r0 = ex * CAP + blk * 128
                    yb, yb_b = ybs.next()
                    for half in range(2):
                        ot, ob = cx.bank()
                        for c in range(4):
                            mm(cx, ob, ot[:, :], act[:, c, blk * 128:(blk + 1) * 128], w2[:, c, half * 512:(half + 1) * 512],
                               c == 0, c == 3, [act_b, w2_b])
                        if half == 0:
                            sc.op("act", lambda e: e.copy(out=yb[:, 0:512], in_=ot[:, :]), reads=[ob], writes=[yb_b])
                        else:
                            sc.op("dve", lambda e: e.tensor_copy(out=yb[:, 512:1024], in_=ot[:, :]), reads=[ob], writes=[yb_b])
                    sc.dma_store(ys_t.ap()[r0:r0 + 128, :], yb[:], reads=[yb_b], writes=[ys_b])

            PX = {0: prep(0)}
            for ex in range(NEXP):
                main1(ex, PX[ex])
                if ex + 1 < NEXP:
                    PX[ex + 1] = prep(ex + 1)
                main2(ex, PX.pop(ex))
        sc.barrier()
        with contextlib.ExitStack() as st2:
            xts = cx.sbpool(st2, "s7fx", [128, D], F32, 4)
            y1s = cx.sbpool(st2, "s7y1", [128, D], BF16, 4)
            y2s = cx.sbpool(st2, "s7y2", [128, D], BF16, 4)
            x3s = cx.sbpool(st2, "s7x3", [128, D], F32, 4)
            ous = cx.sbpool(st2, "s7ou", [128, D], F32, 4)
            sms = cx.sbpool(st2, "s7fsm", [128, 4], F32, 4)
            for t in range(NT):
                rows = slice(t * 128, (t + 1) * 128)
                xt, xt_b = xts.next(); y1, y1_b = y1s.next(); y2, y2_b = y2s.next()
                x3, x3_b = x3s.next(); ou, ou_b = ous.next(); sm, sm_b = sms.next()
                sc.dma("sp", xt[:], SC["x2"][0].ap()[rows, :], reads=[SC["x2"][1]], writes=[xt_b])
                sc.idma(out=y1[:, :], out_offset=None, in_=ys_t.ap()[:, :], in_offset=IOA(ap=dest[:, t, 0:1], axis=0),
                        reads=[dest_b, ys_b], writes=[y1_b])
                sc.idma(out=y2[:, :], out_offset=None, in_=ys_t.ap()[:, :], in_offset=IOA(ap=dest[:, t, 1:2], axis=0),
                        reads=[dest_b, ys_b], writes=[y2_b])
                sc.op("dve", lambda e: e.scalar_tensor_tensor(out=x3[:], in0=y1[:], scalar=wall[:, t, 0:1], in1=xt[:],
                                                              op0=ALU.mult, op1=ALU.add),
                      reads=[y1_b, wall_b, xt_b], writes=[x3_b])
                sc.op("dve", lambda e: e.scalar_tensor_tensor(out=x3[:], in0=y2[:], scalar=wall[:, t, 1:2], in1=x3[:],
                                                              op0=ALU.mult, op1=ALU.add),
                      reads=[y2_b, wall_b, x3_b], writes=[x3_b])
                rms_rows(cx, x3, x3_b, gfin, gfin_b, ou, ou_b, junk, junk_b, sm, sm_b)
                sc.dma_store(out_ap[rows, :], ou[:], reads=[ou_b], writes=[out_b])


def EPS_AP(cx):
    return cx.eps[:, 0:1]


def build(debug=None):
    nc = bass.Bass("TRN2", target_bir_lowering=False)
    cx = Ctx(nc)
    sc = cx.sc
    I = {}

    def inp(name, shape, dt):
        I[name] = nc.dram_tensor(name, shape, dt, kind="ExternalInput").ap()

    inp("x", [S, D], F32)
    inp("w_in", [D, D_IN], F32)
    inp("g_mix", [128, D], F32)
    inp("ident_bf", [128, 128], BF16)
    inp("rope_q_tab", [S, 2, 512], F32)
    inp("rope_k_tab", [S, 2, 512], F32)
    inp("decayT", [128, 512], F32)
    inp("xi_full", [128, 512], F32)
    inp("zeta_full", [128, 512], F32)
    for sfx in ("k", "v"):
        inp("cmp_w1_" + sfx, [32, 128, 128], F32)
        inp("cmp_w2_" + sfx, [128, 128], F32)
        inp("cmp_peT_" + sfx, [128, 32], F32)
    inp("vext_const", [2, 128, 65], BF16)
    inp("mask_bias", [128, 8, 512], BF16)
    inp("cmp_bias", [128, 2, S], BF16)
    inp("eexp", [128, S], BF16)
    inp("imp_masks", [S, 2, 2, 64], F32)
    for nm in ("w_ret_o", "w_nsa_o", "w_out", "w_xq", "w_xo"):
        inp(nm, [D, D], F32)
    inp("w_xkv", [D, 2 * D], F32)
    inp("mem", [256, D], F32)
    inp("g_x", [128, D], F32)
    inp("g_mem", [128, D], F32)
    inp("g_ffn", [128, D], F32)
    inp("g_fin", [128, D], F32)
    inp("ident_f", [128, 128], F32)
    inp("w_router", [D, 36], F32)
    inp("b_router", [1, 36], F32)
    inp("ustrict", [128, 128], BF16)
    inp("slotbase", [128, 32], F32)
    inp("tokid", [128, NT], I32)
    inp("w_e1", [NEXP, D, 512], F32)
    inp("w_e3", [NEXP, D, 512], F32)
    inp("w_e2", [NEXP, 512, D], F32)
    out = nc.dram_tensor("out", [S, D], F32, kind="ExternalOutput")
    SC = {}
    for name, shape, dt in [
        ("q_r", [S, 512], BF16), ("k_r", [S, 512], BF16), ("v_r", [S, 1024], BF16),
        ("g_r", [S, 1024], BF16), ("svwv", [S, 512], BF16), ("gl", [S, 24], F32),
        ("nqT", [8, 128, S], BF16), ("ckT", [2, 128, S], BF16), ("cvT", [2, 128, S], BF16),
        ("skT", [2, 128, S], BF16), ("wkT", [2, 128, S], BF16),
        ("gaT", [8, 128, S], BF16), ("gbT", [8, 128, S], BF16),
        ("retT", [NT, 128, D], BF16), ("nsaT", [NT, 128, D], BF16),
        ("x1", [S, D], F32), ("x2", [S, D], F32),
        ("hf_bf", [S, D], BF16), ("ys", [NEXP * CAP, D], BF16), ("buf_tok", [NEXP * CAP, 1], I32),
        ("dbg_kcmpT", [128, 2, 256], BF16), ("dbg_vext", [128, 2, 2, 193], BF16),
    ]:
        if debug and name in debug:
            t = nc.dram_tensor(name, shape, dt, kind="ExternalOutput")
            SC[name] = (t, Buf(name, multi=True))
        else:
            SC[name] = cx.dram(name, shape, dt)
    cx.eps, cx.eps_b = cx.sb(cx.es, "eps", [128, 1], F32)
    sc.op("dve", lambda e: e.memset(cx.eps[:], EPS), writes=[cx.eps_b])

    stop = int(_os.environ.get("STOPAFTER", 99))
    stage1(cx, I, SC)
    sc.barrier()
    if stop <= 1:
        return nc
    kcmpT, kcmpT_b = cx.sb(cx.es, "kcmpT", [128, 2, 256], BF16)
    vext, vext_b = cx.sb(cx.es, "vext", [128, 2, 2, 193], BF16)
    stage2(cx, I, SC, side=stage3_gen(cx, I, SC, kcmpT, kcmpT_b, vext, vext_b))
    sc.barrier()
    if stop <= 3:
        return nc
    HM = host_masks()
    stage4(cx, I, SC, kcmpT, kcmpT_b, vext, vext_b, HM)
    sc.barrier()
    if stop <= 4:
        return nc
    stage5(cx, I, SC)
    sc.barrier()
    if stop <= 5:
        return nc
    stage6(cx, I, SC)
    sc.barrier()
    if stop <= 6:
        return nc
    out_b = Buf("out", multi=True)
    stage7(cx, I, SC, out.ap(), out_b)
    sc.finish([out_b])
    if debug and "dbg_kcmpT" in debug:
        sc.dma("sp", SC["dbg_kcmpT"][0].ap()[:, :, :], kcmpT[:], reads=[kcmpT_b], writes=[SC["dbg_kcmpT"][1]])
        sc.dma("sp", SC["dbg_vext"][0].ap()[:, :, :, :], vext[:], reads=[vext_b], writes=[SC["dbg_vext"][1]])

    sc.finish([b for (_, b) in SC.values()])
    return nc


def host_consts():
    c = {}
    c["ident_bf"] = np.eye(128, dtype=np.float32).astype(ml_dtypes.bfloat16)
    pos = np.arange(S, dtype=np.float32)
    inv_freq = (10000.0 ** (-np.arange(0, 128, 2, dtype=np.float32) / 128)).astype(np.float32)
    ang = pos[:, None] * inv_freq[None, :]
    cos = np.cos(ang).astype(np.float32)
    sin = np.sin(ang).astype(np.float32)
    tq = np.stack([np.tile(cos, (1, 8)), np.tile(sin, (1, 8))], axis=1)
    c["rope_q_tab"] = np.ascontiguousarray(tq, dtype=np.float32)
    c["rope_k_tab"] = np.ascontiguousarray(tq * np.float32(128 ** -0.5), dtype=np.float32)
    n = np.arange(128, dtype=np.float64)
    decT = np.zeros((128, 4, 128), np.float64)
    xi = np.zeros((128, 4, 128), np.float64)
    ze = np.zeros((128, 4, 128), np.float64)
    for h in range(4):
        lg = np.log(GAMMAS[h])
        diff = n[None, :] - n[:, None]
        decT[:, h, :] = np.where(diff >= 0, np.exp(lg * np.maximum(diff, 0.0)), 0.0)
        xi[:, h, :] = np.exp(lg * (n + 1.0))[:, None]
        ze[:, h, :] = np.exp(lg * (127.0 - n))[:, None]
    c["decayT"] = decT.reshape(128, 512).astype(np.float32)
    c["xi_full"] = xi.reshape(128, 512).astype(np.float32)
    c["zeta_full"] = ze.reshape(128, 512).astype(np.float32)
    cstart = np.arange(256) * 16
    jstart = np.arange(NB) * 64
    ov = ((cstart[:, None] < jstart[None, :] + 64) & (cstart[:, None] + 32 > jstart[None, :])).astype(np.float32)
    ov[255, :] = 0.0
    vc = np.concatenate([np.ones((256, 1), np.float32), ov], axis=1)
    c["vext_const"] = np.ascontiguousarray(vc.reshape(2, 128, 65)).astype(ml_dtypes.bfloat16)
    return c


def host_masks():
    t = np.arange(S)
    n = np.arange(256)
    valid = (16 * n[:, None] + 31 <= t[None, :]) & (n[:, None] < NCMP)
    return {"cm_valid": [valid[0:128], valid[128:256]]}


def host_consts2():
    c = {}
    hm = host_masks()
    cb = np.where(np.stack(hm["cm_valid"], axis=1), 0.0, NEG).astype(np.float32)
    c["cmp_bias"] = np.ascontiguousarray(cb).astype(ml_dtypes.bfloat16)
    m = np.arange(128)[:, None]
    nn = np.arange(512)[None, :]
    mbias = np.zeros((128, 8, 512), np.float32)
    for r in range(4):
        mbias[:, r, :] = np.where(128 * r + m <= nn, 0.0, NEG)
    for r in range(-4, 0):
        mbias[:, 8 + r, :] = np.where(128 * r + m > nn - 512, 0.0, NEG)
    c["mask_bias"] = mbias.astype(ml_dtypes.bfloat16)
    e = np.zeros((128, S), np.float32)
    e[np.arange(S) // 64, np.arange(S)] = -NEG
    c["eexp"] = e.astype(ml_dtypes.bfloat16)
    t = np.arange(S)
    tb = t // 64
    jj = np.arange(NB)
    forced = (jj[None, :] == 0) | (jj[None, :] == tb[:, None]) | (jj[None, :] == tb[:, None] - 1)
    future = jj[None, :] > tb[:, None]
    m1 = np.where(future | forced, 0.0, 1.0)
    m2 = np.where(future, -1e4, np.where(forced, 1e4, 0.0))
    mm_ = np.stack([m1, m2], axis=1)[:, :, None, :]
    c["imp_masks"] = np.ascontiguousarray(np.broadcast_to(mm_, (S, 2, 2, NB))).astype(np.float32)
    c["ident_f"] = np.eye(128, dtype=np.float32)
    c["ustrict"] = np.triu(np.ones((128, 128), np.float32), 1).astype(ml_dtypes.bfloat16)
    c["slotbase"] = np.ascontiguousarray(np.broadcast_to((np.arange(32) * CAP).astype(np.float32)[None, :], (128, 32)))
    c["tokid"] = np.ascontiguousarray((np.arange(NT)[None, :] * 128 + np.arange(128)[:, None]).astype(np.int32))
    return c


def permute_w_in(w):
    offs = np.cumsum([0, 512, 512, 1024, 1024, 1024, 256, 256, 256, 256, 256, 256, 24, 1024, 1024])
    rq, rk, rv, rg, nq, ck, cv, sk, sv, wk, wv, ngl, ga, gb = [np.arange(offs[i], offs[i + 1]) for i in range(14)]
    hp = np.concatenate([np.arange(0, 128, 2), np.arange(1, 128, 2)])
    perm4 = np.concatenate([h * 128 + hp for h in range(4)])
    order = np.concatenate([rq[perm4], rk[perm4], rv, rg, sv, wv, ngl, nq, ck, cv, sk, wk, ga, gb])
    return np.ascontiguousarray(w[:, order])


def kernel(**inputs):
    f = lambda a: np.ascontiguousarray(np.asarray(a, dtype=np.float32))
    bc = lambda v: np.ascontiguousarray(np.broadcast_to(f(v).reshape(1, D), (128, D)))
    shared = {}
    shared.update(host_consts())
    shared.update(host_consts2())
    shared["w_in"] = permute_w_in(f(inputs["w_in"])[0])
    shared["g_mix"] = bc(inputs["norm_mix_g"][0])
    for sfx in ("k", "v"):
        shared["cmp_w1_" + sfx] = f(inputs["cmp_w1_" + sfx][0])
        shared["cmp_w2_" + sfx] = f(inputs["cmp_w2_" + sfx][0])
        shared["cmp_peT_" + sfx] = np.ascontiguousarray(f(inputs["cmp_pe_" + sfx][0]).T)
    for nm in ("w_ret_o", "w_nsa_o", "w_out", "w_xq", "w_xo", "w_xkv"):
        shared[nm] = f(inputs[nm][0])
    shared["g_x"] = bc(inputs["norm_x_g"][0])
    shared["g_mem"] = bc(inputs["norm_mem_g"][0])
    shared["g_ffn"] = bc(inputs["norm_ffn_g"][0])
    shared["g_fin"] = bc(inputs["norm_f_g"])
    shared["w_router"] = np.ascontiguousarray(np.concatenate([f(inputs["w_grp"][0]), f(inputs["w_rt"][0])], axis=1))
    shared["b_router"] = np.ascontiguousarray(np.concatenate([f(inputs["b_grp"][0]), f(inputs["b_rt"][0])])[None, :])
    for nm in ("w_e1", "w_e3", "w_e2"):
        shared[nm] = f(inputs[nm][0])
    x = f(inputs["x"])
    mem = f(inputs["mem"])
    n = x.shape[0]
    in_maps = []
    for b in range(n):
        m = dict(shared)
        m["x"] = np.ascontiguousarray(x[b])
        m["mem"] = np.ascontiguousarray(mem[b])
        in_maps.append(m)
    nc = build()
    res = run_bass_kernel_spmd(nc, in_maps, core_ids=list(range(n)))
    return np.stack([np.asarray(r["out"], dtype=np.float32) for r in res.results], axis=0)
```
